# Optimizing a Trainium2 kernel written in Bass

```python
import math
import jax, jax.numpy as jnp
from jax import lax
import numpy as np

D_MODEL = 1024
BATCH = 8
SEQ = 2048
DEPTH = 2

N_A_LAYERS = DEPTH // 2
N_B_LAYERS = DEPTH - N_A_LAYERS
D_FF = 2816
MACARON_WEIGHT = 0.5
D_RNN = D_MODEL
LRU_HEADS = 4
LRU_BLOCK = D_RNN // LRU_HEADS
CONV_WIDTH = 4
LRU_C = 8.0
N_HEADS = 8
HEAD_DIM = D_MODEL // N_HEADS
MOBA_BLOCK = 256
MOBA_TOPK = 3
Q_CHUNK = 16
N_BUCKETS = 32
MAX_DISTANCE = 128
RMS_EPS = 1e-6

kernel_name = "yoco_rglru_moba_macaron"


def rms_norm(x, g):
    xf = x.astype(jnp.float32)
    y = xf * lax.rsqrt(jnp.mean(xf * xf, axis=-1, keepdims=True) + RMS_EPS)
    return (y * g.astype(jnp.float32)).astype(x.dtype)


def swiglu(x, w_gate, w_up, w_down):
    return (jax.nn.silu(x @ w_gate) * (x @ w_up)) @ w_down


def macaron_ffn(x, pre_g, post_g, w_gate, w_up, w_down):
    h = swiglu(rms_norm(x, pre_g), w_gate, w_up, w_down)
    return x + MACARON_WEIGHT * rms_norm(h, post_g)


def causal_dwconv(x, w, b):
    c = x.shape[-1]
    out = lax.conv_general_dilated(
        x, w[:, None, :].astype(x.dtype), window_strides=(1,),
        padding=[(CONV_WIDTH - 1, 0)], dimension_numbers=('NWC', 'WIO', 'NWC'),
        feature_group_count=c)
    return out + b


def rg_lru(x, w_r, b_r, w_i, b_i, lam):
    bn, s, c = x.shape
    xh = x.reshape(bn, s, LRU_HEADS, LRU_BLOCK)
    r = jax.nn.sigmoid(jnp.einsum('bshi,hij->bshj', xh, w_r).reshape(bn, s, c) + b_r)
    i = jax.nn.sigmoid(jnp.einsum('bshi,hij->bshj', xh, w_i).reshape(bn, s, c) + b_i)
    log_a = -LRU_C * r.astype(jnp.float32) * jax.nn.softplus(-lam.astype(jnp.float32))
    a = jnp.exp(log_a)
    mult = jnp.sqrt(-jnp.expm1(2.0 * log_a))
    u = mult * (i * x).astype(jnp.float32)

    def combine(left, right):
        a_l, b_l = left
        a_r, b_r2 = right
        return a_l * a_r, a_r * b_l + b_r2

    _, h = lax.associative_scan(combine, (a, u), axis=1)
    return h.astype(x.dtype)


def recurrent_block(xn, w_in, b_in, conv_w, conv_b, w_r, b_r, w_i, b_i, lam, w_out, b_out):
    proj = xn @ w_in + b_in
    xb, yb = jnp.split(proj, 2, axis=-1)
    xb = causal_dwconv(xb, conv_w, conv_b)
    h = rg_lru(xb, w_r, b_r, w_i, b_i, lam)
    return (h * jax.nn.gelu(yb)) @ w_out + b_out


def t5_bucket(dist):
    n = jnp.maximum(dist, 0)
    max_exact = N_BUCKETS // 2
    nf = jnp.maximum(n, 1).astype(jnp.float32)
    large = max_exact + (jnp.log(nf / max_exact) / math.log(MAX_DISTANCE / max_exact)
                         * (N_BUCKETS - max_exact)).astype(jnp.int32)
    large = jnp.minimum(large, N_BUCKETS - 1)
    return jnp.where(n < max_exact, n, large)


def shared_kv(x, kv_norm_g, w_kv):
    bn, s, _ = x.shape
    nblk = -(-s // MOBA_BLOCK)
    kv = rms_norm(x, kv_norm_g) @ w_kv
    k, v = jnp.split(kv, 2, axis=-1)
    pad = nblk * MOBA_BLOCK - s
    k = jnp.pad(k, ((0, 0), (0, pad), (0, 0)))
    v = jnp.pad(v, ((0, 0), (0, pad), (0, 0)))
    k_blk = k.reshape(bn, nblk, MOBA_BLOCK, N_HEADS, HEAD_DIM).transpose(0, 3, 1, 2, 4)
    v_blk = v.reshape(bn, nblk, MOBA_BLOCK, N_HEADS, HEAD_DIM).transpose(0, 3, 1, 2, 4)
    k_mean = jnp.mean(k_blk, axis=3)
    return k_blk, v_blk, k_mean


def moba_attention(q, k_blk, v_blk, k_mean, rel_bias):
    bn, s = q.shape[0], q.shape[1]
    nblk = k_blk.shape[2]
    topk = min(MOBA_TOPK, max(nblk - 1, 1))
    pos = jnp.arange(s)
    cur = pos // MOBA_BLOCK
    gate = jnp.einsum('bshd,bhnd->bhsn', q, k_mean).astype(jnp.float32)
    past = jnp.arange(nblk)[None, :] < cur[:, None]
    gate = jnp.where(past, gate, -jnp.inf)
    _, idx = lax.top_k(gate, topk)
    valid = idx < cur[None, None, :, None]

    nc = s // Q_CHUNK
    q_c = q.transpose(0, 2, 1, 3).reshape(bn, N_HEADS, nc, Q_CHUNK, HEAD_DIM).transpose(2, 0, 1, 3, 4)
    idx_c = idx.reshape(bn, N_HEADS, nc, Q_CHUNK, topk).transpose(2, 0, 1, 3, 4)
    valid_c = valid.reshape(bn, N_HEADS, nc, Q_CHUNK, topk).transpose(2, 0, 1, 3, 4)
    starts = jnp.arange(nc) * Q_CHUNK
    b_ix = jnp.arange(bn)[:, None, None, None]
    h_ix = jnp.arange(N_HEADS)[None, :, None, None]
    offs = jnp.arange(MOBA_BLOCK)
    scale = HEAD_DIM ** -0.5
    n_sel = topk * MOBA_BLOCK

    def chunk(args):
        qc, ic, vc, start = args
        t = start + jnp.arange(Q_CHUNK)
        ks = k_blk[b_ix, h_ix, ic]
        vs = v_blk[b_ix, h_ix, ic]
        kpos = ic[..., None] * MOBA_BLOCK + offs
        bias_s = rel_bias[h_ix[..., None], t5_bucket(t[:, None, None] - kpos)]
        s_sel = jnp.einsum('bhqd,bhqkjd->bhqkj', qc, ks).astype(jnp.float32) * scale + bias_s
        s_sel = jnp.where(vc[..., None], s_sel, -jnp.inf)
        cb = start // MOBA_BLOCK
        k_own = lax.dynamic_index_in_dim(k_blk, cb, axis=2, keepdims=False)
        v_own = lax.dynamic_index_in_dim(v_blk, cb, axis=2, keepdims=False)
        d_own = t[:, None] - (cb * MOBA_BLOCK + offs)[None, :]
        bias_o = rel_bias[:, t5_bucket(d_own)]
        s_own = jnp.einsum('bhqd,bhjd->bhqj', qc, k_own).astype(jnp.float32) * scale + bias_o
        s_own = jnp.where(d_own >= 0, s_own, -jnp.inf)
        logits = jnp.concatenate([s_sel.reshape(bn, N_HEADS, Q_CHUNK, n_sel), s_own], axis=-1)
        p = jax.nn.softmax(logits, axis=-1)
        p_sel = p[..., :n_sel].reshape(bn, N_HEADS, Q_CHUNK, topk, MOBA_BLOCK).astype(vs.dtype)
        p_own = p[..., n_sel:].astype(v_own.dtype)
        return (jnp.einsum('bhqkj,bhqkjd->bhqd', p_sel, vs)
                + jnp.einsum('bhqj,bhjd->bhqd', p_own, v_own))

    out = lax.map(chunk, (q_c, idx_c, valid_c, starts))
    return out.transpose(1, 0, 3, 2, 4).reshape(bn, s, N_HEADS * HEAD_DIM)


def setup_inputs(seed: int = 0) -> dict:
    key = jax.random.key(seed)
    ks = iter(jax.random.split(key, 64))

    def nrm(shape, scale):
        return jax.random.normal(next(ks), shape, jnp.float32) * scale

    def gain(shape):
        return 1.0 + nrm(shape, 0.05)

    d, f = D_MODEL, D_FF
    u = jax.random.uniform(next(ks), (N_A_LAYERS, D_RNN), jnp.float32, 0.9, 0.999)
    return {
        "x": nrm((BATCH, SEQ, d), 1.0),
        "ffn1_pre_g": gain((DEPTH, d)),
        "ffn1_post_g": gain((DEPTH, d)),
        "ffn1_w_gate": nrm((DEPTH, d, f), d ** -0.5),
        "ffn1_w_up": nrm((DEPTH, d, f), d ** -0.5),
        "ffn1_w_down": nrm((DEPTH, f, d), f ** -0.5),
        "ffn2_pre_g": gain((DEPTH, d)),
        "ffn2_post_g": gain((DEPTH, d)),
        "ffn2_w_gate": nrm((DEPTH, d, f), d ** -0.5),
        "ffn2_w_up": nrm((DEPTH, d, f), d ** -0.5),
        "ffn2_w_down": nrm((DEPTH, f, d), f ** -0.5),
        "mix_pre_g": gain((DEPTH, d)),
        "mix_post_g": gain((DEPTH, d)),
        "lru_w_in": nrm((N_A_LAYERS, d, 2 * D_RNN), d ** -0.5),
        "lru_b_in": nrm((N_A_LAYERS, 2 * D_RNN), 0.02),
        "lru_conv_w": nrm((N_A_LAYERS, CONV_WIDTH, D_RNN), CONV_WIDTH ** -0.5),
        "lru_conv_b": nrm((N_A_LAYERS, D_RNN), 0.02),
        "lru_w_r": nrm((N_A_LAYERS, LRU_HEADS, LRU_BLOCK, LRU_BLOCK), LRU_BLOCK ** -0.5),
        "lru_b_r": nrm((N_A_LAYERS, D_RNN), 0.02),
        "lru_w_i": nrm((N_A_LAYERS, LRU_HEADS, LRU_BLOCK, LRU_BLOCK), LRU_BLOCK ** -0.5),
        "lru_b_i": nrm((N_A_LAYERS, D_RNN), 0.02),
        "lru_lambda": jnp.log(u / (1.0 - u)),
        "lru_w_out": nrm((N_A_LAYERS, D_RNN, d), D_RNN ** -0.5),
        "lru_b_out": nrm((N_A_LAYERS, d), 0.02),
        "kv_norm_g": gain((d,)),
        "w_kv": nrm((d, 2 * N_HEADS * HEAD_DIM), d ** -0.5),
        "attn_w_q": nrm((N_B_LAYERS, d, N_HEADS * HEAD_DIM), d ** -0.5),
        "attn_w_o": nrm((N_B_LAYERS, N_HEADS * HEAD_DIM, d), (N_HEADS * HEAD_DIM) ** -0.5),
        "rel_bias": nrm((N_HEADS, N_BUCKETS), 0.5),
    }


def reference(x, ffn1_pre_g, ffn1_post_g, ffn1_w_gate, ffn1_w_up, ffn1_w_down,
              ffn2_pre_g, ffn2_post_g, ffn2_w_gate, ffn2_w_up, ffn2_w_down,
              mix_pre_g, mix_post_g,
              lru_w_in, lru_b_in, lru_conv_w, lru_conv_b, lru_w_r, lru_b_r, lru_w_i, lru_b_i,
              lru_lambda, lru_w_out, lru_b_out,
              kv_norm_g, w_kv, attn_w_q, attn_w_o, rel_bias):
    bn, s, _ = x.shape
    k_blk = v_blk = k_mean = None
    for l in range(DEPTH):
        x = macaron_ffn(x, ffn1_pre_g[l], ffn1_post_g[l], ffn1_w_gate[l], ffn1_w_up[l], ffn1_w_down[l])
        xn = rms_norm(x, mix_pre_g[l])
        if l < N_A_LAYERS:
            m = recurrent_block(xn, lru_w_in[l], lru_b_in[l], lru_conv_w[l], lru_conv_b[l],
                                lru_w_r[l], lru_b_r[l], lru_w_i[l], lru_b_i[l], lru_lambda[l],
                                lru_w_out[l], lru_b_out[l])
        else:
            j = l - N_A_LAYERS
            q = (xn @ attn_w_q[j]).reshape(bn, s, N_HEADS, HEAD_DIM)
            m = moba_attention(q, k_blk, v_blk, k_mean, rel_bias) @ attn_w_o[j]
        x = x + rms_norm(m, mix_post_g[l])
        x = macaron_ffn(x, ffn2_pre_g[l], ffn2_post_g[l], ffn2_w_gate[l], ffn2_w_up[l], ffn2_w_down[l])
        if l == N_A_LAYERS - 1:
            k_blk, v_blk, k_mean = shared_kv(x, kv_norm_g, w_kv)
    return x
```

```python
from contextlib import ExitStack
import math
import numpy as np
import concourse.bass as bass
import concourse.mybir as mybir
from concourse.bass_utils import run_bass_kernel_spmd

F32 = mybir.dt.float32
BF16 = mybir.dt.bfloat16
AF = mybir.ActivationFunctionType
ALU = mybir.AluOpType
AX = mybir.AxisListType

D = 1024
S_LEN = 2048
NB = 8
DFF = 2816
KC = D // 128
FC = DFF // 128
EPS = 1e-6
NHEAD = 8
HD = 128
BLK = 256
NBLK = S_LEN // BLK
NEG = -30000.0
FIN_POOL_FROM = 5
FIN_ORDER = [5, 6, 7, 0, 1, 2, 3, 4]


_UNIQ = [0]


def SB(nc, name, shape, dt, **kw):
    _UNIQ[0] += 1
    return nc.sbuf_tensor(f"{name}_{_UNIQ[0]}", shape, dt, **kw)


class Buf:
    __slots__ = ("name", "w", "r", "excl")

    def __init__(self, name, excl=False):
        self.name = name
        self.w = None
        self.r = []
        self.excl = excl


class Sched:
    ENGS = ("pe", "act", "dve", "pool", "sp")

    def __init__(self, nc, stack):
        self.nc = nc
        self.stack = stack
        self.eng = dict(pe=nc.tensor, act=nc.scalar, dve=nc.vector, pool=nc.gpsimd, sp=nc.sync)
        self.sem = {}
        self.cnt = {}
        self.seen = {e: {} for e in self.ENGS}
        for e in self.ENGS:
            self.new_sem("c_" + e)

    def new_sem(self, name):
        self.sem[name] = self.stack.enter_context(self.nc.semaphore(name))
        self.cnt[name] = 0
        return name

    def _wait(self, e, tok):
        if tok is None:
            return
        s, v = tok
        if e == "pe" and s == "c_pe":
            return
        if self.seen[e].get(s, 0) >= v:
            return
        self.eng[e].wait_ge(self.sem[s], v)
        self.seen[e][s] = v

    @staticmethod
    def _split(R, W):
        if any(b.excl for b in R):
            W = list(W) + [b for b in R if b.excl]
            R = [b for b in R if not b.excl]
        return R, W

    def _deps(self, e, R, W):
        for b in R:
            self._wait(e, b.w)
        for b in W:
            self._wait(e, b.w)
            for t in b.r:
                self._wait(e, t)

    def _commit(self, tok, R, W):
        for b in W:
            b.w = tok
            b.r = []
        for b in R:
            b.r.append(tok)
            if len(b.r) > 12:
                best = {}
                for (s, v) in b.r:
                    if best.get(s, 0) < v:
                        best[s] = v
                b.r = list(best.items())

    def op(self, e, fn, R=(), W=()):
        R, W = self._split(R, W)
        self._deps(e, R, W)
        ins = fn(self.eng[e])
        s = "c_" + e
        self.cnt[s] += 1
        ins.then_inc(self.sem[s], 1)
        tok = (s, self.cnt[s])
        self._commit(tok, R, W)
        return tok

    def group(self, e, fns, R=(), W=()):
        R, W = self._split(R, W)
        self._deps(e, R, W)
        ins = None
        for fn in fns:
            ins = fn(self.eng[e])
        s = "c_" + e
        self.cnt[s] += 1
        ins.then_inc(self.sem[s], 1)
        tok = (s, self.cnt[s])
        self._commit(tok, R, W)
        return tok

    def dma(self, e, sem, out, in_, R=(), W=(), n=1):
        self._deps(e, R, W)
        self.eng[e].dma_start(out=out, in_=in_).then_inc(self.sem[sem], 16)
        self.cnt[sem] += 16
        tok = (sem, self.cnt[sem])
        self._commit(tok, R, W)
        return tok

    def barrier(self):
        for e in self.ENGS:
            for p in self.ENGS:
                if self.cnt["c_" + p] > 0:
                    self._wait(e, ("c_" + p, self.cnt["c_" + p]))


PV_LAYOUT = {}


def _pv_plan():
    if PV_LAYOUT:
        return
    col = 0

    def add(name, n):
        nonlocal col
        PV_LAYOUT[name] = (col, n // 128)
        col += n // 128

    for l in range(2):
        for nm in ("ffn1_pre_g", "ffn1_post_g", "ffn2_pre_g", "ffn2_post_g", "mix_pre_g", "mix_post_g"):
            add(f"{nm}{l}", D)
    add("lru_b_in", 2 * D)
    for k in range(4):
        add(f"lru_conv_w{k}", D)
    for nm in ("lru_conv_b", "lru_b_r", "lru_b_i", "lru_lambda", "lru_b_out", "kv_norm_g"):
        add(nm, D)
    PV_LAYOUT["_ncol"] = (col, 0)


def _fm(v):
    v = np.asarray(v, dtype=np.float32).reshape(-1, 128)
    return np.ascontiguousarray(v.T)


def pack_pv(inp):
    _pv_plan()
    ncol = PV_LAYOUT["_ncol"][0]
    pv = np.zeros((128, ncol), np.float32)

    def put(name, v):
        c0, n = PV_LAYOUT[name]
        pv[:, c0:c0 + n] = _fm(v)

    for l in range(2):
        for nm in ("ffn1_pre_g", "ffn1_post_g", "ffn2_pre_g", "ffn2_post_g", "mix_pre_g", "mix_post_g"):
            put(f"{nm}{l}", inp[nm][l])
    put("lru_b_in", inp["lru_b_in"][0])
    for k in range(4):
        put(f"lru_conv_w{k}", inp["lru_conv_w"][0, k])
    for nm in ("lru_conv_b", "lru_b_r", "lru_b_i", "lru_lambda", "lru_b_out"):
        put(nm, inp[nm][0])
    put("kv_norm_g", inp["kv_norm_g"])
    return pv


def tile_w_out_chunks(w):
    K, N = w.shape
    a = np.asarray(w, np.float32).reshape(K // 128, 128, N // 128, 128)
    return np.ascontiguousarray(a.transpose(2, 1, 0, 3))


def tile_w_rows(w):
    K, N = w.shape
    return np.ascontiguousarray(np.asarray(w, np.float32).reshape(K // 128, 128, N))


def pack_weights(inp):
    out = {}
    for l in range(2):
        for which in ("ffn1", "ffn2"):
            g = tile_w_out_chunks(inp[f"{which}_w_gate"][l])
            u = tile_w_out_chunks(inp[f"{which}_w_up"][l])
            out[f"{which}_wgu{l}"] = np.ascontiguousarray(np.stack([g, u], axis=2))
            out[f"{which}_wd{l}"] = tile_w_out_chunks(inp[f"{which}_w_down"][l])
    out["lru_win"] = tile_w_out_chunks(inp["lru_w_in"][0])
    out["lru_wout"] = tile_w_out_chunks(inp["lru_w_out"][0])
    wg = np.stack([np.asarray(inp["lru_w_r"][0], np.float32), np.asarray(inp["lru_w_i"][0], np.float32)], axis=1)
    wg = wg.reshape(4, 2, 2, 128, 2, 128)
    out["lru_wg"] = np.ascontiguousarray(wg.transpose(0, 3, 1, 2, 4, 5).reshape(4, 128, 8, 128))
    out["wq"] = tile_w_out_chunks(inp["attn_w_q"][0])
    out["wo"] = tile_w_out_chunks(inp["attn_w_o"][0])
    out["wk"] = tile_w_out_chunks(np.asarray(inp["w_kv"])[:, :D])
    out["wv"] = tile_w_rows(np.asarray(inp["w_kv"])[:, D:])
    out["rbT"] = np.ascontiguousarray(np.asarray(inp["rel_bias"], np.float32).T)
    return out


def t5_bucket_np(d):
    n = np.maximum(d, 0)
    nf = np.maximum(n, 1).astype(np.float32)
    large = 16 + (np.log(nf / np.float32(16.0)) / np.float32(math.log(128 / 16)) * np.float32(16.0)).astype(np.int32)
    large = np.minimum(large, 31)
    return np.where(n < 16, n, large)


FREP_W = 640
FAR_D0 = 129


def make_consts():
    c = {}
    c["ones"] = np.ones((128, 128), np.float32)
    c["ident"] = np.eye(128, dtype=np.float32)
    far = t5_bucket_np(np.arange(FAR_D0, S_LEN))
    assert (far == far[0]).all()
    oh = np.zeros((32, FREP_W + 1), np.float32)
    dd = np.arange(FREP_W)
    d = dd - 255
    bk = t5_bucket_np(d)
    for i in range(FREP_W):
        if d[i] >= 0:
            oh[bk[i], i] = 1.0
    oh[far[0], FREP_W] = 1.0
    c["oh"] = oh
    e8 = np.zeros((8, 8, 128), np.float32)
    for n in range(8):
        e8[n, n, :] = 1.0
    c["e8"] = e8
    return c


class Ctx:
    pass


class WStream:
    def __init__(self, K, st, name, shape, nslot, sem0=0):
        self.K = K
        self.name = name
        self.n = nslot
        self.t = [st.enter_context(SB(K.nc, f"{name}{i}", shape, BF16)) for i in range(nslot)]
        self.b = [Buf(f"{name}{i}") for i in range(nslot)]
        self.s = [K.wsem[sem0 + i] for i in range(nslot)]
        self.plan = []
        self.issued = 0
        self.base = 0

    def extend(self, srcs):
        self.plan.extend(srcs)
        self._pump()

    def _pump(self):
        while self.issued < len(self.plan) and self.issued < self.base + self.n:
            i = self.issued
            sl = i % self.n
            src = self.plan[i]
            dst = self.t[sl]
            self.K.S.dma("pool", self.s[sl], dst[:], src, W=[self.b[sl]])
            self.issued += 1

    def get(self):
        sl = self.base % self.n
        return self.t[sl], self.b[sl]

    def done(self):
        self.base += 1
        self._pump()


def mm(out, lhsT, rhs, start, stop):
    return lambda e: e.matmul(out, lhsT, rhs, start=start, stop=stop)


def rms_rstd(K, ps_stat, ps_b, rstd_t, rstd_b):
    S = K.S
    S.op("act", lambda e: e.activation(out=K.srt[:], in_=ps_stat[:], func=AF.Sqrt, scale=1.0 / D, bias=K.eps_ap),
         R=[ps_b], W=[K.srt_b])
    S.op("dve", lambda e: e.reciprocal(out=rstd_t, in_=K.srt[:]), R=[K.srt_b], W=[rstd_b])


def prenorm(K, t5, gcol, xn_t, xn_b, rstd_t, rstd_b, ps, ps_b):
    S = K.S
    t0 = t5 * 512
    xbs = [K.x_b[t5][c] for c in range(KC)]
    S.op("act", lambda e: e.activation(out=K.sq[:], in_=K.x[:, :, t0:t0 + 512], func=AF.Square),
         R=xbs, W=[K.sq_b])
    S.group("pe", [mm(ps[:], K.ones[:], K.sq[:, c, :], c == 0, c == KC - 1) for c in range(KC)],
            R=[K.sq_b], W=[ps_b])
    rms_rstd(K, ps, ps_b, rstd_t, rstd_b)
    for c in range(KC):
        S.op("dve", lambda e, c=c: e.scalar_tensor_tensor(
            out=xn_t[:, c, :], in0=K.x[:, c, t0:t0 + 512], scalar=K.pv[:, gcol + c:gcol + c + 1],
            in1=rstd_t, op0=ALU.mult, op1=ALU.mult), R=[xbs[c], rstd_b],
            W=[xn_b[c] if isinstance(xn_b, list) else xn_b])


def ffn_stage(K, l, which, TT):
    S, nc = K.S, K.nc
    nsub = TT // 512
    ntt = S_LEN // TT
    gpre = PV_LAYOUT[f"{which}_pre_g{l}"][0]
    gpost = PV_LAYOUT[f"{which}_post_g{l}"][0]
    wgu_src = K.dram[f"{which}_wgu{l}"]
    wd_src = K.dram[f"{which}_wd{l}"]
    P, Pb = K.ps, K.ps_b
    with ExitStack() as st:
        xn = st.enter_context(SB(nc, "f_xn", [128, nsub, KC, 512], BF16))
        h = st.enter_context(SB(nc, "f_h", [128, FC, nsub, 512], BF16))
        y = st.enter_context(SB(nc, "f_y", [128, nsub, KC, 512], F32))
        sg = [st.enter_context(SB(nc, f"f_sg{i}", [128, 512], F32)) for i in range(2)]
        ysq = [st.enter_context(SB(nc, f"f_ysq{i}", [128, 512], BF16)) for i in range(2)]
        rstd = st.enter_context(SB(nc, "f_rstd", [128, nsub, 512], F32))
        rstd2 = st.enter_context(SB(nc, "f_rstd2", [128, nsub, 512], F32))
        rstd2_b = [Buf("rstd2") for _ in range(nsub)]
        xn_b = [Buf("xn") for _ in range(nsub)]
        h_b = [[Buf("h") for _ in range(nsub)] for _ in range(FC)]
        y_b = [[Buf("y") for _ in range(KC)] for _ in range(nsub)]
        sg_b = [Buf("sg") for _ in range(2)]
        ysq_b = [Buf("ysq") for _ in range(2)]
        rstd_b = [Buf("rstd") for _ in range(nsub)]
        K.wgu = WStream(K, st, "wgu", [128, 2, KC, 128], 3, 0)
        K.wd = WStream(K, st, "wd", [128, FC, 128], 2, 3)

        K.wgu.extend([wgu_src[f] for _ in range(ntt) for f in range(FC)])
        K.wd.extend([wd_src[c] for _ in range(ntt) for c in range(KC)])

        def front(tt):
            for sub in range(nsub):
                t5 = tt * nsub + sub
                prenorm(K, t5, gpre, xn[:, sub], xn_b[sub], rstd2[:, sub, :], rstd2_b[sub], P[sub], Pb[sub])

        front(0)
        for tt in range(ntt):
            it = 0
            for f in range(FC):
                wt, wb = K.wgu.get()
                for sub in range(nsub):
                    s2 = it % 2
                    it += 1
                    gp, up = P[2 * s2], P[2 * s2 + 1]
                    S.group("pe", [mm(gp[:], wt[:, 0, k, :], xn[:, sub, k, :], k == 0, k == KC - 1) for k in range(KC)],
                            R=[wb, xn_b[sub]], W=[Pb[2 * s2]])
                    S.group("pe", [mm(up[:], wt[:, 1, k, :], xn[:, sub, k, :], k == 0, k == KC - 1) for k in range(KC)],
                            R=[wb, xn_b[sub]], W=[Pb[2 * s2 + 1]])
                    S.op("act", lambda e, gp=gp, s2=s2: e.activation(out=sg[s2][:], in_=gp[:], func=AF.Silu),
                         R=[Pb[2 * s2]], W=[sg_b[s2]])
                    S.op("dve", lambda e, up=up, s2=s2, f=f, sub=sub: e.tensor_tensor(
                        out=h[:, f, sub, :], in0=up[:], in1=sg[s2][:], op=ALU.mult),
                        R=[Pb[2 * s2 + 1], sg_b[s2]], W=[h_b[f][sub]])
                K.wgu.done()
            pend = None
            it = 0
            for c in range(KC):
                wt, wb = K.wd.get()
                for sub in range(nsub):
                    s2 = it % 2
                    it += 1
                    yp, ypb = P[4 + s2], Pb[4 + s2]
                    S.group("pe", [mm(yp[:], wt[:, f, :], h[:, f, sub, :], f == 0, f == FC - 1) for f in range(FC)],
                            R=[wb] + [h_b[f][sub] for f in range(FC)], W=[ypb])
                    S.op("dve", lambda e, yp=yp, sub=sub, c=c: e.tensor_copy(out=y[:, sub, c, :], in_=yp[:]),
                         R=[ypb], W=[y_b[sub][c]])
                    S.op("act", lambda e, sub=sub, c=c, s2=s2: e.activation(out=ysq[s2][:], in_=y[:, sub, c, :], func=AF.Square),
                         R=[y_b[sub][c]], W=[ysq_b[s2]])
                    if pend is not None:
                        pend()
                    pend = (lambda s2=s2, sub=sub, c=c: S.group(
                        "pe", [mm(P[6 + sub][:], K.ones[:], ysq[s2][:], c == 0, c == KC - 1)],
                        R=[ysq_b[s2]], W=[Pb[6 + sub]]))
                K.wd.done()
                if c == KC // 2 and tt + 1 < ntt:
                    front(tt + 1)
            pend()
            for sub in range(nsub):
                t5 = tt * nsub + sub
                t0 = t5 * 512
                rms_rstd(K, P[6 + sub], Pb[6 + sub], rstd[:, sub, :], rstd_b[sub])
                for c in FIN_ORDER:
                    S.op("dve", lambda e, c=c, sub=sub: e.scalar_tensor_tensor(
                        out=y[:, sub, c, :], in0=y[:, sub, c, :], scalar=K.hp[:, gpost + c:gpost + c + 1],
                        in1=rstd[:, sub, :], op0=ALU.mult, op1=ALU.mult),
                        R=[rstd_b[sub]], W=[y_b[sub][c]])
                    if c >= FIN_POOL_FROM:
                        S.op("pool", lambda e, c=c, sub=sub, t0=t0: e.tensor_tensor(
                            out=K.x[:, c, t0:t0 + 512], in0=K.x[:, c, t0:t0 + 512], in1=y[:, sub, c, :], op=ALU.add),
                            R=[y_b[sub][c]], W=[K.x_b[t5][c]])
                    else:
                        S.op("dve", lambda e, c=c, sub=sub, t0=t0: e.scalar_tensor_tensor(
                            out=K.x[:, c, t0:t0 + 512], in0=y[:, sub, c, :], scalar=1.0, in1=K.x[:, c, t0:t0 + 512],
                            op0=ALU.mult, op1=ALU.add), R=[y_b[sub][c]], W=[K.x_b[t5][c]])
        S.barrier()


class PostNorm:
    def __init__(self, K, st, tag, nbuf=1):
        nc = K.nc
        self.K = K
        self.ys = [st.enter_context(SB(nc, f"{tag}_y{i}", [128, KC, 512], F32)) for i in range(nbuf)]
        self.y_bs = [[Buf("y") for _ in range(KC)] for _ in range(nbuf)]
        self.y = self.ys[0]
        self.ysq = [st.enter_context(SB(nc, f"{tag}_ysq{i}", [128, 512], BF16)) for i in range(2)]
        self.rstd = st.enter_context(SB(nc, tag + "_rstd", [128, 512], F32))
        self.y_b = self.y_bs[0]
        self.ysq_b = [Buf("ysq") for _ in range(2)]
        self.rstd_b = Buf("rstd")
        self.pend = None
        self.it = 0

    def use(self, slot):
        self.y = self.ys[slot]
        self.y_b = self.y_bs[slot]

    def evac(self, c, yp, ypb, bias_ap=None, stat_bank=6):
        K, S = self.K, self.K.S
        s2 = self.it % 2
        self.it += 1
        y, y_b = self.y, self.y_b
        if bias_ap is None:
            S.op("dve", lambda e: e.tensor_copy(out=y[:, c, :], in_=yp[:]), R=[ypb], W=[y_b[c]])
        else:
            S.op("dve", lambda e: e.tensor_scalar(out=y[:, c, :], in0=yp[:], scalar1=bias_ap, scalar2=None,
                                                  op0=ALU.add), R=[ypb], W=[y_b[c]])
        S.op("act", lambda e: e.activation(out=self.ysq[s2][:], in_=y[:, c, :], func=AF.Square),
             R=[y_b[c]], W=[self.ysq_b[s2]])
        if self.pend is not None:
            self.pend()
        P, Pb = K.ps[stat_bank], K.ps_b[stat_bank]
        self.pend = lambda: S.group("pe", [mm(P[:], K.ones[:], self.ysq[s2][:], c == 0, c == KC - 1)],
                                    R=[self.ysq_b[s2]], W=[Pb])

    def flush(self):
        if self.pend is not None:
            self.pend()
            self.pend = None

    def finish(self, t5, gt, gcol, stat_bank=6, slot=None):
        K, S = self.K, self.K.S
        self.flush()
        y, y_b = (self.y, self.y_b) if slot is None else (self.ys[slot], self.y_bs[slot])
        t0 = t5 * 512
        rms_rstd(K, K.ps[stat_bank], K.ps_b[stat_bank], self.rstd[:], self.rstd_b)
        for c in FIN_ORDER:
            S.op("dve", lambda e, c=c: e.scalar_tensor_tensor(
                out=y[:, c, :], in0=y[:, c, :], scalar=gt[:, gcol + c:gcol + c + 1],
                in1=self.rstd[:], op0=ALU.mult, op1=ALU.mult),
                R=[self.rstd_b], W=[y_b[c]])
            if c >= FIN_POOL_FROM:
                S.op("pool", lambda e, c=c: e.tensor_tensor(
                    out=K.x[:, c, t0:t0 + 512], in0=K.x[:, c, t0:t0 + 512], in1=y[:, c, :], op=ALU.add),
                    R=[y_b[c]], W=[K.x_b[t5][c]])
            else:
                S.op("dve", lambda e, c=c: e.scalar_tensor_tensor(
                    out=K.x[:, c, t0:t0 + 512], in0=y[:, c, :], scalar=1.0, in1=K.x[:, c, t0:t0 + 512],
                    op0=ALU.mult, op1=ALU.add), R=[y_b[c]], W=[K.x_b[t5][c]])


def lru_stage(K):
    S, nc = K.S, K.nc
    P, Pb = K.ps, K.ps_b
    NT = S_LEN // 512
    c_bin = PV_LAYOUT["lru_b_in"][0]
    c_cw = [PV_LAYOUT[f"lru_conv_w{k}"][0] for k in range(4)]
    c_cb = PV_LAYOUT["lru_conv_b"][0]
    c_br = PV_LAYOUT["lru_b_r"][0]
    c_bi = PV_LAYOUT["lru_b_i"][0]
    c_lam = PV_LAYOUT["lru_lambda"][0]
    c_bo = PV_LAYOUT["lru_b_out"][0]
    gpre = PV_LAYOUT["mix_pre_g0"][0]
    gpost = PV_LAYOUT["mix_post_g0"][0]
    win, wout, wg = K.dram["lru_win"], K.dram["lru_wout"], K.dram["lru_wg"]
    pv, hp = K.pv, K.hp
    with ExitStack() as st0, ExitStack() as st:
        T = lambda name, shape, dt=F32: st.enter_context(SB(nc, "l_" + name, shape, dt))
        xn = st0.enter_context(SB(nc, "l_xn", [128, KC, S_LEN], BF16))
        gT = st0.enter_context(SB(nc, "l_gT", [128, KC, S_LEN], BF16))
        K.wp = WStream(K, st0, "wp", [128, KC, 128], 6, 0)
        xbr = [T(f"xbr{i}", [128, 2, 515]) for i in range(2)]
        xc = [T(f"xc{i}", [128, 2, 512]) for i in range(2)]
        xcb = [T(f"xcb{i}", [128, 2, 512], BF16) for i in range(2)]
        gy = [T(f"gy{i}", [128, 2, 512]) for i in range(2)]
        r_t = T("r", [128, 2, 512])
        i_t = T("i", [128, 2, 512])
        a_t = T("a", [128, 2, 512])
        m_t = T("m", [128, 2, 512])
        u_t = T("u", [128, 2, 512])
        h_t = T("h", [128, 2, 512])
        hst = T("hst", [128, KC])
        cl = T("cl", [128, 2 * KC])
        rstd = T("rstd", [128, 512])
        xn_b = [Buf("xn") for _ in range(NT)]
        gT_b = [[Buf("gT") for _ in range(KC)] for _ in range(NT)]
        xbr_b = [[Buf("xbr") for _ in range(2)] for _ in range(2)]
        xc_b = [[Buf("xc") for _ in range(2)] for _ in range(2)]
        xcb_b = [[Buf("xcb") for _ in range(2)] for _ in range(2)]
        gy_b = [[Buf("gy") for _ in range(2)] for _ in range(2)]
        r_b = [Buf("r") for _ in range(2)]
        i_b = [Buf("i") for _ in range(2)]
        a_b = [Buf("a") for _ in range(2)]
        m_b = [Buf("m") for _ in range(2)]
        u_b = [Buf("u") for _ in range(2)]
        h_b = [Buf("h") for _ in range(2)]
        hst_b = [Buf("hst") for _ in range(KC)]
        cl_b, rstd_b = Buf("cl"), Buf("rstd")

        iters = [(hb, tt) for hb in range(4) for tt in range(NT)]
        a_tiles = lambda n: [win[iters[n][0] * 2], win[iters[n][0] * 2 + 1], win[8 + iters[n][0] * 2], win[8 + iters[n][0] * 2 + 1]]
        plan = a_tiles(0)
        for n in range(len(iters)):
            plan.append(wg[iters[n][0]])
            if n + 1 < len(iters):
                plan += a_tiles(n + 1)
        for t5 in range(NT):
            plan += [wout[c] for c in range(KC)]
        K.wp.extend(plan)

        S.op("act", lambda e: e.activation(out=cl[:, 0:KC], in_=pv[:, c_lam:c_lam + KC], func=AF.Exp, scale=-1.0), W=[cl_b])
        S.op("act", lambda e: e.activation(out=cl[:, 0:KC], in_=cl[:, 0:KC], func=AF.Ln, bias=K.one_ap), R=[cl_b], W=[cl_b])
        S.op("dve", lambda e: e.tensor_scalar(out=cl[:, KC:2 * KC], in0=cl[:, 0:KC], scalar1=-8.0, scalar2=None, op0=ALU.mult), R=[cl_b], W=[cl_b])
        S.op("dve", lambda e: e.tensor_scalar(out=cl[:, 0:KC], in0=cl[:, 0:KC], scalar1=-4.0, scalar2=None, op0=ALU.mult), R=[cl_b], W=[cl_b])

        def lru_prenorm(t5):
            prenorm(K, t5, gpre, xn[:, :, t5 * 512:(t5 + 1) * 512], xn_b[t5], rstd[:], rstd_b, P[6], Pb[6])

        lru_prenorm(0)

        iters = [(hb, tt) for hb in range(4) for tt in range(NT)]

        def stage_a_pe(n):
            hb, tt = iters[n]
            t0 = tt * 512
            for br in range(2):
                for jc in range(2):
                    wt, wb = K.wp.get()
                    pp, ppb = P[br * 2 + jc], Pb[br * 2 + jc]
                    S.group("pe", [mm(pp[:], wt[:, k, :], xn[:, k, t0:t0 + 512], k == 0, k == KC - 1) for k in range(KC)],
                            R=[wb, xn_b[tt]], W=[ppb])
                    K.wp.done()

        def stage_a_rest(n):
            hb, tt = iters[n]
            t0 = tt * 512
            d = n % 2
            cur, prv = xbr[d], xbr[1 - d]
            cur_b, prv_b = xbr_b[d], xbr_b[1 - d]
            for br in range(2):
                for jc in range(2):
                    ch = hb * 2 + jc
                    pp, ppb = P[br * 2 + jc], Pb[br * 2 + jc]
                    bcol = c_bin + br * KC + ch
                    if br == 0:
                        S.op("act", lambda e, pp=pp, jc=jc, bcol=bcol: e.activation(
                            out=cur[:, jc, 3:515], in_=pp[:], func=AF.Identity, bias=pv[:, bcol:bcol + 1]),
                            R=[ppb], W=[cur_b[jc]])
                    else:
                        S.op("act", lambda e, pp=pp, jc=jc, bcol=bcol: e.activation(
                            out=gy[d][:, jc, :], in_=pp[:], func=AF.Gelu_apprx_tanh, bias=pv[:, bcol:bcol + 1]),
                            R=[ppb], W=[gy_b[d][jc]])
            for jc in range(2):
                ch = hb * 2 + jc
                if tt == 0:
                    S.op("dve", lambda e, jc=jc: e.memset(cur[:, jc, 0:3], 0.0), W=[cur_b[jc]])
                else:
                    S.op("dve", lambda e, jc=jc: e.tensor_copy(out=cur[:, jc, 0:3], in_=prv[:, jc, 512:515]),
                         R=[prv_b[jc]], W=[cur_b[jc]])
                S.op("dve", lambda e, jc=jc, ch=ch: e.tensor_scalar(
                    out=xc[d][:, jc, :], in0=cur[:, jc, 0:512], scalar1=pv[:, c_cw[0] + ch:c_cw[0] + ch + 1],
                    scalar2=pv[:, c_cb + ch:c_cb + ch + 1], op0=ALU.mult, op1=ALU.add),
                    R=[cur_b[jc]], W=[xc_b[d][jc]])
                for k in range(1, 4):
                    S.op("dve", lambda e, jc=jc, ch=ch, k=k: e.scalar_tensor_tensor(
                        out=xc[d][:, jc, :], in0=cur[:, jc, k:k + 512], scalar=pv[:, c_cw[k] + ch:c_cw[k] + ch + 1],
                        in1=xc[d][:, jc, :], op0=ALU.mult, op1=ALU.add),
                        R=[cur_b[jc]], W=[xc_b[d][jc]])
                S.op("act", lambda e, jc=jc: e.activation(out=xcb[d][:, jc, :], in_=xc[d][:, jc, :], func=AF.Copy),
                     R=[xc_b[d][jc]], W=[xcb_b[d][jc]])

        def stage_b_pe(n):
            hb, tt = iters[n]
            d = n % 2
            wt, wb = K.wp.get()
            for jc in range(2):
                for g in range(2):
                    pp, ppb = P[4 + jc * 2 + g], Pb[4 + jc * 2 + g]
                    S.group("pe", [mm(pp[:], wt[:, g * 4 + ic * 2 + jc, :], xcb[d][:, ic, :], ic == 0, ic == 1) for ic in range(2)],
                            R=[wb, xcb_b[d][0], xcb_b[d][1]], W=[ppb])
            K.wp.done()

        def stage_b_rest(n):
            hb, tt = iters[n]
            t0 = tt * 512
            d = n % 2
            for jc in range(2):
                ch = hb * 2 + jc
                S.op("act", lambda e, jc=jc, ch=ch: e.activation(
                    out=r_t[:, jc, :], in_=P[4 + jc * 2][:], func=AF.Tanh, scale=0.5, bias=hp[:, c_br + ch:c_br + ch + 1]),
                    R=[Pb[4 + jc * 2]], W=[r_b[jc]])
                S.op("act", lambda e, jc=jc, ch=ch: e.activation(
                    out=i_t[:, jc, :], in_=P[4 + jc * 2 + 1][:], func=AF.Tanh, scale=0.5, bias=hp[:, c_bi + ch:c_bi + ch + 1]),
                    R=[Pb[4 + jc * 2 + 1]], W=[i_b[jc]])
            for jc in range(2):
                ch = hb * 2 + jc
                S.op("act", lambda e, jc=jc, ch=ch: e.activation(
                    out=a_t[:, jc, :], in_=r_t[:, jc, :], func=AF.Exp, scale=cl[:, ch:ch + 1], bias=cl[:, ch:ch + 1]),
                    R=[r_b[jc], cl_b], W=[a_b[jc]])
                S.op("act", lambda e, jc=jc, ch=ch: e.activation(
                    out=m_t[:, jc, :], in_=r_t[:, jc, :], func=AF.Exp, scale=cl[:, KC + ch:KC + ch + 1], bias=cl[:, KC + ch:KC + ch + 1]),
                    R=[r_b[jc], cl_b], W=[m_b[jc]])
                S.op("dve", lambda e, jc=jc: e.scalar_tensor_tensor(
                    out=u_t[:, jc, :], in0=i_t[:, jc, :], scalar=1.0, in1=xc[d][:, jc, :], op0=ALU.add, op1=ALU.mult),
                    R=[i_b[jc], xc_b[d][jc]], W=[u_b[jc]])
            for jc in range(2):
                S.op("act", lambda e, jc=jc: e.activation(
                    out=m_t[:, jc, :], in_=m_t[:, jc, :], func=AF.Ln, scale=-1.0, bias=K.one_ap),
                    R=[m_b[jc]], W=[m_b[jc]])
            for jc in range(2):
                S.op("act", lambda e, jc=jc: e.activation(
                    out=m_t[:, jc, :], in_=m_t[:, jc, :], func=AF.Exp, scale=0.5),
                    R=[m_b[jc]], W=[m_b[jc]])

        def stage_b_rest2(n):
            hb, tt = iters[n]
            t0 = tt * 512
            d = n % 2
            for jc in range(2):
                ch = hb * 2 + jc
                S.op("dve", lambda e, jc=jc: e.scalar_tensor_tensor(
                    out=u_t[:, jc, :], in0=u_t[:, jc, :], scalar=0.5, in1=m_t[:, jc, :], op0=ALU.mult, op1=ALU.mult),
                    R=[m_b[jc]], W=[u_b[jc]])
                init = 0.0 if tt == 0 else hst[:, ch:ch + 1]
                S.op("dve", lambda e, jc=jc, init=init: e.tensor_tensor_scan(
                    out=h_t[:, jc, :], data0=a_t[:, jc, :], data1=u_t[:, jc, :], initial=init, op0=ALU.mult, op1=ALU.add),
                    R=[a_b[jc], u_b[jc], hst_b[ch]], W=[h_b[jc]])
                S.op("dve", lambda e, jc=jc, ch=ch: e.tensor_copy(out=hst[:, ch:ch + 1], in_=h_t[:, jc, 511:512]),
                     R=[h_b[jc]], W=[hst_b[ch]])
                S.op("pool", lambda e, jc=jc, ch=ch: e.tensor_tensor(
                    out=gT[:, ch, t0:t0 + 512], in0=h_t[:, jc, :], in1=gy[d][:, jc, :], op=ALU.mult),
                    R=[h_b[jc], gy_b[d][jc]], W=[gT_b[tt][ch]])

        lru_prenorm(1)
        stage_a_pe(0)
        stage_a_rest(0)
        for n in range(len(iters)):
            stage_b_pe(n)
            if n + 1 < len(iters):
                stage_a_pe(n + 1)
            stage_b_rest(n)
            if n + 2 < NT:
                lru_prenorm(n + 2)
            if n + 1 < len(iters):
                stage_a_rest(n + 1)
            stage_b_rest2(n)
        S.barrier()
        st.close()
        pn = PostNorm(K, st, "l_pn", nbuf=2)
        it = 0

        def body(t5):
            nonlocal it
            t0 = t5 * 512
            pn.use(t5 % 2)
            for c in range(KC):
                wt, wb = K.wp.get()
                pp, ppb = P[it % 2], Pb[it % 2]
                it += 1
                S.group("pe", [mm(pp[:], wt[:, k, :], gT[:, k, t0:t0 + 512], k == 0, k == KC - 1) for k in range(KC)],
                        R=[wb] + gT_b[t5], W=[ppb])
                K.wp.done()
                pn.evac(c, pp, ppb, bias_ap=pv[:, c_bo + c:c_bo + c + 1], stat_bank=6 + t5 % 2)
            pn.flush()

        body(0)
        for t5 in range(NT):
            if t5 + 1 < NT:
                body(t5 + 1)
            pn.finish(t5, K.pv, gpost, stat_bank=6 + t5 % 2, slot=t5 % 2)
        S.barrier()


def kvnorm_stage(K):
    S, nc = K.S, K.nc
    P, Pb = K.ps, K.ps_b
    scale = float(HD) ** -0.5
    K.cst_stack = ExitStack()
    if True:
        TR = lambda name, shape, dt=F32: K.cst_stack.enter_context(SB(nc, "c_" + name, shape, dt, side="right"))
        M = K.M = TR("M", [128, NHEAD, 512], BF16)
        cb = K.cb = TR("cb", [128, NHEAD])
        identb = K.identb = TR("identb", [128, 128], BF16)
        ident32 = K.ident32 = TR("ident32", [128, 128])
        E8 = K.E8 = K.cst_stack.enter_context(SB(nc, "c_E8", [8, NBLK, 128], BF16, side="right"))
        zt = K.zt = TR("zero", [128, 1])
        M_b, cb_b, cst_b = Buf("M"), Buf("cb"), Buf("cst")
        st1 = ExitStack()
        if True:
            rbT = st1.enter_context(SB(nc, "a_rbT", [32, NHEAD], F32))
            oh = st1.enter_context(SB(nc, "a_oh", [32, FREP_W + 1], F32))
            ones32 = st1.enter_context(SB(nc, "a_ones32", [32, 128], F32))
            rbb = st1.enter_context(SB(nc, "a_rbb", [32, NHEAD, 128], F32))
            frep = st1.enter_context(SB(nc, "a_frep", [128, NHEAD, FREP_W], BF16))
            rb_b, rbb_b, frep_b, fd_b = Buf("rb"), Buf("rbb"), Buf("frep"), Buf("fd")
            S.dma("sp", "ld_c", rbT[:], K.dram["rbT"][:, :], W=[rb_b])
            S.dma("sp", "ld_c", oh[:], K.dram["oh"][:, :], W=[rb_b])
            tk2 = S.dma("sp", "ld_c", ident32[:], K.dram["ident"][:, :], W=[cst_b])
            rb_b.w = tk2
            S.dma("pool", "ld_c2", identb[:], K.dram["ident"][:, :], W=[cst_b])
            tk3 = S.dma("pool", "ld_c2", E8[:], K.dram["e8"][:, :, :], W=[cst_b])
            S.op("dve", lambda e: e.memset(zt[:], 0.0), W=[cst_b])
            S.op("dve", lambda e: e.memset(ones32[:], 1.0), W=[rbb_b])
            for h in range(NHEAD):
                S.op("dve", lambda e, h=h: e.tensor_scalar(out=rbb[:, h, :], in0=ones32[:], scalar1=rbT[:, h:h + 1],
                                                           scalar2=None, op0=ALU.mult), R=[rb_b], W=[rbb_b])
            h2 = FREP_W // 2
            for h in range(NHEAD):
                pa, pab = P[(2 * h) % 4], Pb[(2 * h) % 4]
                pb_, pbb = P[(2 * h + 1) % 4], Pb[(2 * h + 1) % 4]
                S.group("pe", [mm(pa[:, 0:h2], rbb[:, h, :], oh[:, 0:h2], True, True)], R=[rbb_b, rb_b], W=[pab])
                S.group("pe", [mm(pb_[:, 0:h2 + 1], rbb[:, h, :], oh[:, h2:FREP_W + 1], True, True)], R=[rbb_b, rb_b], W=[pbb])
                S.op("act", lambda e, h=h, pa=pa: e.activation(out=frep[:, h, 0:h2], in_=pa[:, 0:h2], func=AF.Copy, scale=1.0 / scale),
                     R=[pab], W=[frep_b])
                S.op("act", lambda e, h=h, pb_=pb_: e.activation(out=frep[:, h, h2:FREP_W], in_=pb_[:, 0:h2], func=AF.Copy, scale=1.0 / scale),
                     R=[pbb], W=[frep_b])
                S.op("act", lambda e, h=h, pb_=pb_: e.activation(out=cb[:, h:h + 1], in_=pb_[:, h2:h2 + 1], func=AF.Copy),
                     R=[pbb], W=[cb_b])
            S.op("dve", lambda e: e.memset(frep[:, :, 0:255], NEG), R=[frep_b], W=[frep_b])
            fd = K.frep_dram
            S.dma("sp", "ld_c", fd.ap(), frep[:].rearrange("p h w -> p (h w)"), R=[frep_b], W=[fd_b])
            skew = bass.AP(tensor=fd, offset=127, ap=[[NHEAD * FREP_W - 1, 128], [FREP_W, NHEAD], [1, 512]])
            with nc.allow_non_contiguous_dma(reason="toeplitz skew"):
                S.dma("sp", "ld_c", M[:], skew, R=[fd_b], W=[M_b])
    K.kv_stack = ExitStack()
    K.xnkv = K.kv_stack.enter_context(SB(nc, "xnkv", [128, KC, S_LEN], BF16, side="right"))
    K.xnkv_b = [Buf("xnkv") for _ in range(S_LEN // 512)]
    g = PV_LAYOUT["kv_norm_g"][0]
    with ExitStack() as st:
        rstd = st.enter_context(SB(nc, "kv_rstd", [128, 512], F32))
        rstd_b = Buf("rstd")
        for t5 in range(S_LEN // 512):
            prenorm(K, t5, g, K.xnkv[:, :, t5 * 512:(t5 + 1) * 512], K.xnkv_b[t5], rstd[:], rstd_b, K.ps[6], K.ps_b[6])
        S.barrier()
        for e in ("pe", "act", "dve", "pool"):
            S._wait(e, M_b.w)
            S._wait(e, tk3)
            S._wait(e, tk2)
    st1.close()


def attn_stage(K):
    S, nc = K.S, K.nc
    P, Pb = K.ps, K.ps_b
    NT = S_LEN // 512
    pv = K.pv
    gpre = PV_LAYOUT["mix_pre_g1"][0]
    gpost = PV_LAYOUT["mix_post_g1"][0]
    scale = float(HD) ** -0.5
    with ExitStack() as st0:
        T0 = lambda name, shape, dt=F32: st0.enter_context(SB(nc, "a_" + name, shape, dt))
        KT = T0("KT", [128, NHEAD, S_LEN], BF16)
        V = T0("V", [128, S_LEN // 128, D], BF16)
        kmT = T0("kmT", [128, NHEAD, NBLK], BF16)
        K.wp = WStream(K, st0, "wp", [128, KC, 128], 3, 0)
        KT_b = [[Buf("KT") for _ in range(NT)] for _ in range(NHEAD)]
        V_b = [Buf("V") for _ in range(S_LEN // 128)]
        kmT_b = Buf("kmT")
        with ExitStack() as st1:
            wv = st1.enter_context(SB(nc, "a_wv", [128, KC, D], BF16))
            kms = st1.enter_context(SB(nc, "a_kms", [128, NHEAD, NBLK], F32))
            wv_bs, kms_b = [Buf("wv") for _ in range(KC)], Buf("kms")
            K.wp.extend([K.dram["wk"][h] for h in range(NHEAD)])
            tok = None
            for k in range(KC):
                tok = S.dma("pool", K.wsem[6], wv[:, k, :], K.dram["wv"][k], W=[wv_bs[k]])
            for b in wv_bs:
                b.w = tok
            it = 0
            for h in range(NHEAD):
                wt, wb = K.wp.get()
                for t5 in range(NT):
                    t0 = t5 * 512
                    pp, ppb = P[it % 2], Pb[it % 2]
                    it += 1
                    S.group("pe", [mm(pp[:], wt[:, k, :], K.xnkv[:, k, t0:t0 + 512], k == 0, k == KC - 1) for k in range(KC)],
                            R=[wb, K.xnkv_b[t5]], W=[ppb])
                    S.op("act", lambda e, pp=pp, h=h, t0=t0: e.activation(out=KT[:, h, t0:t0 + 512], in_=pp[:], func=AF.Copy),
                         R=[ppb], W=[KT_b[h][t5]])
                    S.op("dve", lambda e, pp=pp, h=h, t5=t5: e.tensor_reduce(
                        out=kms[:, h, 2 * t5:2 * t5 + 2], in_=pp[:].rearrange("p (b j) -> p b j", j=BLK), axis=AX.X, op=ALU.add),
                        R=[ppb], W=[kms_b])
                K.wp.done()
            S.op("dve", lambda e: e.tensor_scalar(out=kmT[:], in0=kms[:], scalar1=1.0 / BLK, scalar2=None, op0=ALU.mult),
                 R=[kms_b], W=[kmT_b])
            it = 0
            for tk in range(S_LEN // 128):
                for half in range(2):
                    pp, ppb = P[2 + it % 2], Pb[2 + it % 2]
                    S.group("pe", [mm(pp[:], K.xnkv[:, k, tk * 128:(tk + 1) * 128], wv[:, k, half * 512:(half + 1) * 512],
                                      k == 0, k == KC - 1) for k in range(KC)],
                            R=wv_bs + [K.xnkv_b[tk // 4]], W=[ppb])
                    if it % 2 == 0:
                        S.op("act", lambda e, pp=pp, tk=tk, half=half: e.activation(
                            out=V[:, tk, half * 512:(half + 1) * 512], in_=pp[:], func=AF.Copy), R=[ppb], W=[V_b[tk]])
                    else:
                        S.op("dve", lambda e, pp=pp, tk=tk, half=half: e.tensor_copy(
                            out=V[:, tk, half * 512:(half + 1) * 512], in_=pp[:]), R=[ppb], W=[V_b[tk]])
                    it += 1
            S.barrier()
        K.kv_stack.close()
        M, cb, identb, ident32, E8, zt = K.M, K.cb, K.identb, K.ident32, K.E8, K.zt
        T1 = T0
        xn = K.sq
        aT_t = T1("aT", [128, NHEAD, 512], BF16)
        QT = T1("QT", [128, NHEAD, 512], BF16)
        aT = aT_t
        AT = st0.enter_context(SB(nc, "a_AT", [8, NHEAD, 512], BF16))
        PT = [T1(f"PT{i}", [128, 256], BF16) for i in range(6)]
        psum_acc = [T1(f"pacc{i}", [128, 256]) for i in range(2)]
        rden = psum_acc
        pacc_b = [Buf("pacc") for _ in range(2)]
        paccbf_b = [Buf("paccbf") for _ in range(2)]
        gs = T1("gs", [128, NHEAD, NBLK])
        mx = T1("mx", [128, NHEAD, 8])
        Am = T1("Am", [128, NHEAD, NBLK])
        pn = PostNorm(K, st0, "a_pn")
        rstd, rstd_b = pn.rstd, pn.rstd_b
        xn_b = K.sq_b
        QT_b = [Buf("QT") for _ in range(NHEAD)]
        aT_b = [Buf("aT") for _ in range(NHEAD)]
        AT_b = [Buf("AT") for _ in range(4)]
        PT_b = [Buf("PT") for _ in range(6)]
        gs_b, mx_b, Am_b = Buf("gs"), Buf("mx"), Buf("Am")
        ipt = 0
        iod = 0
        def front_norm(t5):
            prenorm(K, t5, gpre, xn[:], xn_b, rstd[:], rstd_b, P[7], Pb[7])

        def front_q(t5):
            K.wp.extend([K.dram["wq"][h] for h in range(NHEAD)])
            for h in range(NHEAD):
                wt, wb = K.wp.get()
                pp, ppb = P[6 + h % 2], Pb[6 + h % 2]
                S.group("pe", [mm(pp[:], wt[:, k, :], xn[:, k, :], k == 0, k == KC - 1) for k in range(KC)],
                        R=[wb, xn_b], W=[ppb])
                K.wp.done()
                if h % 2 == 0:
                    S.op("act", lambda e, pp=pp, h=h: e.activation(out=QT[:, h, :], in_=pp[:], func=AF.Copy), R=[ppb], W=[QT_b[h]])
                else:
                    S.op("dve", lambda e, pp=pp, h=h: e.tensor_copy(out=QT[:, h, :], in_=pp[:]), R=[ppb], W=[QT_b[h]])

        def front_gates(t5):
            if 2 * t5 >= 4:
                for s4 in range(4):
                    qb = 2 * t5 + s4 // 2
                    c0 = s4 * 128
                    S.group("pe", [mm(P[6][:, h * NBLK:(h + 1) * NBLK], QT[:, h, c0:c0 + 128], kmT[:, h, :], True, True)
                                   for h in range(NHEAD)], R=QT_b + [kmT_b], W=[Pb[6]])
                    S.op("dve", lambda e: e.memset(gs[:], -1e30), W=[gs_b])
                    S.op("dve", lambda e, qb=qb: e.tensor_copy(
                        out=gs[:, :, 0:qb], in_=P[6][:, 0:NHEAD * NBLK].rearrange("p (h n) -> p h n", n=NBLK)[:, :, 0:qb]),
                        R=[Pb[6]], W=[gs_b])
                    for h in range(NHEAD):
                        S.op("dve", lambda e, h=h: e.max(out=mx[:, h, :], in_=gs[:, h, :]), R=[gs_b], W=[mx_b])
                    for h in range(NHEAD):
                        S.op("dve", lambda e, h=h: e.tensor_scalar(out=Am[:, h, :], in0=gs[:, h, :], scalar1=mx[:, h, 2:3],
                                                                   scalar2=NEG, op0=ALU.is_lt, op1=ALU.mult),
                             R=[gs_b, mx_b], W=[Am_b])
                    for g in range(2):
                        pp, ppb = P[7], Pb[7]
                        S.group("pe", [(lambda e, j=j, g=g, pp=pp: e.transpose(pp[0:8, j * 128:(j + 1) * 128], Am[:, g * 4 + j, :], ident32[:]))
                                       for j in range(4)], R=[Am_b], W=[ppb])
                        S.op("act", lambda e, g=g, c0=c0, pp=pp: e.activation(
                            out=AT[0:8, g * 4:(g + 1) * 4, c0:c0 + 128], in_=pp[0:8, :].rearrange("p (h t) -> p h t", t=128),
                            func=AF.Copy), R=[ppb], W=[AT_b[s4]])

        def inner(t5, hook=None):
            items = []
            for h in range(NHEAD):
                for qb2 in range(2):
                    qb = 2 * t5 + qb2
                    for kt in range(2 * qb + 2):
                        items.append((h, qb2, kt))
            SK = 3
            SB_ = [0, 1, 6, 7]
            st_items = {}

            def emit_qk(idx):
                h, qb2, kt = items[idx]
                qb = 2 * t5 + qb2
                qo = qb2 * 256
                far = kt <= 2 * qb - 2
                bk = SB_[idx % 4]
                ps, psb = P[bk], Pb[bk]
                ops = [(KT[:, h, kt * 128:(kt + 1) * 128], QT[:, h, qo:qo + 256])]
                R = [KT_b[h][kt // 4], QT_b[h]]
                if kt < 2 * qb and qb >= 4:
                    ops.append((E8[0:8, kt // 2, :], AT[0:8, h, qo:qo + 256]))
                    R += [AT_b[2 * qb2], AT_b[2 * qb2 + 1]]
                if not far:
                    off = {2 * qb - 1: 256, 2 * qb: 128, 2 * qb + 1: 0}[kt]
                    ops.append((identb[:], M[:, h, off:off + 256]))
                S.group("pe", [mm(ps[:, 0:256], a, b, i == 0, i == len(ops) - 1) for i, (a, b) in enumerate(ops)],
                        R=R, W=[psb])
                i4 = idx % len(PT)
                bias_ap = cb[:, h:h + 1] if far else zt[:, 0:1]
                S.op("act", lambda e: e.activation(out=PT[i4][:], in_=ps[:, 0:256], func=AF.Exp, scale=scale, bias=bias_ap),
                     R=[psb], W=[PT_b[i4]])

            def emit_pv(idx):
                h, qb2, kt = items[idx]
                qb = 2 * t5 + qb2
                qo = qb2 * 256
                nkt = 2 * qb + 2
                j = (h * 2 + qb2) % 2
                po, pob, pd, pdb = P[2 + j], Pb[2 + j], P[4 + j], Pb[4 + j]
                i4 = idx % len(PT)
                S.group("pe", [mm(po[:, 0:256], V[:, kt, h * 128:(h + 1) * 128], PT[i4][:], kt == 0, kt == nkt - 1)],
                        R=[V_b[kt], PT_b[i4]], W=[pob])
                S.group("pe", [mm(pd[:, 0:256], K.ones[:], PT[i4][:], kt == 0, kt == nkt - 1)],
                        R=[PT_b[i4]], W=[pdb])
                if kt == nkt - 1:
                    S.op("dve", lambda e: e.reciprocal(out=rden[j][:], in_=pd[:, 0:256]), R=[pdb], W=[pacc_b[j]])
                    S.op("dve", lambda e: e.tensor_tensor(out=aT[:, h, qo:qo + 256], in0=po[:, 0:256], in1=rden[j][:], op=ALU.mult),
                         R=[pob, pacc_b[j]], W=[aT_b[h]])

            hook_at = (len(items) * 3) // 4
            for idx in range(len(items) + SK):
                if idx < len(items):
                    emit_qk(idx)
                if idx >= SK:
                    emit_pv(idx - SK)
                if idx == hook_at and hook is not None:
                    hook()

        def back(t5):
            K.wp.extend([K.dram["wo"][c] for c in range(KC)])
            for c in range(KC):
                wt, wb = K.wp.get()
                pp, ppb = P[6 + c % 2], Pb[6 + c % 2]
                S.group("pe", [mm(pp[:], wt[:, k, :], aT[:, k, :], k == 0, k == KC - 1) for k in range(KC)],
                        R=[wb] + aT_b, W=[ppb])
                K.wp.done()
                pn.evac(c, pp, ppb, stat_bank=5)
            pn.flush()

        front_norm(0)
        front_q(0)
        front_gates(0)
        for t5 in range(NT):
            nxt = t5 + 1 < NT
            inner(t5, hook=(lambda t=t5 + 1: front_norm(t)) if nxt else None)
            back(t5)
            if nxt:
                front_q(t5 + 1)
                front_gates(t5 + 1)
            pn.finish(t5, K.pv, gpost, stat_bank=5)
        S.barrier()
    K.cst_stack.close()

def build_program(stages, winfo):
    _pv_plan()
    nc = bass.Bass("TRN2", target_bir_lowering=False)
    K = Ctx()
    K.nc = nc
    K.dram = {}
    for name, shape in winfo.items():
        K.dram[name] = nc.dram_tensor(name, list(shape), F32, kind="ExternalInput").ap()
    xT = nc.dram_tensor("xT", [D, S_LEN], F32, kind="ExternalInput").ap()
    pvd = nc.dram_tensor("pv", [128, PV_LAYOUT["_ncol"][0]], F32, kind="ExternalInput").ap()
    onesd = nc.dram_tensor("ones", [128, 128], F32, kind="ExternalInput").ap()
    outT = nc.dram_tensor("outT", [D, S_LEN], F32, kind="ExternalOutput").ap()
    K.frep_dram = nc.dram_tensor("frep_scratch", [128, NHEAD * FREP_W], BF16, kind="Internal")
    ncol = PV_LAYOUT["_ncol"][0]
    with ExitStack() as stack:
        K.stack = stack
        S = K.S = Sched(nc, stack)
        K.x = stack.enter_context(SB(nc, "x_res", [128, KC, S_LEN], F32))
        K.x_b = [[Buf(f"x{t}_{c}") for c in range(KC)] for t in range(S_LEN // 512)]
        K.pv = stack.enter_context(SB(nc, "pv_sb", [128, ncol], F32))
        K.hp = stack.enter_context(SB(nc, "hp_sb", [128, ncol], F32))
        K.ones = stack.enter_context(SB(nc, "ones_sb", [128, 128], BF16))
        K.epst = stack.enter_context(SB(nc, "eps_sb", [128, 1], F32))
        K.eps_ap = K.epst[:, 0:1]
        K.sq = stack.enter_context(SB(nc, "sq", [128, KC, 512], BF16))
        K.sq_b = Buf("sq")
        K.srt = stack.enter_context(SB(nc, "srt", [128, 512], F32))
        K.srt_b = Buf("srt")
        K.ps = [stack.enter_context(nc.psum_tensor(f"ps{i}", [128, 512], F32)) for i in range(8)]
        K.ps_b = [Buf(f"ps{i}", excl=True) for i in range(8)]
        K.wsem = [S.new_sem(f"w{i}") for i in range(8)]
        K.onet = stack.enter_context(SB(nc, "one_sb", [128, 1], F32))
        K.one_ap = K.onet[:, 0:1]
        pv_b, ones_b, eps_b, hp_b = Buf("pv"), Buf("ones"), Buf("eps"), Buf("hp")
        S.new_sem("ld_x")
        S.new_sem("ld_c")
        S.new_sem("st_x")
        S.new_sem("ld_c2")
        S.dma("sp", "ld_c", K.pv[:], pvd[:, :], W=[pv_b])
        tokc = S.dma("pool", "ld_c2", K.ones[:], onesd[:, :], W=[ones_b])
        S.op("dve", lambda e: e.memset(K.epst[:], EPS), W=[eps_b])
        S.op("dve", lambda e: e.memset(K.onet[:], 1.0), W=[eps_b])
        S.op("dve", lambda e: e.tensor_scalar(out=K.hp[:], in0=K.pv[:], scalar1=0.5, scalar2=None, op0=ALU.mult),
             R=[pv_b], W=[hp_b])
        tok = None
        for c in range(KC):
            tok = S.dma("sp", "ld_x", K.x[:, c, :], xT[c * 128:(c + 1) * 128, :],
                        W=[K.x_b[t][c] for t in range(S_LEN // 512)])
        for t in range(S_LEN // 512):
            for c in range(KC):
                K.x_b[t][c].w = tok
        S.barrier()
        for e in ("pe", "act", "dve", "pool"):
            S._wait(e, tokc)
            S._wait(e, hp_b.w)
            S._wait(e, eps_b.w)
        for stg in stages:
            if stg[0] == "ffn":
                _, l, which, TT = stg
                ffn_stage(K, l, which, TT)
            elif stg[0] == "lru":
                lru_stage(K)
            elif stg[0] == "kvnorm":
                kvnorm_stage(K)
            elif stg[0] == "attn":
                attn_stage(K)
            else:
                raise ValueError(stg)
        tok = None
        for c in range(KC):
            tok = S.dma("sp", "st_x", outT[c * 128:(c + 1) * 128, :], K.x[:, c, :],
                        R=[K.x_b[t][c] for t in range(S_LEN // 512)])
        S._wait("sp", tok)
    return nc


ALL_STAGES = [("ffn", 0, "ffn1", 1024), ("lru",), ("ffn", 0, "ffn2", 1024), ("kvnorm",),
              ("ffn", 1, "ffn1", 512), ("attn",), ("ffn", 1, "ffn2", 1024)]


def stage_inputs(stages, W, C):
    need = []
    for stg in stages:
        if stg[0] == "ffn":
            need += [f"{stg[2]}_wgu{stg[1]}", f"{stg[2]}_wd{stg[1]}"]
        elif stg[0] == "lru":
            need += ["lru_win", "lru_wout", "lru_wg"]
        elif stg[0] == "attn":
            need += ["wq", "wo", "wk", "wv", "rbT"]
    d = {k: W[k] for k in need}
    if any(s[0] == "attn" for s in stages):
        for k in ("oh", "ident", "e8"):
            d[k] = C[k]
    return d


def run_stages(stages, xT_list, pv, W, C):
    use = stage_inputs(stages, W, C)
    nc = build_program(stages, {k: v.shape for k, v in use.items()})
    in_maps = []
    for xT in xT_list:
        m = dict(use)
        m["xT"] = xT
        m["pv"] = pv
        m["ones"] = C["ones"]
        in_maps.append(m)
    res = run_bass_kernel_spmd(nc, in_maps, core_ids=list(range(len(xT_list))))
    return [r["outT"] for r in res.results]


def kernel(**inputs):
    inp = {k: np.asarray(v) for k, v in inputs.items()}
    x = inp["x"].astype(np.float32, copy=False)
    pv = pack_pv(inp)
    W = pack_weights(inp)
    C = make_consts()
    xT = [np.ascontiguousarray(x[b].T) for b in range(NB)]
    outT = run_stages(ALL_STAGES, xT, pv, W, C)
    return np.stack([o.T for o in outT], axis=0).astype(np.float32)
```

```python
from contextlib import ExitStack
import math
import numpy as np
import concourse.bass as bass
import concourse.mybir as mybir
from concourse.bass_utils import run_bass_kernel_spmd

F32 = mybir.dt.float32
BF16 = mybir.dt.bfloat16
AF = mybir.ActivationFunctionType
ALU = mybir.AluOpType
AX = mybir.AxisListType

D = 1024
S_LEN = 2048
NB = 8
DFF = 2816
KC = D // 128
FC = DFF // 128
EPS = 1e-6
NHEAD = 8
HD = 128
BLK = 256
NBLK = S_LEN // BLK
NEG = -30000.0
FIN_POOL_FROM = 5
FIN_ORDER = [5, 6, 7, 0, 1, 2, 3, 4]


_UNIQ = [0]


def SB(nc, name, shape, dt, **kw):
    _UNIQ[0] += 1
    return nc.sbuf_tensor(f"{name}_{_UNIQ[0]}", shape, dt, **kw)


class Buf:
    __slots__ = ("name", "w", "r", "excl")

    def __init__(self, name, excl=False):
        self.name = name
        self.w = None
        self.r = []
        self.excl = excl


class Sched:
    ENGS = ("pe", "act", "dve", "pool", "sp")

    def __init__(self, nc, stack):
        self.nc = nc
        self.stack = stack
        self.eng = dict(pe=nc.tensor, act=nc.scalar, dve=nc.vector, pool=nc.gpsimd, sp=nc.sync)
        self.sem = {}
        self.cnt = {}
        self.seen = {e: {} for e in self.ENGS}
        for e in self.ENGS:
            self.new_sem("c_" + e)

    def new_sem(self, name):
        self.sem[name] = self.stack.enter_context(self.nc.semaphore(name))
        self.cnt[name] = 0
        return name

    def _wait(self, e, tok):
        if tok is None:
            return
        s, v = tok
        if e == "pe" and s == "c_pe":
            return
        if self.seen[e].get(s, 0) >= v:
            return
        self.eng[e].wait_ge(self.sem[s], v)
        self.seen[e][s] = v

    @staticmethod
    def _split(R, W):
        if any(b.excl for b in R):
            W = list(W) + [b for b in R if b.excl]
            R = [b for b in R if not b.excl]
        return R, W

    def _deps(self, e, R, W):
        for b in R:
            self._wait(e, b.w)
        for b in W:
            self._wait(e, b.w)
            for t in b.r:
                self._wait(e, t)

    def _commit(self, tok, R, W):
        for b in W:
            b.w = tok
            b.r = []
        for b in R:
            b.r.append(tok)
            if len(b.r) > 12:
                best = {}
                for (s, v) in b.r:
                    if best.get(s, 0) < v:
                        best[s] = v
                b.r = list(best.items())

    def op(self, e, fn, R=(), W=()):
        R, W = self._split(R, W)
        self._deps(e, R, W)
        ins = fn(self.eng[e])
        s = "c_" + e
        self.cnt[s] += 1
        ins.then_inc(self.sem[s], 1)
        tok = (s, self.cnt[s])
        self._commit(tok, R, W)
        return tok

    def group(self, e, fns, R=(), W=()):
        R, W = self._split(R, W)
        self._deps(e, R, W)
        ins = None
        for fn in fns:
            ins = fn(self.eng[e])
        s = "c_" + e
        self.cnt[s] += 1
        ins.then_inc(self.sem[s], 1)
        tok = (s, self.cnt[s])
        self._commit(tok, R, W)
        return tok

    def dma(self, e, sem, out, in_, R=(), W=(), n=1):
        self._deps(e, R, W)
        self.eng[e].dma_start(out=out, in_=in_).then_inc(self.sem[sem], 16)
        self.cnt[sem] += 16
        tok = (sem, self.cnt[sem])
        self._commit(tok, R, W)
        return tok

    def barrier(self):
        for e in self.ENGS:
            for p in self.ENGS:
                if self.cnt["c_" + p] > 0:
                    self._wait(e, ("c_" + p, self.cnt["c_" + p]))


PV_LAYOUT = {}


def _pv_plan():
    if PV_LAYOUT:
        return
    col = 0

    def add(name, n):
        nonlocal col
        PV_LAYOUT[name] = (col, n // 128)
        col += n // 128

    for l in range(2):
        for nm in ("ffn1_pre_g", "ffn1_post_g", "ffn2_pre_g", "ffn2_post_g", "mix_pre_g", "mix_post_g"):
            add(f"{nm}{l}", D)
    add("lru_b_in", 2 * D)
    for k in range(4):
        add(f"lru_conv_w{k}", D)
    for nm in ("lru_conv_b", "lru_b_r", "lru_b_i", "lru_lambda", "lru_b_out", "kv_norm_g"):
        add(nm, D)
    PV_LAYOUT["_ncol"] = (col, 0)


def _fm(v):
    v = np.asarray(v, dtype=np.float32).reshape(-1, 128)
    return np.ascontiguousarray(v.T)


def pack_pv(inp):
    _pv_plan()
    ncol = PV_LAYOUT["_ncol"][0]
    pv = np.zeros((128, ncol), np.float32)

    def put(name, v):
        c0, n = PV_LAYOUT[name]
        pv[:, c0:c0 + n] = _fm(v)

    for l in range(2):
        for nm in ("ffn1_pre_g", "ffn1_post_g", "ffn2_pre_g", "ffn2_post_g", "mix_pre_g", "mix_post_g"):
            put(f"{nm}{l}", inp[nm][l])
    put("lru_b_in", inp["lru_b_in"][0])
    for k in range(4):
        put(f"lru_conv_w{k}", inp["lru_conv_w"][0, k])
    for nm in ("lru_conv_b", "lru_b_r", "lru_b_i", "lru_lambda", "lru_b_out"):
        put(nm, inp[nm][0])
    put("kv_norm_g", inp["kv_norm_g"])
    return pv


def tile_w_out_chunks(w):
    K, N = w.shape
    a = np.asarray(w, np.float32).reshape(K // 128, 128, N // 128, 128)
    return np.ascontiguousarray(a.transpose(2, 1, 0, 3))


def tile_w_rows(w):
    K, N = w.shape
    return np.ascontiguousarray(np.asarray(w, np.float32).reshape(K // 128, 128, N))


def pack_weights(inp):
    out = {}
    for l in range(2):
        for which in ("ffn1", "ffn2"):
            g = tile_w_out_chunks(inp[f"{which}_w_gate"][l])
            u = tile_w_out_chunks(inp[f"{which}_w_up"][l])
            out[f"{which}_wgu{l}"] = np.ascontiguousarray(np.stack([g, u], axis=2))
            out[f"{which}_wd{l}"] = tile_w_out_chunks(inp[f"{which}_w_down"][l])
    out["lru_win"] = tile_w_out_chunks(inp["lru_w_in"][0])
    out["lru_wout"] = tile_w_out_chunks(inp["lru_w_out"][0])
    wg = np.stack([np.asarray(inp["lru_w_r"][0], np.float32), np.asarray(inp["lru_w_i"][0], np.float32)], axis=1)
    wg = wg.reshape(4, 2, 2, 128, 2, 128)
    out["lru_wg"] = np.ascontiguousarray(wg.transpose(0, 3, 1, 2, 4, 5).reshape(4, 128, 8, 128))
    out["wq"] = tile_w_out_chunks(inp["attn_w_q"][0])
    out["wo"] = tile_w_out_chunks(inp["attn_w_o"][0])
    out["wk"] = tile_w_out_chunks(np.asarray(inp["w_kv"])[:, :D])
    out["wv"] = tile_w_rows(np.asarray(inp["w_kv"])[:, D:])
    out["rbT"] = np.ascontiguousarray(np.asarray(inp["rel_bias"], np.float32).T)
    return out


def t5_bucket_np(d):
    n = np.maximum(d, 0)
    nf = np.maximum(n, 1).astype(np.float32)
    large = 16 + (np.log(nf / np.float32(16.0)) / np.float32(math.log(128 / 16)) * np.float32(16.0)).astype(np.int32)
    large = np.minimum(large, 31)
    return np.where(n < 16, n, large)


FREP_W = 640
FAR_D0 = 129


def make_consts():
    c = {}
    c["ones"] = np.ones((128, 128), np.float32)
    c["ident"] = np.eye(128, dtype=np.float32)
    far = t5_bucket_np(np.arange(FAR_D0, S_LEN))
    assert (far == far[0]).all()
    oh = np.zeros((32, FREP_W + 1), np.float32)
    dd = np.arange(FREP_W)
    d = dd - 255
    bk = t5_bucket_np(d)
    for i in range(FREP_W):
        if d[i] >= 0:
            oh[bk[i], i] = 1.0
    oh[far[0], FREP_W] = 1.0
    c["oh"] = oh
    e8 = np.zeros((8, 8, 128), np.float32)
    for n in range(8):
        e8[n, n, :] = 1.0
    c["e8"] = e8
    return c


class Ctx:
    pass


class WStream:
    def __init__(self, K, st, name, shape, nslot, sem0=0):
        self.K = K
        self.name = name
        self.n = nslot
        self.t = [st.enter_context(SB(K.nc, f"{name}{i}", shape, BF16)) for i in range(nslot)]
        self.b = [Buf(f"{name}{i}") for i in range(nslot)]
        self.s = [K.wsem[sem0 + i] for i in range(nslot)]
        self.plan = []
        self.issued = 0
        self.base = 0

    def extend(self, srcs):
        self.plan.extend(srcs)
        self._pump()

    def _pump(self):
        while self.issued < len(self.plan) and self.issued < self.base + self.n:
            i = self.issued
            sl = i % self.n
            src = self.plan[i]
            dst = self.t[sl]
            self.K.S.dma("pool", self.s[sl], dst[:], src, W=[self.b[sl]])
            self.issued += 1

    def get(self):
        sl = self.base % self.n
        return self.t[sl], self.b[sl]

    def done(self):
        self.base += 1
        self._pump()


def mm(out, lhsT, rhs, start, stop):
    return lambda e: e.matmul(out, lhsT, rhs, start=start, stop=stop)


def rms_rstd(K, ps_stat, ps_b, rstd_t, rstd_b):
    S = K.S
    S.op("act", lambda e: e.activation(out=K.srt[:], in_=ps_stat[:], func=AF.Sqrt, scale=1.0 / D, bias=K.eps_ap),
         R=[ps_b], W=[K.srt_b])
    S.op("dve", lambda e: e.reciprocal(out=rstd_t, in_=K.srt[:]), R=[K.srt_b], W=[rstd_b])


def prenorm(K, t5, gcol, xn_t, xn_b, rstd_t, rstd_b, ps, ps_b):
    S = K.S
    t0 = t5 * 512
    xbs = [K.x_b[t5][c] for c in range(KC)]
    S.op("act", lambda e: e.activation(out=K.sq[:], in_=K.x[:, :, t0:t0 + 512], func=AF.Square),
         R=xbs, W=[K.sq_b])
    S.group("pe", [mm(ps[:], K.ones[:], K.sq[:, c, :], c == 0, c == KC - 1) for c in range(KC)],
            R=[K.sq_b], W=[ps_b])
    rms_rstd(K, ps, ps_b, rstd_t, rstd_b)
    for c in range(KC):
        S.op("dve", lambda e, c=c: e.scalar_tensor_tensor(
            out=xn_t[:, c, :], in0=K.x[:, c, t0:t0 + 512], scalar=K.pv[:, gcol + c:gcol + c + 1],
            in1=rstd_t, op0=ALU.mult, op1=ALU.mult), R=[xbs[c], rstd_b],
            W=[xn_b[c] if isinstance(xn_b, list) else xn_b])


def ffn_stage(K, l, which, TT):
    S, nc = K.S, K.nc
    nsub = TT // 512
    ntt = S_LEN // TT
    gpre = PV_LAYOUT[f"{which}_pre_g{l}"][0]
    gpost = PV_LAYOUT[f"{which}_post_g{l}"][0]
    wgu_src = K.dram[f"{which}_wgu{l}"]
    wd_src = K.dram[f"{which}_wd{l}"]
    P, Pb = K.ps, K.ps_b
    with ExitStack() as st:
        xn = st.enter_context(SB(nc, "f_xn", [128, nsub, KC, 512], BF16))
        h = st.enter_context(SB(nc, "f_h", [128, FC, nsub, 512], BF16))
        y = st.enter_context(SB(nc, "f_y", [128, nsub, KC, 512], F32))
        sg = [st.enter_context(SB(nc, f"f_sg{i}", [128, 512], F32)) for i in range(2)]
        ysq = [st.enter_context(SB(nc, f"f_ysq{i}", [128, 512], BF16)) for i in range(2)]
        rstd = st.enter_context(SB(nc, "f_rstd", [128, nsub, 512], F32))
        rstd2 = st.enter_context(SB(nc, "f_rstd2", [128, nsub, 512], F32))
        rstd2_b = [Buf("rstd2") for _ in range(nsub)]
        xn_b = [Buf("xn") for _ in range(nsub)]
        h_b = [[Buf("h") for _ in range(nsub)] for _ in range(FC)]
        y_b = [[Buf("y") for _ in range(KC)] for _ in range(nsub)]
        sg_b = [Buf("sg") for _ in range(2)]
        ysq_b = [Buf("ysq") for _ in range(2)]
        rstd_b = [Buf("rstd") for _ in range(nsub)]
        K.wgu = WStream(K, st, "wgu", [128, 2, KC, 128], 3, 0)
        K.wd = WStream(K, st, "wd", [128, FC, 128], 2, 3)

        K.wgu.extend([wgu_src[f] for _ in range(ntt) for f in range(FC)])
        K.wd.extend([wd_src[c] for _ in range(ntt) for c in range(KC)])

        def front(tt):
            for sub in range(nsub):
                t5 = tt * nsub + sub
                prenorm(K, t5, gpre, xn[:, sub], xn_b[sub], rstd2[:, sub, :], rstd2_b[sub], P[sub], Pb[sub])

        front(0)
        carry = []
        for tt in range(ntt):
            it = 0
            for f in range(FC):
                wt, wb = K.wgu.get()
                for sub in range(nsub):
                    s2 = it % 2
                    it += 1
                    gp, up = P[2 * s2], P[2 * s2 + 1]
                    S.group("pe", [mm(gp[:], wt[:, 0, k, :], xn[:, sub, k, :], k == 0, k == KC - 1) for k in range(KC)],
                            R=[wb, xn_b[sub]], W=[Pb[2 * s2]])
                    S.group("pe", [mm(up[:], wt[:, 1, k, :], xn[:, sub, k, :], k == 0, k == KC - 1) for k in range(KC)],
                            R=[wb, xn_b[sub]], W=[Pb[2 * s2 + 1]])
                    S.op("act", lambda e, gp=gp, s2=s2: e.activation(out=sg[s2][:], in_=gp[:], func=AF.Silu),
                         R=[Pb[2 * s2]], W=[sg_b[s2]])
                    S.op("dve", lambda e, up=up, s2=s2, f=f, sub=sub: e.tensor_tensor(
                        out=h[:, f, sub, :], in0=up[:], in1=sg[s2][:], op=ALU.mult),
                        R=[Pb[2 * s2 + 1], sg_b[s2]], W=[h_b[f][sub]])
                    if carry and f >= 1:
                        carry.pop(0)()
                K.wgu.done()
            while carry:
                carry.pop(0)()
            pend = None
            it = 0
            for c in range(KC):
                wt, wb = K.wd.get()
                for sub in range(nsub):
                    s2 = it % 2
                    it += 1
                    yp, ypb = P[4 + s2], Pb[4 + s2]
                    S.group("pe", [mm(yp[:], wt[:, f, :], h[:, f, sub, :], f == 0, f == FC - 1) for f in range(FC)],
                            R=[wb] + [h_b[f][sub] for f in range(FC)], W=[ypb])
                    S.op("dve", lambda e, yp=yp, sub=sub, c=c: e.tensor_copy(out=y[:, sub, c, :], in_=yp[:]),
                         R=[ypb], W=[y_b[sub][c]])
                    S.op("act", lambda e, sub=sub, c=c, s2=s2: e.activation(out=ysq[s2][:], in_=y[:, sub, c, :], func=AF.Square),
                         R=[y_b[sub][c]], W=[ysq_b[s2]])
                    if pend is not None:
                        pend()
                    pend = (lambda s2=s2, sub=sub, c=c: S.group(
                        "pe", [mm(P[6 + sub][:], K.ones[:], ysq[s2][:], c == 0, c == KC - 1)],
                        R=[ysq_b[s2]], W=[Pb[6 + sub]]))
                K.wd.done()
                if c == KC // 2 and tt + 1 < ntt:
                    front(tt + 1)
            pend()
            pieces = []
            for sub in range(nsub):
                t5 = tt * nsub + sub
                t0 = t5 * 512
                pieces.append(lambda sub=sub: rms_rstd(K, P[6 + sub], Pb[6 + sub], rstd[:, sub, :], rstd_b[sub]))
                for c in FIN_ORDER:
                    def piece(sub=sub, c=c, t0=t0, t5=t5):
                        S.op("dve", lambda e: e.scalar_tensor_tensor(
                            out=y[:, sub, c, :], in0=y[:, sub, c, :], scalar=K.hp[:, gpost + c:gpost + c + 1],
                            in1=rstd[:, sub, :], op0=ALU.mult, op1=ALU.mult),
                            R=[rstd_b[sub]], W=[y_b[sub][c]])
                        if c >= FIN_POOL_FROM:
                            S.op("pool", lambda e: e.tensor_tensor(
                                out=K.x[:, c, t0:t0 + 512], in0=K.x[:, c, t0:t0 + 512], in1=y[:, sub, c, :], op=ALU.add),
                                R=[y_b[sub][c]], W=[K.x_b[t5][c]])
                        else:
                            S.op("dve", lambda e: e.scalar_tensor_tensor(
                                out=K.x[:, c, t0:t0 + 512], in0=y[:, sub, c, :], scalar=1.0, in1=K.x[:, c, t0:t0 + 512],
                                op0=ALU.mult, op1=ALU.add), R=[y_b[sub][c]], W=[K.x_b[t5][c]])
                    pieces.append(piece)
            if tt + 1 < ntt:
                carry.extend(pieces)
            else:
                for p_ in pieces:
                    p_()
        S.barrier()


class PostNorm:
    def __init__(self, K, st, tag, nbuf=1):
        nc = K.nc
        self.K = K
        self.ys = [st.enter_context(SB(nc, f"{tag}_y{i}", [128, KC, 512], F32)) for i in range(nbuf)]
        self.y_bs = [[Buf("y") for _ in range(KC)] for _ in range(nbuf)]
        self.y = self.ys[0]
        self.ysq = [st.enter_context(SB(nc, f"{tag}_ysq{i}", [128, 512], BF16)) for i in range(2)]
        self.rstd = st.enter_context(SB(nc, tag + "_rstd", [128, 512], F32))
        self.y_b = self.y_bs[0]
        self.ysq_b = [Buf("ysq") for _ in range(2)]
        self.rstd_b = Buf("rstd")
        self.pend = None
        self.it = 0

    def use(self, slot):
        self.y = self.ys[slot]
        self.y_b = self.y_bs[slot]

    def evac(self, c, yp, ypb, bias_ap=None, stat_bank=6):
        K, S = self.K, self.K.S
        s2 = self.it % 2
        self.it += 1
        y, y_b = self.y, self.y_b
        if bias_ap is None:
            S.op("dve", lambda e: e.tensor_copy(out=y[:, c, :], in_=yp[:]), R=[ypb], W=[y_b[c]])
        else:
            S.op("dve", lambda e: e.tensor_scalar(out=y[:, c, :], in0=yp[:], scalar1=bias_ap, scalar2=None,
                                                  op0=ALU.add), R=[ypb], W=[y_b[c]])
        S.op("act", lambda e: e.activation(out=self.ysq[s2][:], in_=y[:, c, :], func=AF.Square),
             R=[y_b[c]], W=[self.ysq_b[s2]])
        if self.pend is not None:
            self.pend()
        P, Pb = K.ps[stat_bank], K.ps_b[stat_bank]
        self.pend = lambda: S.group("pe", [mm(P[:], K.ones[:], self.ysq[s2][:], c == 0, c == KC - 1)],
                                    R=[self.ysq_b[s2]], W=[Pb])

    def flush(self):
        if self.pend is not None:
            self.pend()
            self.pend = None

    def finish(self, t5, gt, gcol, stat_bank=6, slot=None):
        K, S = self.K, self.K.S
        self.flush()
        y, y_b = (self.y, self.y_b) if slot is None else (self.ys[slot], self.y_bs[slot])
        t0 = t5 * 512
        rms_rstd(K, K.ps[stat_bank], K.ps_b[stat_bank], self.rstd[:], self.rstd_b)
        for c in FIN_ORDER:
            S.op("dve", lambda e, c=c: e.scalar_tensor_tensor(
                out=y[:, c, :], in0=y[:, c, :], scalar=gt[:, gcol + c:gcol + c + 1],
                in1=self.rstd[:], op0=ALU.mult, op1=ALU.mult),
                R=[self.rstd_b], W=[y_b[c]])
            if c >= FIN_POOL_FROM:
                S.op("pool", lambda e, c=c: e.tensor_tensor(
                    out=K.x[:, c, t0:t0 + 512], in0=K.x[:, c, t0:t0 + 512], in1=y[:, c, :], op=ALU.add),
                    R=[y_b[c]], W=[K.x_b[t5][c]])
            else:
                S.op("dve", lambda e, c=c: e.scalar_tensor_tensor(
                    out=K.x[:, c, t0:t0 + 512], in0=y[:, c, :], scalar=1.0, in1=K.x[:, c, t0:t0 + 512],
                    op0=ALU.mult, op1=ALU.add), R=[y_b[c]], W=[K.x_b[t5][c]])


def lru_stage(K):
    S, nc = K.S, K.nc
    P, Pb = K.ps, K.ps_b
    NT = S_LEN // 512
    c_bin = PV_LAYOUT["lru_b_in"][0]
    c_cw = [PV_LAYOUT[f"lru_conv_w{k}"][0] for k in range(4)]
    c_cb = PV_LAYOUT["lru_conv_b"][0]
    c_br = PV_LAYOUT["lru_b_r"][0]
    c_bi = PV_LAYOUT["lru_b_i"][0]
    c_lam = PV_LAYOUT["lru_lambda"][0]
    c_bo = PV_LAYOUT["lru_b_out"][0]
    gpre = PV_LAYOUT["mix_pre_g0"][0]
    gpost = PV_LAYOUT["mix_post_g0"][0]
    win, wout, wg = K.dram["lru_win"], K.dram["lru_wout"], K.dram["lru_wg"]
    pv, hp = K.pv, K.hp
    with ExitStack() as st0, ExitStack() as st:
        T = lambda name, shape, dt=F32: st.enter_context(SB(nc, "l_" + name, shape, dt))
        xn = st0.enter_context(SB(nc, "l_xn", [128, KC, S_LEN], BF16))
        gT = st0.enter_context(SB(nc, "l_gT", [128, KC, S_LEN], BF16))
        K.wp = WStream(K, st0, "wp", [128, KC, 128], 6, 0)
        xbr = [T(f"xbr{i}", [128, 2, 515]) for i in range(2)]
        xc = [T(f"xc{i}", [128, 2, 512]) for i in range(2)]
        xcb = [T(f"xcb{i}", [128, 2, 512], BF16) for i in range(2)]
        gy = [T(f"gy{i}", [128, 2, 512]) for i in range(2)]
        r_t = T("r", [128, 2, 512])
        i_t = T("i", [128, 2, 512])
        a_t = T("a", [128, 2, 512])
        m_t = T("m", [128, 2, 512])
        u_t = T("u", [128, 2, 512])
        h_t = T("h", [128, 2, 512])
        hst = T("hst", [128, KC])
        cl = T("cl", [128, 2 * KC])
        rstd = T("rstd", [128, 512])
        xn_b = [Buf("xn") for _ in range(NT)]
        gT_b = [[Buf("gT") for _ in range(KC)] for _ in range(NT)]
        xbr_b = [[Buf("xbr") for _ in range(2)] for _ in range(2)]
        xc_b = [[Buf("xc") for _ in range(2)] for _ in range(2)]
        xcb_b = [[Buf("xcb") for _ in range(2)] for _ in range(2)]
        gy_b = [[Buf("gy") for _ in range(2)] for _ in range(2)]
        r_b = [Buf("r") for _ in range(2)]
        i_b = [Buf("i") for _ in range(2)]
        a_b = [Buf("a") for _ in range(2)]
        m_b = [Buf("m") for _ in range(2)]
        u_b = [Buf("u") for _ in range(2)]
        h_b = [Buf("h") for _ in range(2)]
        hst_b = [Buf("hst") for _ in range(KC)]
        cl_b, rstd_b = Buf("cl"), Buf("rstd")

        iters = [(hb, tt) for hb in range(4) for tt in range(NT)]
        a_tiles = lambda n: [win[iters[n][0] * 2], win[iters[n][0] * 2 + 1], win[8 + iters[n][0] * 2], win[8 + iters[n][0] * 2 + 1]]
        plan = a_tiles(0)
        for n in range(len(iters)):
            plan.append(wg[iters[n][0]])
            if n + 1 < len(iters):
                plan += a_tiles(n + 1)
        for t5 in range(NT):
            plan += [wout[c] for c in range(KC)]
        K.wp.extend(plan)

        S.op("act", lambda e: e.activation(out=cl[:, 0:KC], in_=pv[:, c_lam:c_lam + KC], func=AF.Exp, scale=-1.0), W=[cl_b])
        S.op("act", lambda e: e.activation(out=cl[:, 0:KC], in_=cl[:, 0:KC], func=AF.Ln, bias=K.one_ap), R=[cl_b], W=[cl_b])
        S.op("dve", lambda e: e.tensor_scalar(out=cl[:, KC:2 * KC], in0=cl[:, 0:KC], scalar1=-8.0, scalar2=None, op0=ALU.mult), R=[cl_b], W=[cl_b])
        S.op("dve", lambda e: e.tensor_scalar(out=cl[:, 0:KC], in0=cl[:, 0:KC], scalar1=-4.0, scalar2=None, op0=ALU.mult), R=[cl_b], W=[cl_b])

        def lru_prenorm(t5):
            prenorm(K, t5, gpre, xn[:, :, t5 * 512:(t5 + 1) * 512], xn_b[t5], rstd[:], rstd_b, P[6], Pb[6])

        lru_prenorm(0)

        iters = [(hb, tt) for hb in range(4) for tt in range(NT)]

        def stage_a_pe(n):
            hb, tt = iters[n]
            t0 = tt * 512
            for br in range(2):
                for jc in range(2):
                    wt, wb = K.wp.get()
                    pp, ppb = P[br * 2 + jc], Pb[br * 2 + jc]
                    S.group("pe", [mm(pp[:], wt[:, k, :], xn[:, k, t0:t0 + 512], k == 0, k == KC - 1) for k in range(KC)],
                            R=[wb, xn_b[tt]], W=[ppb])
                    K.wp.done()

        def stage_a_rest(n):
            hb, tt = iters[n]
            t0 = tt * 512
            d = n % 2
            cur, prv = xbr[d], xbr[1 - d]
            cur_b, prv_b = xbr_b[d], xbr_b[1 - d]
            for br in range(2):
                for jc in range(2):
                    ch = hb * 2 + jc
                    pp, ppb = P[br * 2 + jc], Pb[br * 2 + jc]
                    bcol = c_bin + br * KC + ch
                    if br == 0:
                        S.op("act", lambda e, pp=pp, jc=jc, bcol=bcol: e.activation(
                            out=cur[:, jc, 3:515], in_=pp[:], func=AF.Identity, bias=pv[:, bcol:bcol + 1]),
                            R=[ppb], W=[cur_b[jc]])
                    else:
                        S.op("act", lambda e, pp=pp, jc=jc, bcol=bcol: e.activation(
                            out=gy[d][:, jc, :], in_=pp[:], func=AF.Gelu_apprx_tanh, bias=pv[:, bcol:bcol + 1]),
                            R=[ppb], W=[gy_b[d][jc]])
            for jc in range(2):
                ch = hb * 2 + jc
                if tt == 0:
                    S.op("dve", lambda e, jc=jc: e.memset(cur[:, jc, 0:3], 0.0), W=[cur_b[jc]])
                else:
                    S.op("dve", lambda e, jc=jc: e.tensor_copy(out=cur[:, jc, 0:3], in_=prv[:, jc, 512:515]),
                         R=[prv_b[jc]], W=[cur_b[jc]])
                S.op("dve", lambda e, jc=jc, ch=ch: e.tensor_scalar(
                    out=xc[d][:, jc, :], in0=cur[:, jc, 0:512], scalar1=pv[:, c_cw[0] + ch:c_cw[0] + ch + 1],
                    scalar2=pv[:, c_cb + ch:c_cb + ch + 1], op0=ALU.mult, op1=ALU.add),
                    R=[cur_b[jc]], W=[xc_b[d][jc]])
                for k in range(1, 4):
                    S.op("dve", lambda e, jc=jc, ch=ch, k=k: e.scalar_tensor_tensor(
                        out=xc[d][:, jc, :], in0=cur[:, jc, k:k + 512], scalar=pv[:, c_cw[k] + ch:c_cw[k] + ch + 1],
                        in1=xc[d][:, jc, :], op0=ALU.mult, op1=ALU.add),
                        R=[cur_b[jc]], W=[xc_b[d][jc]])
                S.op("act", lambda e, jc=jc: e.activation(out=xcb[d][:, jc, :], in_=xc[d][:, jc, :], func=AF.Copy),
                     R=[xc_b[d][jc]], W=[xcb_b[d][jc]])

        def stage_b_pe(n):
            hb, tt = iters[n]
            d = n % 2
            wt, wb = K.wp.get()
            for jc in range(2):
                for g in range(2):
                    pp, ppb = P[4 + jc * 2 + g], Pb[4 + jc * 2 + g]
                    S.group("pe", [mm(pp[:], wt[:, g * 4 + ic * 2 + jc, :], xcb[d][:, ic, :], ic == 0, ic == 1) for ic in range(2)],
                            R=[wb, xcb_b[d][0], xcb_b[d][1]], W=[ppb])
            K.wp.done()

        def stage_b_rest(n):
            hb, tt = iters[n]
            t0 = tt * 512
            d = n % 2
            for jc in range(2):
                ch = hb * 2 + jc
                S.op("act", lambda e, jc=jc, ch=ch: e.activation(
                    out=r_t[:, jc, :], in_=P[4 + jc * 2][:], func=AF.Tanh, scale=0.5, bias=hp[:, c_br + ch:c_br + ch + 1]),
                    R=[Pb[4 + jc * 2]], W=[r_b[jc]])
                S.op("act", lambda e, jc=jc, ch=ch: e.activation(
                    out=i_t[:, jc, :], in_=P[4 + jc * 2 + 1][:], func=AF.Tanh, scale=0.5, bias=hp[:, c_bi + ch:c_bi + ch + 1]),
                    R=[Pb[4 + jc * 2 + 1]], W=[i_b[jc]])
            for jc in range(2):
                ch = hb * 2 + jc
                S.op("act", lambda e, jc=jc, ch=ch: e.activation(
                    out=a_t[:, jc, :], in_=r_t[:, jc, :], func=AF.Exp, scale=cl[:, ch:ch + 1], bias=cl[:, ch:ch + 1]),
                    R=[r_b[jc], cl_b], W=[a_b[jc]])
                S.op("act", lambda e, jc=jc, ch=ch: e.activation(
                    out=m_t[:, jc, :], in_=r_t[:, jc, :], func=AF.Exp, scale=cl[:, KC + ch:KC + ch + 1], bias=cl[:, KC + ch:KC + ch + 1]),
                    R=[r_b[jc], cl_b], W=[m_b[jc]])
                S.op("dve", lambda e, jc=jc: e.scalar_tensor_tensor(
                    out=u_t[:, jc, :], in0=i_t[:, jc, :], scalar=1.0, in1=xc[d][:, jc, :], op0=ALU.add, op1=ALU.mult),
                    R=[i_b[jc], xc_b[d][jc]], W=[u_b[jc]])
            for jc in range(2):
                S.op("act", lambda e, jc=jc: e.activation(
                    out=m_t[:, jc, :], in_=m_t[:, jc, :], func=AF.Ln, scale=-1.0, bias=K.one_ap),
                    R=[m_b[jc]], W=[m_b[jc]])
            for jc in range(2):
                S.op("act", lambda e, jc=jc: e.activation(
                    out=m_t[:, jc, :], in_=m_t[:, jc, :], func=AF.Exp, scale=0.5),
                    R=[m_b[jc]], W=[m_b[jc]])

        def stage_b_rest2(n):
            hb, tt = iters[n]
            t0 = tt * 512
            d = n % 2
            for jc in range(2):
                ch = hb * 2 + jc
                S.op("dve", lambda e, jc=jc: e.scalar_tensor_tensor(
                    out=u_t[:, jc, :], in0=u_t[:, jc, :], scalar=0.5, in1=m_t[:, jc, :], op0=ALU.mult, op1=ALU.mult),
                    R=[m_b[jc]], W=[u_b[jc]])
                init = 0.0 if tt == 0 else hst[:, ch:ch + 1]
                S.op("dve", lambda e, jc=jc, init=init: e.tensor_tensor_scan(
                    out=h_t[:, jc, :], data0=a_t[:, jc, :], data1=u_t[:, jc, :], initial=init, op0=ALU.mult, op1=ALU.add),
                    R=[a_b[jc], u_b[jc], hst_b[ch]], W=[h_b[jc]])
                S.op("dve", lambda e, jc=jc, ch=ch: e.tensor_copy(out=hst[:, ch:ch + 1], in_=h_t[:, jc, 511:512]),
                     R=[h_b[jc]], W=[hst_b[ch]])
                S.op("pool", lambda e, jc=jc, ch=ch: e.tensor_tensor(
                    out=gT[:, ch, t0:t0 + 512], in0=h_t[:, jc, :], in1=gy[d][:, jc, :], op=ALU.mult),
                    R=[h_b[jc], gy_b[d][jc]], W=[gT_b[tt][ch]])

        lru_prenorm(1)
        stage_a_pe(0)
        stage_a_rest(0)
        for n in range(len(iters)):
            stage_b_pe(n)
            if n + 1 < len(iters):
                stage_a_pe(n + 1)
            stage_b_rest(n)
            if n + 2 < NT:
                lru_prenorm(n + 2)
            if n + 1 < len(iters):
                stage_a_rest(n + 1)
            stage_b_rest2(n)
        S.barrier()
        st.close()
        pn = PostNorm(K, st, "l_pn", nbuf=2)
        it = 0

        def body(t5):
            nonlocal it
            t0 = t5 * 512
            pn.use(t5 % 2)
            for c in range(KC):
                wt, wb = K.wp.get()
                pp, ppb = P[it % 2], Pb[it % 2]
                it += 1
                S.group("pe", [mm(pp[:], wt[:, k, :], gT[:, k, t0:t0 + 512], k == 0, k == KC - 1) for k in range(KC)],
                        R=[wb] + gT_b[t5], W=[ppb])
                K.wp.done()
                pn.evac(c, pp, ppb, bias_ap=pv[:, c_bo + c:c_bo + c + 1], stat_bank=6 + t5 % 2)
            pn.flush()

        body(0)
        for t5 in range(NT):
            if t5 + 1 < NT:
                body(t5 + 1)
            pn.finish(t5, K.pv, gpost, stat_bank=6 + t5 % 2, slot=t5 % 2)
        S.barrier()


def kvnorm_stage(K):
    S, nc = K.S, K.nc
    P, Pb = K.ps, K.ps_b
    scale = float(HD) ** -0.5
    K.cst_stack = ExitStack()
    if True:
        TR = lambda name, shape, dt=F32: K.cst_stack.enter_context(SB(nc, "c_" + name, shape, dt, side="right"))
        M = K.M = TR("M", [128, NHEAD, 512], BF16)
        cb = K.cb = TR("cb", [128, NHEAD])
        identb = K.identb = TR("identb", [128, 128], BF16)
        ident32 = K.ident32 = TR("ident32", [128, 128])
        E8 = K.E8 = K.cst_stack.enter_context(SB(nc, "c_E8", [8, NBLK, 128], BF16, side="right"))
        zt = K.zt = TR("zero", [128, 1])
        M_b, cb_b, cst_b = Buf("M"), Buf("cb"), Buf("cst")
        st1 = ExitStack()
        if True:
            rbT = st1.enter_context(SB(nc, "a_rbT", [32, NHEAD], F32))
            oh = st1.enter_context(SB(nc, "a_oh", [32, FREP_W + 1], F32))
            ones32 = st1.enter_context(SB(nc, "a_ones32", [32, 128], F32))
            rbb = st1.enter_context(SB(nc, "a_rbb", [32, NHEAD, 128], F32))
            frep = st1.enter_context(SB(nc, "a_frep", [128, NHEAD, FREP_W], BF16))
            rb_b, rbb_b, frep_b, fd_b = Buf("rb"), Buf("rbb"), Buf("frep"), Buf("fd")
            S.dma("sp", "ld_c", rbT[:], K.dram["rbT"][:, :], W=[rb_b])
            S.dma("sp", "ld_c", oh[:], K.dram["oh"][:, :], W=[rb_b])
            tk2 = S.dma("sp", "ld_c", ident32[:], K.dram["ident"][:, :], W=[cst_b])
            rb_b.w = tk2
            S.dma("pool", "ld_c2", identb[:], K.dram["ident"][:, :], W=[cst_b])
            tk3 = S.dma("pool", "ld_c2", E8[:], K.dram["e8"][:, :, :], W=[cst_b])
            S.op("dve", lambda e: e.memset(zt[:], 0.0), W=[cst_b])
            S.op("dve", lambda e: e.memset(ones32[:], 1.0), W=[rbb_b])
            for h in range(NHEAD):
                S.op("dve", lambda e, h=h: e.tensor_scalar(out=rbb[:, h, :], in0=ones32[:], scalar1=rbT[:, h:h + 1],
                                                           scalar2=None, op0=ALU.mult), R=[rb_b], W=[rbb_b])
            h2 = FREP_W // 2
            for h in range(NHEAD):
                pa, pab = P[(2 * h) % 4], Pb[(2 * h) % 4]
                pb_, pbb = P[(2 * h + 1) % 4], Pb[(2 * h + 1) % 4]
                S.group("pe", [mm(pa[:, 0:h2], rbb[:, h, :], oh[:, 0:h2], True, True)], R=[rbb_b, rb_b], W=[pab])
                S.group("pe", [mm(pb_[:, 0:h2 + 1], rbb[:, h, :], oh[:, h2:FREP_W + 1], True, True)], R=[rbb_b, rb_b], W=[pbb])
                S.op("act", lambda e, h=h, pa=pa: e.activation(out=frep[:, h, 0:h2], in_=pa[:, 0:h2], func=AF.Copy, scale=1.0 / scale),
                     R=[pab], W=[frep_b])
                S.op("act", lambda e, h=h, pb_=pb_: e.activation(out=frep[:, h, h2:FREP_W], in_=pb_[:, 0:h2], func=AF.Copy, scale=1.0 / scale),
                     R=[pbb], W=[frep_b])
                S.op("act", lambda e, h=h, pb_=pb_: e.activation(out=cb[:, h:h + 1], in_=pb_[:, h2:h2 + 1], func=AF.Copy),
                     R=[pbb], W=[cb_b])
            S.op("dve", lambda e: e.memset(frep[:, :, 0:255], NEG), R=[frep_b], W=[frep_b])
            fd = K.frep_dram
            S.dma("sp", "ld_c", fd.ap(), frep[:].rearrange("p h w -> p (h w)"), R=[frep_b], W=[fd_b])
            skew = bass.AP(tensor=fd, offset=127, ap=[[NHEAD * FREP_W - 1, 128], [FREP_W, NHEAD], [1, 512]])
            with nc.allow_non_contiguous_dma(reason="toeplitz skew"):
                S.dma("sp", "ld_c", M[:], skew, R=[fd_b], W=[M_b])
    K.kv_stack = ExitStack()
    K.xnkv = K.kv_stack.enter_context(SB(nc, "xnkv", [128, KC, S_LEN], BF16, side="right"))
    K.xnkv_b = [Buf("xnkv") for _ in range(S_LEN // 512)]
    g = PV_LAYOUT["kv_norm_g"][0]
    with ExitStack() as st:
        rstd = st.enter_context(SB(nc, "kv_rstd", [128, 512], F32))
        rstd_b = Buf("rstd")
        for t5 in range(S_LEN // 512):
            prenorm(K, t5, g, K.xnkv[:, :, t5 * 512:(t5 + 1) * 512], K.xnkv_b[t5], rstd[:], rstd_b, K.ps[6], K.ps_b[6])
        S.barrier()
        for e in ("pe", "act", "dve", "pool"):
            S._wait(e, M_b.w)
            S._wait(e, tk3)
            S._wait(e, tk2)
    st1.close()


def attn_stage(K):
    S, nc = K.S, K.nc
    P, Pb = K.ps, K.ps_b
    NT = S_LEN // 512
    pv = K.pv
    gpre = PV_LAYOUT["mix_pre_g1"][0]
    gpost = PV_LAYOUT["mix_post_g1"][0]
    scale = float(HD) ** -0.5
    with ExitStack() as st0:
        T0 = lambda name, shape, dt=F32: st0.enter_context(SB(nc, "a_" + name, shape, dt))
        KT = T0("KT", [128, NHEAD, S_LEN], BF16)
        V = T0("V", [128, S_LEN // 128, D], BF16)
        kmT = T0("kmT", [128, NHEAD, NBLK], BF16)
        K.wp = WStream(K, st0, "wp", [128, KC, 128], 3, 0)
        KT_b = [[Buf("KT") for _ in range(NT)] for _ in range(NHEAD)]
        V_b = [Buf("V") for _ in range(S_LEN // 128)]
        kmT_b = Buf("kmT")
        with ExitStack() as st1:
            wv = st1.enter_context(SB(nc, "a_wv", [128, KC, D], BF16))
            kms = st1.enter_context(SB(nc, "a_kms", [128, NHEAD, NBLK], F32))
            wv_bs, kms_b = [Buf("wv") for _ in range(KC)], Buf("kms")
            K.wp.extend([K.dram["wk"][h] for h in range(NHEAD)])
            tok = None
            for k in range(KC):
                tok = S.dma("pool", K.wsem[6], wv[:, k, :], K.dram["wv"][k], W=[wv_bs[k]])
            for b in wv_bs:
                b.w = tok
            it = 0
            for h in range(NHEAD):
                wt, wb = K.wp.get()
                for t5 in range(NT):
                    t0 = t5 * 512
                    pp, ppb = P[it % 2], Pb[it % 2]
                    it += 1
                    S.group("pe", [mm(pp[:], wt[:, k, :], K.xnkv[:, k, t0:t0 + 512], k == 0, k == KC - 1) for k in range(KC)],
                            R=[wb, K.xnkv_b[t5]], W=[ppb])
                    S.op("act", lambda e, pp=pp, h=h, t0=t0: e.activation(out=KT[:, h, t0:t0 + 512], in_=pp[:], func=AF.Copy),
                         R=[ppb], W=[KT_b[h][t5]])
                    S.op("dve", lambda e, pp=pp, h=h, t5=t5: e.tensor_reduce(
                        out=kms[:, h, 2 * t5:2 * t5 + 2], in_=pp[:].rearrange("p (b j) -> p b j", j=BLK), axis=AX.X, op=ALU.add),
                        R=[ppb], W=[kms_b])
                K.wp.done()
            S.op("dve", lambda e: e.tensor_scalar(out=kmT[:], in0=kms[:], scalar1=1.0 / BLK, scalar2=None, op0=ALU.mult),
                 R=[kms_b], W=[kmT_b])
            it = 0
            for tk in range(S_LEN // 128):
                for half in range(2):
                    pp, ppb = P[2 + it % 2], Pb[2 + it % 2]
                    S.group("pe", [mm(pp[:], K.xnkv[:, k, tk * 128:(tk + 1) * 128], wv[:, k, half * 512:(half + 1) * 512],
                                      k == 0, k == KC - 1) for k in range(KC)],
                            R=wv_bs + [K.xnkv_b[tk // 4]], W=[ppb])
                    if it % 2 == 0:
                        S.op("act", lambda e, pp=pp, tk=tk, half=half: e.activation(
                            out=V[:, tk, half * 512:(half + 1) * 512], in_=pp[:], func=AF.Copy), R=[ppb], W=[V_b[tk]])
                    else:
                        S.op("dve", lambda e, pp=pp, tk=tk, half=half: e.tensor_copy(
                            out=V[:, tk, half * 512:(half + 1) * 512], in_=pp[:]), R=[ppb], W=[V_b[tk]])
                    it += 1
            S.barrier()
        K.kv_stack.close()
        M, cb, identb, ident32, E8, zt = K.M, K.cb, K.identb, K.ident32, K.E8, K.zt
        T1 = T0
        xn = K.sq
        aT_t = T1("aT", [128, NHEAD, 512], BF16)
        QT = T1("QT", [128, NHEAD, 512], BF16)
        aT = aT_t
        AT = st0.enter_context(SB(nc, "a_AT", [8, NHEAD, 512], BF16))
        PT = [T1(f"PT{i}", [128, 256], BF16) for i in range(6)]
        psum_acc = [T1(f"pacc{i}", [128, 256]) for i in range(2)]
        rden = psum_acc
        pacc_b = [Buf("pacc") for _ in range(2)]
        paccbf_b = [Buf("paccbf") for _ in range(2)]
        gs = T1("gs", [128, NHEAD, NBLK])
        mx = T1("mx", [128, NHEAD, 8])
        Am = T1("Am", [128, NHEAD, NBLK])
        pn = PostNorm(K, st0, "a_pn")
        rstd, rstd_b = pn.rstd, pn.rstd_b
        xn_b = K.sq_b
        QT_b = [Buf("QT") for _ in range(NHEAD)]
        aT_b = [Buf("aT") for _ in range(NHEAD)]
        AT_b = [Buf("AT") for _ in range(4)]
        PT_b = [Buf("PT") for _ in range(6)]
        gs_b, mx_b, Am_b = Buf("gs"), Buf("mx"), Buf("Am")
        ipt = 0
        iod = 0
        def front_norm(t5):
            prenorm(K, t5, gpre, xn[:], xn_b, rstd[:], rstd_b, P[7], Pb[7])

        def front_q(t5):
            K.wp.extend([K.dram["wq"][h] for h in range(NHEAD)])
            for h in range(NHEAD):
                wt, wb = K.wp.get()
                pp, ppb = P[6 + h % 2], Pb[6 + h % 2]
                S.group("pe", [mm(pp[:], wt[:, k, :], xn[:, k, :], k == 0, k == KC - 1) for k in range(KC)],
                        R=[wb, xn_b], W=[ppb])
                K.wp.done()
                if h % 2 == 0:
                    S.op("act", lambda e, pp=pp, h=h: e.activation(out=QT[:, h, :], in_=pp[:], func=AF.Copy), R=[ppb], W=[QT_b[h]])
                else:
                    S.op("dve", lambda e, pp=pp, h=h: e.tensor_copy(out=QT[:, h, :], in_=pp[:]), R=[ppb], W=[QT_b[h]])

        def front_gates(t5):
            if 2 * t5 >= 4:
                for s4 in range(4):
                    qb = 2 * t5 + s4 // 2
                    c0 = s4 * 128
                    S.group("pe", [mm(P[6][:, h * NBLK:(h + 1) * NBLK], QT[:, h, c0:c0 + 128], kmT[:, h, :], True, True)
                                   for h in range(NHEAD)], R=QT_b + [kmT_b], W=[Pb[6]])
                    S.op("dve", lambda e: e.memset(gs[:], -1e30), W=[gs_b])
                    S.op("dve", lambda e, qb=qb: e.tensor_copy(
                        out=gs[:, :, 0:qb], in_=P[6][:, 0:NHEAD * NBLK].rearrange("p (h n) -> p h n", n=NBLK)[:, :, 0:qb]),
                        R=[Pb[6]], W=[gs_b])
                    for h in range(NHEAD):
                        S.op("dve", lambda e, h=h: e.max(out=mx[:, h, :], in_=gs[:, h, :]), R=[gs_b], W=[mx_b])
                    for h in range(NHEAD):
                        S.op("dve", lambda e, h=h: e.tensor_scalar(out=Am[:, h, :], in0=gs[:, h, :], scalar1=mx[:, h, 2:3],
                                                                   scalar2=NEG, op0=ALU.is_lt, op1=ALU.mult),
                             R=[gs_b, mx_b], W=[Am_b])
                    for g in range(2):
                        pp, ppb = P[7], Pb[7]
                        S.group("pe", [(lambda e, j=j, g=g, pp=pp: e.transpose(pp[0:8, j * 128:(j + 1) * 128], Am[:, g * 4 + j, :], ident32[:]))
                                       for j in range(4)], R=[Am_b], W=[ppb])
                        S.op("act", lambda e, g=g, c0=c0, pp=pp: e.activation(
                            out=AT[0:8, g * 4:(g + 1) * 4, c0:c0 + 128], in_=pp[0:8, :].rearrange("p (h t) -> p h t", t=128),
                            func=AF.Copy), R=[ppb], W=[AT_b[s4]])

        def inner(t5, hook=None):
            items = []
            for h in range(NHEAD):
                for qb2 in range(2):
                    qb = 2 * t5 + qb2
                    for kt in range(2 * qb + 2):
                        items.append((h, qb2, kt))
            SK = 3
            SB_ = [0, 1, 6, 7]
            st_items = {}

            def emit_qk(idx):
                h, qb2, kt = items[idx]
                qb = 2 * t5 + qb2
                qo = qb2 * 256
                far = kt <= 2 * qb - 2
                bk = SB_[idx % 4]
                ps, psb = P[bk], Pb[bk]
                ops = [(KT[:, h, kt * 128:(kt + 1) * 128], QT[:, h, qo:qo + 256])]
                R = [KT_b[h][kt // 4], QT_b[h]]
                if kt < 2 * qb and qb >= 4:
                    ops.append((E8[0:8, kt // 2, :], AT[0:8, h, qo:qo + 256]))
                    R += [AT_b[2 * qb2], AT_b[2 * qb2 + 1]]
                if not far:
                    off = {2 * qb - 1: 256, 2 * qb: 128, 2 * qb + 1: 0}[kt]
                    ops.append((identb[:], M[:, h, off:off + 256]))
                S.group("pe", [mm(ps[:, 0:256], a, b, i == 0, i == len(ops) - 1) for i, (a, b) in enumerate(ops)],
                        R=R, W=[psb])
                i4 = idx % len(PT)
                bias_ap = cb[:, h:h + 1] if far else zt[:, 0:1]
                S.op("act", lambda e: e.activation(out=PT[i4][:], in_=ps[:, 0:256], func=AF.Exp, scale=scale, bias=bias_ap),
                     R=[psb], W=[PT_b[i4]])

            def emit_pv(idx):
                h, qb2, kt = items[idx]
                qb = 2 * t5 + qb2
                qo = qb2 * 256
                nkt = 2 * qb + 2
                j = (h * 2 + qb2) % 2
                po, pob, pd, pdb = P[2 + j], Pb[2 + j], P[4 + j], Pb[4 + j]
                i4 = idx % len(PT)
                S.group("pe", [mm(po[:, 0:256], V[:, kt, h * 128:(h + 1) * 128], PT[i4][:], kt == 0, kt == nkt - 1)],
                        R=[V_b[kt], PT_b[i4]], W=[pob])
                S.group("pe", [mm(pd[:, 0:256], K.ones[:], PT[i4][:], kt == 0, kt == nkt - 1)],
                        R=[PT_b[i4]], W=[pdb])
                if kt == nkt - 1:
                    S.op("dve", lambda e: e.reciprocal(out=rden[j][:], in_=pd[:, 0:256]), R=[pdb], W=[pacc_b[j]])
                    S.op("dve", lambda e: e.tensor_tensor(out=aT[:, h, qo:qo + 256], in0=po[:, 0:256], in1=rden[j][:], op=ALU.mult),
                         R=[pob, pacc_b[j]], W=[aT_b[h]])

            hook_at = (len(items) * 3) // 4
            for idx in range(len(items) + SK):
                if idx < len(items):
                    emit_qk(idx)
                if idx >= SK:
                    emit_pv(idx - SK)
                if idx == hook_at and hook is not None:
                    hook()

        def back(t5):
            K.wp.extend([K.dram["wo"][c] for c in range(KC)])
            for c in range(KC):
                wt, wb = K.wp.get()
                pp, ppb = P[6 + c % 2], Pb[6 + c % 2]
                S.group("pe", [mm(pp[:], wt[:, k, :], aT[:, k, :], k == 0, k == KC - 1) for k in range(KC)],
                        R=[wb] + aT_b, W=[ppb])
                K.wp.done()
                pn.evac(c, pp, ppb, stat_bank=5)
            pn.flush()

        front_norm(0)
        front_q(0)
        front_gates(0)
        for t5 in range(NT):
            nxt = t5 + 1 < NT
            inner(t5, hook=(lambda t=t5 + 1: front_norm(t)) if nxt else None)
            back(t5)
            if nxt:
                front_q(t5 + 1)
                front_gates(t5 + 1)
            pn.finish(t5, K.pv, gpost, stat_bank=5)
        S.barrier()
    K.cst_stack.close()

def build_program(stages, winfo):
    _pv_plan()
    nc = bass.Bass("TRN2", target_bir_lowering=False)
    K = Ctx()
    K.nc = nc
    K.dram = {}
    for name, shape in winfo.items():
        K.dram[name] = nc.dram_tensor(name, list(shape), F32, kind="ExternalInput").ap()
    xT = nc.dram_tensor("xT", [D, S_LEN], F32, kind="ExternalInput").ap()
    pvd = nc.dram_tensor("pv", [128, PV_LAYOUT["_ncol"][0]], F32, kind="ExternalInput").ap()
    onesd = nc.dram_tensor("ones", [128, 128], F32, kind="ExternalInput").ap()
    outT = nc.dram_tensor("outT", [D, S_LEN], F32, kind="ExternalOutput").ap()
    K.frep_dram = nc.dram_tensor("frep_scratch", [128, NHEAD * FREP_W], BF16, kind="Internal")
    ncol = PV_LAYOUT["_ncol"][0]
    with ExitStack() as stack:
        K.stack = stack
        S = K.S = Sched(nc, stack)
        K.x = stack.enter_context(SB(nc, "x_res", [128, KC, S_LEN], F32))
        K.x_b = [[Buf(f"x{t}_{c}") for c in range(KC)] for t in range(S_LEN // 512)]
        K.pv = stack.enter_context(SB(nc, "pv_sb", [128, ncol], F32))
        K.hp = stack.enter_context(SB(nc, "hp_sb", [128, ncol], F32))
        K.ones = stack.enter_context(SB(nc, "ones_sb", [128, 128], BF16))
        K.epst = stack.enter_context(SB(nc, "eps_sb", [128, 1], F32))
        K.eps_ap = K.epst[:, 0:1]
        K.sq = stack.enter_context(SB(nc, "sq", [128, KC, 512], BF16))
        K.sq_b = Buf("sq")
        K.srt = stack.enter_context(SB(nc, "srt", [128, 512], F32))
        K.srt_b = Buf("srt")
        K.ps = [stack.enter_context(nc.psum_tensor(f"ps{i}", [128, 512], F32)) for i in range(8)]
        K.ps_b = [Buf(f"ps{i}", excl=True) for i in range(8)]
        K.wsem = [S.new_sem(f"w{i}") for i in range(8)]
        K.onet = stack.enter_context(SB(nc, "one_sb", [128, 1], F32))
        K.one_ap = K.onet[:, 0:1]
        pv_b, ones_b, eps_b, hp_b = Buf("pv"), Buf("ones"), Buf("eps"), Buf("hp")
        S.new_sem("ld_x")
        S.new_sem("ld_c")
        S.new_sem("st_x")
        S.new_sem("ld_c2")
        S.dma("sp", "ld_c", K.pv[:], pvd[:, :], W=[pv_b])
        tokc = S.dma("pool", "ld_c2", K.ones[:], onesd[:, :], W=[ones_b])
        S.op("dve", lambda e: e.memset(K.epst[:], EPS), W=[eps_b])
        S.op("dve", lambda e: e.memset(K.onet[:], 1.0), W=[eps_b])
        S.op("dve", lambda e: e.tensor_scalar(out=K.hp[:], in0=K.pv[:], scalar1=0.5, scalar2=None, op0=ALU.mult),
             R=[pv_b], W=[hp_b])
        tok = None
        for c in range(KC):
            tok = S.dma("sp", "ld_x", K.x[:, c, :], xT[c * 128:(c + 1) * 128, :],
                        W=[K.x_b[t][c] for t in range(S_LEN // 512)])
        for t in range(S_LEN // 512):
            for c in range(KC):
                K.x_b[t][c].w = tok
        S.barrier()
        for e in ("pe", "act", "dve", "pool"):
            S._wait(e, tokc)
            S._wait(e, hp_b.w)
            S._wait(e, eps_b.w)
        for stg in stages:
            if stg[0] == "ffn":
                _, l, which, TT = stg
                ffn_stage(K, l, which, TT)
            elif stg[0] == "lru":
                lru_stage(K)
            elif stg[0] == "kvnorm":
                kvnorm_stage(K)
            elif stg[0] == "attn":
                attn_stage(K)
            else:
                raise ValueError(stg)
        tok = None
        for c in range(KC):
            tok = S.dma("sp", "st_x", outT[c * 128:(c + 1) * 128, :], K.x[:, c, :],
                        R=[K.x_b[t][c] for t in range(S_LEN // 512)])
        S._wait("sp", tok)
    return nc


ALL_STAGES = [("ffn", 0, "ffn1", 1024), ("lru",), ("ffn", 0, "ffn2", 1024), ("kvnorm",),
              ("ffn", 1, "ffn1", 512), ("attn",), ("ffn", 1, "ffn2", 1024)]


def stage_inputs(stages, W, C):
    need = []
    for stg in stages:
        if stg[0] == "ffn":
            need += [f"{stg[2]}_wgu{stg[1]}", f"{stg[2]}_wd{stg[1]}"]
        elif stg[0] == "lru":
            need += ["lru_win", "lru_wout", "lru_wg"]
        elif stg[0] == "attn":
            need += ["wq", "wo", "wk", "wv", "rbT"]
    d = {k: W[k] for k in need}
    if any(s[0] == "attn" for s in stages):
        for k in ("oh", "ident", "e8"):
            d[k] = C[k]
    return d


def run_stages(stages, xT_list, pv, W, C):
    use = stage_inputs(stages, W, C)
    nc = build_program(stages, {k: v.shape for k, v in use.items()})
    in_maps = []
    for xT in xT_list:
        m = dict(use)
        m["xT"] = xT
        m["pv"] = pv
        m["ones"] = C["ones"]
        in_maps.append(m)
    res = run_bass_kernel_spmd(nc, in_maps, core_ids=list(range(len(xT_list))))
    return [r["outT"] for r in res.results]


def kernel(**inputs):
    inp = {k: np.asarray(v) for k, v in inputs.items()}
    x = inp["x"].astype(np.float32, copy=False)
    pv = pack_pv(inp)
    W = pack_weights(inp)
    C = make_consts()
    xT = [np.ascontiguousarray(x[b].T) for b in range(NB)]
    outT = run_stages(ALL_STAGES, xT, pv, W, C)
    return np.stack([o.T for o in outT], axis=0).astype(np.float32)
```

```python
from contextlib import ExitStack
import math
import numpy as np
import concourse.bass as bass
import concourse.mybir as mybir
from concourse.bass_utils import run_bass_kernel_spmd

F32 = mybir.dt.float32
BF16 = mybir.dt.bfloat16
AF = mybir.ActivationFunctionType
ALU = mybir.AluOpType
AX = mybir.AxisListType

D = 1024
S_LEN = 2048
NB = 8
DFF = 2816
KC = D // 128
FC = DFF // 128
EPS = 1e-6
NHEAD = 8
HD = 128
BLK = 256
NBLK = S_LEN // BLK
NEG = -30000.0
FIN_POOL_FROM = 5
FIN_ORDER = [5, 6, 7, 0, 1, 2, 3, 4]


_UNIQ = [0]


def SB(nc, name, shape, dt, **kw):
    _UNIQ[0] += 1
    return nc.sbuf_tensor(f"{name}_{_UNIQ[0]}", shape, dt, **kw)


class Buf:
    __slots__ = ("name", "w", "r", "excl")

    def __init__(self, name, excl=False):
        self.name = name
        self.w = None
        self.r = []
        self.excl = excl


class Sched:
    ENGS = ("pe", "act", "dve", "pool", "sp")

    def __init__(self, nc, stack):
        self.nc = nc
        self.stack = stack
        self.eng = dict(pe=nc.tensor, act=nc.scalar, dve=nc.vector, pool=nc.gpsimd, sp=nc.sync)
        self.sem = {}
        self.cnt = {}
        self.seen = {e: {} for e in self.ENGS}
        for e in self.ENGS:
            self.new_sem("c_" + e)

    def new_sem(self, name):
        self.sem[name] = self.stack.enter_context(self.nc.semaphore(name))
        self.cnt[name] = 0
        return name

    def _wait(self, e, tok):
        if tok is None:
            return
        s, v = tok
        if e == "pe" and s == "c_pe":
            return
        if self.seen[e].get(s, 0) >= v:
            return
        self.eng[e].wait_ge(self.sem[s], v)
        self.seen[e][s] = v

    @staticmethod
    def _split(R, W):
        if any(b.excl for b in R):
            W = list(W) + [b for b in R if b.excl]
            R = [b for b in R if not b.excl]
        return R, W

    def _deps(self, e, R, W):
        for b in R:
            self._wait(e, b.w)
        for b in W:
            self._wait(e, b.w)
            for t in b.r:
                self._wait(e, t)

    def _commit(self, tok, R, W):
        for b in W:
            b.w = tok
            b.r = []
        for b in R:
            b.r.append(tok)
            if len(b.r) > 12:
                best = {}
                for (s, v) in b.r:
                    if best.get(s, 0) < v:
                        best[s] = v
                b.r = list(best.items())

    def op(self, e, fn, R=(), W=()):
        R, W = self._split(R, W)
        self._deps(e, R, W)
        ins = fn(self.eng[e])
        s = "c_" + e
        self.cnt[s] += 1
        ins.then_inc(self.sem[s], 1)
        tok = (s, self.cnt[s])
        self._commit(tok, R, W)
        return tok

    def group(self, e, fns, R=(), W=()):
        R, W = self._split(R, W)
        self._deps(e, R, W)
        ins = None
        for fn in fns:
            ins = fn(self.eng[e])
        s = "c_" + e
        self.cnt[s] += 1
        ins.then_inc(self.sem[s], 1)
        tok = (s, self.cnt[s])
        self._commit(tok, R, W)
        return tok

    def dma(self, e, sem, out, in_, R=(), W=(), n=1):
        self._deps(e, R, W)
        self.eng[e].dma_start(out=out, in_=in_).then_inc(self.sem[sem], 16)
        self.cnt[sem] += 16
        tok = (sem, self.cnt[sem])
        self._commit(tok, R, W)
        return tok

    def barrier(self):
        for e in self.ENGS:
            for p in self.ENGS:
                if self.cnt["c_" + p] > 0:
                    self._wait(e, ("c_" + p, self.cnt["c_" + p]))


PV_LAYOUT = {}


def _pv_plan():
    if PV_LAYOUT:
        return
    col = 0

    def add(name, n):
        nonlocal col
        PV_LAYOUT[name] = (col, n // 128)
        col += n // 128

    for l in range(2):
        for nm in ("ffn1_pre_g", "ffn1_post_g", "ffn2_pre_g", "ffn2_post_g", "mix_pre_g", "mix_post_g"):
            add(f"{nm}{l}", D)
    add("lru_b_in", 2 * D)
    for k in range(4):
        add(f"lru_conv_w{k}", D)
    for nm in ("lru_conv_b", "lru_b_r", "lru_b_i", "lru_lambda", "lru_b_out", "kv_norm_g"):
        add(nm, D)
    PV_LAYOUT["_ncol"] = (col, 0)


def _fm(v):
    v = np.asarray(v, dtype=np.float32).reshape(-1, 128)
    return np.ascontiguousarray(v.T)


def pack_pv(inp):
    _pv_plan()
    ncol = PV_LAYOUT["_ncol"][0]
    pv = np.zeros((128, ncol), np.float32)

    def put(name, v):
        c0, n = PV_LAYOUT[name]
        pv[:, c0:c0 + n] = _fm(v)

    for l in range(2):
        for nm in ("ffn1_pre_g", "ffn1_post_g", "ffn2_pre_g", "ffn2_post_g", "mix_pre_g", "mix_post_g"):
            put(f"{nm}{l}", inp[nm][l])
    put("lru_b_in", inp["lru_b_in"][0])
    for k in range(4):
        put(f"lru_conv_w{k}", inp["lru_conv_w"][0, k])
    for nm in ("lru_conv_b", "lru_b_r", "lru_b_i", "lru_lambda", "lru_b_out"):
        put(nm, inp[nm][0])
    put("kv_norm_g", inp["kv_norm_g"])
    return pv


def tile_w_out_chunks(w):
    K, N = w.shape
    a = np.asarray(w, np.float32).reshape(K // 128, 128, N // 128, 128)
    return np.ascontiguousarray(a.transpose(2, 1, 0, 3))


def tile_w_rows(w):
    K, N = w.shape
    return np.ascontiguousarray(np.asarray(w, np.float32).reshape(K // 128, 128, N))


def pack_weights(inp):
    out = {}
    for l in range(2):
        for which in ("ffn1", "ffn2"):
            g = tile_w_out_chunks(inp[f"{which}_w_gate"][l])
            u = tile_w_out_chunks(inp[f"{which}_w_up"][l])
            out[f"{which}_wgu{l}"] = np.ascontiguousarray(np.stack([g, u], axis=2))
            out[f"{which}_wd{l}"] = tile_w_out_chunks(inp[f"{which}_w_down"][l])
    out["lru_win"] = tile_w_out_chunks(inp["lru_w_in"][0])
    out["lru_wout"] = tile_w_out_chunks(inp["lru_w_out"][0])
    wg = np.stack([np.asarray(inp["lru_w_r"][0], np.float32), np.asarray(inp["lru_w_i"][0], np.float32)], axis=1)
    wg = wg.reshape(4, 2, 2, 128, 2, 128)
    out["lru_wg"] = np.ascontiguousarray(wg.transpose(0, 3, 1, 2, 4, 5).reshape(4, 128, 8, 128))
    out["wq"] = tile_w_out_chunks(inp["attn_w_q"][0])
    out["wo"] = tile_w_out_chunks(inp["attn_w_o"][0])
    out["wk"] = tile_w_out_chunks(np.asarray(inp["w_kv"])[:, :D])
    out["wv"] = tile_w_rows(np.asarray(inp["w_kv"])[:, D:])
    out["rbT"] = np.ascontiguousarray(np.asarray(inp["rel_bias"], np.float32).T)
    return out


def t5_bucket_np(d):
    n = np.maximum(d, 0)
    nf = np.maximum(n, 1).astype(np.float32)
    large = 16 + (np.log(nf / np.float32(16.0)) / np.float32(math.log(128 / 16)) * np.float32(16.0)).astype(np.int32)
    large = np.minimum(large, 31)
    return np.where(n < 16, n, large)


FREP_W = 640
FAR_D0 = 129


def make_consts():
    c = {}
    c["ones"] = np.ones((128, 128), np.float32)
    c["ident"] = np.eye(128, dtype=np.float32)
    far = t5_bucket_np(np.arange(FAR_D0, S_LEN))
    assert (far == far[0]).all()
    oh = np.zeros((32, FREP_W + 1), np.float32)
    dd = np.arange(FREP_W)
    d = dd - 255
    bk = t5_bucket_np(d)
    for i in range(FREP_W):
        if d[i] >= 0:
            oh[bk[i], i] = 1.0
    oh[far[0], FREP_W] = 1.0
    c["oh"] = oh
    e8 = np.zeros((8, 8, 128), np.float32)
    for n in range(8):
        e8[n, n, :] = 1.0
    c["e8"] = e8
    return c


class Ctx:
    pass


class WStream:
    def __init__(self, K, st, name, shape, nslot, sem0=0):
        self.K = K
        self.name = name
        self.n = nslot
        self.t = [st.enter_context(SB(K.nc, f"{name}{i}", shape, BF16)) for i in range(nslot)]
        self.b = [Buf(f"{name}{i}") for i in range(nslot)]
        self.s = [K.wsem[sem0 + i] for i in range(nslot)]
        self.plan = []
        self.issued = 0
        self.base = 0

    def extend(self, srcs):
        self.plan.extend(srcs)
        self._pump()

    def _pump(self):
        while self.issued < len(self.plan) and self.issued < self.base + self.n:
            i = self.issued
            sl = i % self.n
            src = self.plan[i]
            dst = self.t[sl]
            self.K.S.dma("pool", self.s[sl], dst[:], src, W=[self.b[sl]])
            self.issued += 1

    def get(self):
        sl = self.base % self.n
        return self.t[sl], self.b[sl]

    def done(self):
        self.base += 1
        self._pump()


def mm(out, lhsT, rhs, start, stop):
    return lambda e: e.matmul(out, lhsT, rhs, start=start, stop=stop)


def rms_rstd(K, ps_stat, ps_b, rstd_t, rstd_b, srt=None, srt_b=None):
    S = K.S
    srt = K.srt if srt is None else srt
    srt_b = K.srt_b if srt_b is None else srt_b
    S.op("act", lambda e: e.activation(out=srt[:], in_=ps_stat[:], func=AF.Sqrt, scale=1.0 / D, bias=K.eps_ap),
         R=[ps_b], W=[srt_b])
    S.op("dve", lambda e: e.reciprocal(out=rstd_t, in_=srt[:]), R=[srt_b], W=[rstd_b])


def prenorm(K, t5, gcol, xn_t, xn_b, rstd_t, rstd_b, ps, ps_b, alt=None):
    S = K.S
    t0 = t5 * 512
    sq, sq_b, srt, srt_b = (K.sq, K.sq_b, K.srt, K.srt_b) if alt is None else alt
    xbs = [K.x_b[t5][c] for c in range(KC)]
    S.op("act", lambda e: e.activation(out=sq[:], in_=K.x[:, :, t0:t0 + 512], func=AF.Square),
         R=xbs, W=[sq_b])
    S.group("pe", [mm(ps[:], K.ones[:], sq[:, c, :], c == 0, c == KC - 1) for c in range(KC)],
            R=[sq_b], W=[ps_b])
    rms_rstd(K, ps, ps_b, rstd_t, rstd_b, srt, srt_b)
    for c in range(KC):
        S.op("dve", lambda e, c=c: e.scalar_tensor_tensor(
            out=xn_t[:, c, :], in0=K.x[:, c, t0:t0 + 512], scalar=K.pv[:, gcol + c:gcol + c + 1],
            in1=rstd_t, op0=ALU.mult, op1=ALU.mult), R=[xbs[c], rstd_b],
            W=[xn_b[c] if isinstance(xn_b, list) else xn_b])


def ffn_stage(K, l, which, TT):
    S, nc = K.S, K.nc
    nsub = TT // 512
    ntt = S_LEN // TT
    gpre = PV_LAYOUT[f"{which}_pre_g{l}"][0]
    gpost = PV_LAYOUT[f"{which}_post_g{l}"][0]
    wgu_src = K.dram[f"{which}_wgu{l}"]
    wd_src = K.dram[f"{which}_wd{l}"]
    P, Pb = K.ps, K.ps_b
    with ExitStack() as st:
        xn = st.enter_context(SB(nc, "f_xn", [128, nsub, KC, 512], BF16))
        h = st.enter_context(SB(nc, "f_h", [128, FC, nsub, 512], BF16))
        y = st.enter_context(SB(nc, "f_y", [128, nsub, KC, 512], F32))
        sg = [st.enter_context(SB(nc, f"f_sg{i}", [128, 512], F32)) for i in range(2)]
        ysq = [st.enter_context(SB(nc, f"f_ysq{i}", [128, 512], BF16)) for i in range(2)]
        rstd = st.enter_context(SB(nc, "f_rstd", [128, nsub, 512], F32))
        rstd2 = st.enter_context(SB(nc, "f_rstd2", [128, nsub, 512], F32))
        rstd2_b = [Buf("rstd2") for _ in range(nsub)]
        xn_b = [Buf("xn") for _ in range(nsub)]
        h_b = [[Buf("h") for _ in range(nsub)] for _ in range(FC)]
        y_b = [[Buf("y") for _ in range(KC)] for _ in range(nsub)]
        sg_b = [Buf("sg") for _ in range(2)]
        ysq_b = [Buf("ysq") for _ in range(2)]
        rstd_b = [Buf("rstd") for _ in range(nsub)]
        K.wgu = WStream(K, st, "wgu", [128, 2, KC, 128], 3, 0)
        K.wd = WStream(K, st, "wd", [128, FC, 128], 2, 3)

        K.wgu.extend([wgu_src[f] for _ in range(ntt) for f in range(FC)])
        K.wd.extend([wd_src[c] for _ in range(ntt) for c in range(KC)])

        def front(tt):
            for sub in range(nsub):
                t5 = tt * nsub + sub
                prenorm(K, t5, gpre, xn[:, sub], xn_b[sub], rstd2[:, sub, :], rstd2_b[sub], P[sub], Pb[sub])

        front(0)
        carry = []
        for tt in range(ntt):
            it = 0
            for f in range(FC):
                wt, wb = K.wgu.get()
                for sub in range(nsub):
                    s2 = it % 2
                    it += 1
                    gp, up = P[2 * s2], P[2 * s2 + 1]
                    S.group("pe", [mm(gp[:], wt[:, 0, k, :], xn[:, sub, k, :], k == 0, k == KC - 1) for k in range(KC)],
                            R=[wb, xn_b[sub]], W=[Pb[2 * s2]])
                    S.group("pe", [mm(up[:], wt[:, 1, k, :], xn[:, sub, k, :], k == 0, k == KC - 1) for k in range(KC)],
                            R=[wb, xn_b[sub]], W=[Pb[2 * s2 + 1]])
                    S.op("act", lambda e, gp=gp, s2=s2: e.activation(out=sg[s2][:], in_=gp[:], func=AF.Silu),
                         R=[Pb[2 * s2]], W=[sg_b[s2]])
                    S.op("dve", lambda e, up=up, s2=s2, f=f, sub=sub: e.tensor_tensor(
                        out=h[:, f, sub, :], in0=up[:], in1=sg[s2][:], op=ALU.mult),
                        R=[Pb[2 * s2 + 1], sg_b[s2]], W=[h_b[f][sub]])
                    if carry and f >= 1:
                        carry.pop(0)()
                K.wgu.done()
            while carry:
                carry.pop(0)()
            pend = None
            it = 0
            for c in range(KC):
                wt, wb = K.wd.get()
                for sub in range(nsub):
                    s2 = it % 2
                    it += 1
                    yp, ypb = P[4 + s2], Pb[4 + s2]
                    S.group("pe", [mm(yp[:], wt[:, f, :], h[:, f, sub, :], f == 0, f == FC - 1) for f in range(FC)],
                            R=[wb] + [h_b[f][sub] for f in range(FC)], W=[ypb])
                    S.op("dve", lambda e, yp=yp, sub=sub, c=c: e.tensor_copy(out=y[:, sub, c, :], in_=yp[:]),
                         R=[ypb], W=[y_b[sub][c]])
                    S.op("act", lambda e, sub=sub, c=c, s2=s2: e.activation(out=ysq[s2][:], in_=y[:, sub, c, :], func=AF.Square),
                         R=[y_b[sub][c]], W=[ysq_b[s2]])
                    if pend is not None:
                        pend()
                    pend = (lambda s2=s2, sub=sub, c=c: S.group(
                        "pe", [mm(P[6 + sub][:], K.ones[:], ysq[s2][:], c == 0, c == KC - 1)],
                        R=[ysq_b[s2]], W=[Pb[6 + sub]]))
                K.wd.done()
                if c == KC // 2 and tt + 1 < ntt:
                    front(tt + 1)
            pend()
            pieces = []
            for sub in range(nsub):
                t5 = tt * nsub + sub
                t0 = t5 * 512
                pieces.append(lambda sub=sub: rms_rstd(K, P[6 + sub], Pb[6 + sub], rstd[:, sub, :], rstd_b[sub]))
                for c in FIN_ORDER:
                    def piece(sub=sub, c=c, t0=t0, t5=t5):
                        S.op("dve", lambda e: e.scalar_tensor_tensor(
                            out=y[:, sub, c, :], in0=y[:, sub, c, :], scalar=K.hp[:, gpost + c:gpost + c + 1],
                            in1=rstd[:, sub, :], op0=ALU.mult, op1=ALU.mult),
                            R=[rstd_b[sub]], W=[y_b[sub][c]])
                        if c >= FIN_POOL_FROM:
                            S.op("pool", lambda e: e.tensor_tensor(
                                out=K.x[:, c, t0:t0 + 512], in0=K.x[:, c, t0:t0 + 512], in1=y[:, sub, c, :], op=ALU.add),
                                R=[y_b[sub][c]], W=[K.x_b[t5][c]])
                        else:
                            S.op("dve", lambda e: e.scalar_tensor_tensor(
                                out=K.x[:, c, t0:t0 + 512], in0=y[:, sub, c, :], scalar=1.0, in1=K.x[:, c, t0:t0 + 512],
                                op0=ALU.mult, op1=ALU.add), R=[y_b[sub][c]], W=[K.x_b[t5][c]])
                    pieces.append(piece)
            if tt + 1 < ntt:
                carry.extend(pieces)
            else:
                for p_ in pieces:
                    p_()
        S.barrier()


class PostNorm:
    def __init__(self, K, st, tag, nbuf=1):
        nc = K.nc
        self.K = K
        self.ys = [st.enter_context(SB(nc, f"{tag}_y{i}", [128, KC, 512], F32)) for i in range(nbuf)]
        self.y_bs = [[Buf("y") for _ in range(KC)] for _ in range(nbuf)]
        self.y = self.ys[0]
        self.ysq = [st.enter_context(SB(nc, f"{tag}_ysq{i}", [128, 512], BF16)) for i in range(2)]
        self.rstd = st.enter_context(SB(nc, tag + "_rstd", [128, 512], F32))
        self.y_b = self.y_bs[0]
        self.ysq_b = [Buf("ysq") for _ in range(2)]
        self.rstd_b = Buf("rstd")
        self.pend = None
        self.it = 0

    def use(self, slot):
        self.y = self.ys[slot]
        self.y_b = self.y_bs[slot]

    def evac(self, c, yp, ypb, bias_ap=None, stat_bank=6):
        K, S = self.K, self.K.S
        s2 = self.it % 2
        self.it += 1
        y, y_b = self.y, self.y_b
        if bias_ap is None:
            S.op("dve", lambda e: e.tensor_copy(out=y[:, c, :], in_=yp[:]), R=[ypb], W=[y_b[c]])
        else:
            S.op("dve", lambda e: e.tensor_scalar(out=y[:, c, :], in0=yp[:], scalar1=bias_ap, scalar2=None,
                                                  op0=ALU.add), R=[ypb], W=[y_b[c]])
        S.op("act", lambda e: e.activation(out=self.ysq[s2][:], in_=y[:, c, :], func=AF.Square),
             R=[y_b[c]], W=[self.ysq_b[s2]])
        if self.pend is not None:
            self.pend()
        P, Pb = K.ps[stat_bank], K.ps_b[stat_bank]
        self.pend = lambda: S.group("pe", [mm(P[:], K.ones[:], self.ysq[s2][:], c == 0, c == KC - 1)],
                                    R=[self.ysq_b[s2]], W=[Pb])

    def flush(self):
        if self.pend is not None:
            self.pend()
            self.pend = None

    def finish_pieces(self, t5, gt, gcol, stat_bank=6, slot=None):
        K, S = self.K, self.K.S
        y, y_b = (self.y, self.y_b) if slot is None else (self.ys[slot], self.y_bs[slot])
        t0 = t5 * 512
        pieces = [lambda: rms_rstd(K, K.ps[stat_bank], K.ps_b[stat_bank], self.rstd[:], self.rstd_b)]
        for c in FIN_ORDER:
            def piece(c=c):
                S.op("dve", lambda e: e.scalar_tensor_tensor(
                    out=y[:, c, :], in0=y[:, c, :], scalar=gt[:, gcol + c:gcol + c + 1],
                    in1=self.rstd[:], op0=ALU.mult, op1=ALU.mult),
                    R=[self.rstd_b], W=[y_b[c]])
                if c >= FIN_POOL_FROM:
                    S.op("pool", lambda e: e.tensor_tensor(
                        out=K.x[:, c, t0:t0 + 512], in0=K.x[:, c, t0:t0 + 512], in1=y[:, c, :], op=ALU.add),
                        R=[y_b[c]], W=[K.x_b[t5][c]])
                else:
                    S.op("dve", lambda e: e.scalar_tensor_tensor(
                        out=K.x[:, c, t0:t0 + 512], in0=y[:, c, :], scalar=1.0, in1=K.x[:, c, t0:t0 + 512],
                        op0=ALU.mult, op1=ALU.add), R=[y_b[c]], W=[K.x_b[t5][c]])
            pieces.append(piece)
        return pieces

    def finish(self, t5, gt, gcol, stat_bank=6, slot=None):
        K, S = self.K, self.K.S
        self.flush()
        y, y_b = (self.y, self.y_b) if slot is None else (self.ys[slot], self.y_bs[slot])
        t0 = t5 * 512
        rms_rstd(K, K.ps[stat_bank], K.ps_b[stat_bank], self.rstd[:], self.rstd_b)
        for c in FIN_ORDER:
            S.op("dve", lambda e, c=c: e.scalar_tensor_tensor(
                out=y[:, c, :], in0=y[:, c, :], scalar=gt[:, gcol + c:gcol + c + 1],
                in1=self.rstd[:], op0=ALU.mult, op1=ALU.mult),
                R=[self.rstd_b], W=[y_b[c]])
            if c >= FIN_POOL_FROM:
                S.op("pool", lambda e, c=c: e.tensor_tensor(
                    out=K.x[:, c, t0:t0 + 512], in0=K.x[:, c, t0:t0 + 512], in1=y[:, c, :], op=ALU.add),
                    R=[y_b[c]], W=[K.x_b[t5][c]])
            else:
                S.op("dve", lambda e, c=c: e.scalar_tensor_tensor(
                    out=K.x[:, c, t0:t0 + 512], in0=y[:, c, :], scalar=1.0, in1=K.x[:, c, t0:t0 + 512],
                    op0=ALU.mult, op1=ALU.add), R=[y_b[c]], W=[K.x_b[t5][c]])


def lru_stage(K):
    S, nc = K.S, K.nc
    P, Pb = K.ps, K.ps_b
    NT = S_LEN // 512
    c_bin = PV_LAYOUT["lru_b_in"][0]
    c_cw = [PV_LAYOUT[f"lru_conv_w{k}"][0] for k in range(4)]
    c_cb = PV_LAYOUT["lru_conv_b"][0]
    c_br = PV_LAYOUT["lru_b_r"][0]
    c_bi = PV_LAYOUT["lru_b_i"][0]
    c_lam = PV_LAYOUT["lru_lambda"][0]
    c_bo = PV_LAYOUT["lru_b_out"][0]
    gpre = PV_LAYOUT["mix_pre_g0"][0]
    gpost = PV_LAYOUT["mix_post_g0"][0]
    win, wout, wg = K.dram["lru_win"], K.dram["lru_wout"], K.dram["lru_wg"]
    pv, hp = K.pv, K.hp
    with ExitStack() as st0, ExitStack() as st:
        T = lambda name, shape, dt=F32: st.enter_context(SB(nc, "l_" + name, shape, dt))
        xn = st0.enter_context(SB(nc, "l_xn", [128, KC, S_LEN], BF16))
        gT = st0.enter_context(SB(nc, "l_gT", [128, KC, S_LEN], BF16))
        K.wp = WStream(K, st0, "wp", [128, KC, 128], 6, 0)
        xbr = [T(f"xbr{i}", [128, 2, 515]) for i in range(2)]
        xc = [T(f"xc{i}", [128, 2, 512]) for i in range(2)]
        xcb = [T(f"xcb{i}", [128, 2, 512], BF16) for i in range(2)]
        gy = [T(f"gy{i}", [128, 2, 512]) for i in range(2)]
        r_t = T("r", [128, 2, 512])
        i_t = T("i", [128, 2, 512])
        a_t = T("a", [128, 2, 512])
        m_t = T("m", [128, 2, 512])
        u_t = T("u", [128, 2, 512])
        h_t = T("h", [128, 2, 512])
        hst = T("hst", [128, KC])
        cl = T("cl", [128, 2 * KC])
        rstd = T("rstd", [128, 512])
        xn_b = [Buf("xn") for _ in range(NT)]
        gT_b = [[Buf("gT") for _ in range(KC)] for _ in range(NT)]
        xbr_b = [[Buf("xbr") for _ in range(2)] for _ in range(2)]
        xc_b = [[Buf("xc") for _ in range(2)] for _ in range(2)]
        xcb_b = [[Buf("xcb") for _ in range(2)] for _ in range(2)]
        gy_b = [[Buf("gy") for _ in range(2)] for _ in range(2)]
        r_b = [Buf("r") for _ in range(2)]
        i_b = [Buf("i") for _ in range(2)]
        a_b = [Buf("a") for _ in range(2)]
        m_b = [Buf("m") for _ in range(2)]
        u_b = [Buf("u") for _ in range(2)]
        h_b = [Buf("h") for _ in range(2)]
        hst_b = [Buf("hst") for _ in range(KC)]
        cl_b, rstd_b = Buf("cl"), Buf("rstd")

        iters = [(hb, tt) for hb in range(4) for tt in range(NT)]
        a_tiles = lambda n: [win[iters[n][0] * 2], win[iters[n][0] * 2 + 1], win[8 + iters[n][0] * 2], win[8 + iters[n][0] * 2 + 1]]
        plan = a_tiles(0)
        for n in range(len(iters)):
            plan.append(wg[iters[n][0]])
            if n + 1 < len(iters):
                plan += a_tiles(n + 1)
        for t5 in range(NT):
            plan += [wout[c] for c in range(KC)]
        K.wp.extend(plan)

        S.op("act", lambda e: e.activation(out=cl[:, 0:KC], in_=pv[:, c_lam:c_lam + KC], func=AF.Exp, scale=-1.0), W=[cl_b])
        S.op("act", lambda e: e.activation(out=cl[:, 0:KC], in_=cl[:, 0:KC], func=AF.Ln, bias=K.one_ap), R=[cl_b], W=[cl_b])
        S.op("dve", lambda e: e.tensor_scalar(out=cl[:, KC:2 * KC], in0=cl[:, 0:KC], scalar1=-8.0, scalar2=None, op0=ALU.mult), R=[cl_b], W=[cl_b])
        S.op("dve", lambda e: e.tensor_scalar(out=cl[:, 0:KC], in0=cl[:, 0:KC], scalar1=-4.0, scalar2=None, op0=ALU.mult), R=[cl_b], W=[cl_b])

        def lru_prenorm(t5):
            prenorm(K, t5, gpre, xn[:, :, t5 * 512:(t5 + 1) * 512], xn_b[t5], rstd[:], rstd_b, P[6], Pb[6])

        lru_prenorm(0)

        iters = [(hb, tt) for hb in range(4) for tt in range(NT)]

        def stage_a_pe(n):
            hb, tt = iters[n]
            t0 = tt * 512
            for br in range(2):
                for jc in range(2):
                    wt, wb = K.wp.get()
                    pp, ppb = P[br * 2 + jc], Pb[br * 2 + jc]
                    S.group("pe", [mm(pp[:], wt[:, k, :], xn[:, k, t0:t0 + 512], k == 0, k == KC - 1) for k in range(KC)],
                            R=[wb, xn_b[tt]], W=[ppb])
                    K.wp.done()

        def stage_a_rest(n):
            hb, tt = iters[n]
            t0 = tt * 512
            d = n % 2
            cur, prv = xbr[d], xbr[1 - d]
            cur_b, prv_b = xbr_b[d], xbr_b[1 - d]
            for br in range(2):
                for jc in range(2):
                    ch = hb * 2 + jc
                    pp, ppb = P[br * 2 + jc], Pb[br * 2 + jc]
                    bcol = c_bin + br * KC + ch
                    if br == 0:
                        S.op("act", lambda e, pp=pp, jc=jc, bcol=bcol: e.activation(
                            out=cur[:, jc, 3:515], in_=pp[:], func=AF.Identity, bias=pv[:, bcol:bcol + 1]),
                            R=[ppb], W=[cur_b[jc]])
                    else:
                        S.op("act", lambda e, pp=pp, jc=jc, bcol=bcol: e.activation(
                            out=gy[d][:, jc, :], in_=pp[:], func=AF.Gelu_apprx_tanh, bias=pv[:, bcol:bcol + 1]),
                            R=[ppb], W=[gy_b[d][jc]])
            for jc in range(2):
                ch = hb * 2 + jc
                if tt == 0:
                    S.op("dve", lambda e, jc=jc: e.memset(cur[:, jc, 0:3], 0.0), W=[cur_b[jc]])
                else:
                    S.op("dve", lambda e, jc=jc: e.tensor_copy(out=cur[:, jc, 0:3], in_=prv[:, jc, 512:515]),
                         R=[prv_b[jc]], W=[cur_b[jc]])
                S.op("dve", lambda e, jc=jc, ch=ch: e.tensor_scalar(
                    out=xc[d][:, jc, :], in0=cur[:, jc, 0:512], scalar1=pv[:, c_cw[0] + ch:c_cw[0] + ch + 1],
                    scalar2=pv[:, c_cb + ch:c_cb + ch + 1], op0=ALU.mult, op1=ALU.add),
                    R=[cur_b[jc]], W=[xc_b[d][jc]])
                for k in range(1, 4):
                    S.op("dve", lambda e, jc=jc, ch=ch, k=k: e.scalar_tensor_tensor(
                        out=xc[d][:, jc, :], in0=cur[:, jc, k:k + 512], scalar=pv[:, c_cw[k] + ch:c_cw[k] + ch + 1],
                        in1=xc[d][:, jc, :], op0=ALU.mult, op1=ALU.add),
                        R=[cur_b[jc]], W=[xc_b[d][jc]])
                S.op("act", lambda e, jc=jc: e.activation(out=xcb[d][:, jc, :], in_=xc[d][:, jc, :], func=AF.Copy),
                     R=[xc_b[d][jc]], W=[xcb_b[d][jc]])

        def stage_b_pe(n):
            hb, tt = iters[n]
            d = n % 2
            wt, wb = K.wp.get()
            for jc in range(2):
                for g in range(2):
                    pp, ppb = P[4 + jc * 2 + g], Pb[4 + jc * 2 + g]
                    S.group("pe", [mm(pp[:], wt[:, g * 4 + ic * 2 + jc, :], xcb[d][:, ic, :], ic == 0, ic == 1) for ic in range(2)],
                            R=[wb, xcb_b[d][0], xcb_b[d][1]], W=[ppb])
            K.wp.done()

        def stage_b_rest(n):
            hb, tt = iters[n]
            t0 = tt * 512
            d = n % 2
            for jc in range(2):
                ch = hb * 2 + jc
                S.op("act", lambda e, jc=jc, ch=ch: e.activation(
                    out=r_t[:, jc, :], in_=P[4 + jc * 2][:], func=AF.Tanh, scale=0.5, bias=hp[:, c_br + ch:c_br + ch + 1]),
                    R=[Pb[4 + jc * 2]], W=[r_b[jc]])
                S.op("act", lambda e, jc=jc, ch=ch: e.activation(
                    out=i_t[:, jc, :], in_=P[4 + jc * 2 + 1][:], func=AF.Tanh, scale=0.5, bias=hp[:, c_bi + ch:c_bi + ch + 1]),
                    R=[Pb[4 + jc * 2 + 1]], W=[i_b[jc]])
            for jc in range(2):
                ch = hb * 2 + jc
                S.op("act", lambda e, jc=jc, ch=ch: e.activation(
                    out=a_t[:, jc, :], in_=r_t[:, jc, :], func=AF.Exp, scale=cl[:, ch:ch + 1], bias=cl[:, ch:ch + 1]),
                    R=[r_b[jc], cl_b], W=[a_b[jc]])
                S.op("act", lambda e, jc=jc, ch=ch: e.activation(
                    out=m_t[:, jc, :], in_=r_t[:, jc, :], func=AF.Exp, scale=cl[:, KC + ch:KC + ch + 1], bias=cl[:, KC + ch:KC + ch + 1]),
                    R=[r_b[jc], cl_b], W=[m_b[jc]])
                S.op("dve", lambda e, jc=jc: e.scalar_tensor_tensor(
                    out=u_t[:, jc, :], in0=i_t[:, jc, :], scalar=1.0, in1=xc[d][:, jc, :], op0=ALU.add, op1=ALU.mult),
                    R=[i_b[jc], xc_b[d][jc]], W=[u_b[jc]])
            for jc in range(2):
                S.op("act", lambda e, jc=jc: e.activation(
                    out=m_t[:, jc, :], in_=m_t[:, jc, :], func=AF.Ln, scale=-1.0, bias=K.one_ap),
                    R=[m_b[jc]], W=[m_b[jc]])
            for jc in range(2):
                S.op("act", lambda e, jc=jc: e.activation(
                    out=m_t[:, jc, :], in_=m_t[:, jc, :], func=AF.Exp, scale=0.5),
                    R=[m_b[jc]], W=[m_b[jc]])

        def stage_b_rest2(n):
            hb, tt = iters[n]
            t0 = tt * 512
            d = n % 2
            for jc in range(2):
                ch = hb * 2 + jc
                S.op("dve", lambda e, jc=jc: e.scalar_tensor_tensor(
                    out=u_t[:, jc, :], in0=u_t[:, jc, :], scalar=0.5, in1=m_t[:, jc, :], op0=ALU.mult, op1=ALU.mult),
                    R=[m_b[jc]], W=[u_b[jc]])
                init = 0.0 if tt == 0 else hst[:, ch:ch + 1]
                S.op("dve", lambda e, jc=jc, init=init: e.tensor_tensor_scan(
                    out=h_t[:, jc, :], data0=a_t[:, jc, :], data1=u_t[:, jc, :], initial=init, op0=ALU.mult, op1=ALU.add),
                    R=[a_b[jc], u_b[jc], hst_b[ch]], W=[h_b[jc]])
                S.op("dve", lambda e, jc=jc, ch=ch: e.tensor_copy(out=hst[:, ch:ch + 1], in_=h_t[:, jc, 511:512]),
                     R=[h_b[jc]], W=[hst_b[ch]])
                S.op("pool", lambda e, jc=jc, ch=ch: e.tensor_tensor(
                    out=gT[:, ch, t0:t0 + 512], in0=h_t[:, jc, :], in1=gy[d][:, jc, :], op=ALU.mult),
                    R=[h_b[jc], gy_b[d][jc]], W=[gT_b[tt][ch]])

        lru_prenorm(1)
        stage_a_pe(0)
        stage_a_rest(0)
        for n in range(len(iters)):
            stage_b_pe(n)
            if n + 1 < len(iters):
                stage_a_pe(n + 1)
            stage_b_rest(n)
            if n + 2 < NT:
                lru_prenorm(n + 2)
            if n + 1 < len(iters):
                stage_a_rest(n + 1)
            stage_b_rest2(n)
        S.barrier()
        st.close()
        pn = PostNorm(K, st, "l_pn", nbuf=2)
        it = 0

        def body(t5, pieces=()):
            nonlocal it
            pieces = list(pieces)
            t0 = t5 * 512
            pn.use(t5 % 2)
            for c in range(KC):
                wt, wb = K.wp.get()
                pp, ppb = P[it % 2], Pb[it % 2]
                it += 1
                S.group("pe", [mm(pp[:], wt[:, k, :], gT[:, k, t0:t0 + 512], k == 0, k == KC - 1) for k in range(KC)],
                        R=[wb] + gT_b[t5], W=[ppb])
                K.wp.done()
                pn.evac(c, pp, ppb, bias_ap=pv[:, c_bo + c:c_bo + c + 1], stat_bank=6 + t5 % 2)
                for _ in range(3):
                    if pieces:
                        pieces.pop(0)()
            pn.flush()
            for p_ in pieces:
                p_()

        body(0)
        for t5 in range(NT):
            pcs = pn.finish_pieces(t5, K.pv, gpost, stat_bank=6 + t5 % 2, slot=t5 % 2)
            if t5 + 1 < NT:
                body(t5 + 1, pcs)
            else:
                for p_ in pcs:
                    p_()
        S.barrier()


def kvnorm_stage(K):
    S, nc = K.S, K.nc
    P, Pb = K.ps, K.ps_b
    scale = float(HD) ** -0.5
    K.cst_stack = ExitStack()
    if True:
        TR = lambda name, shape, dt=F32: K.cst_stack.enter_context(SB(nc, "c_" + name, shape, dt, side="right"))
        M = K.M = TR("M", [128, NHEAD, 512], BF16)
        cb = K.cb = TR("cb", [128, NHEAD])
        identb = K.identb = TR("identb", [128, 128], BF16)
        ident32 = K.ident32 = TR("ident32", [128, 128])
        E8 = K.E8 = K.cst_stack.enter_context(SB(nc, "c_E8", [8, NBLK, 128], BF16, side="right"))
        zt = K.zt = TR("zero", [128, 1])
        M_b, cb_b, cst_b = Buf("M"), Buf("cb"), Buf("cst")
        st1 = ExitStack()
        if True:
            rbT = st1.enter_context(SB(nc, "a_rbT", [32, NHEAD], F32))
            oh = st1.enter_context(SB(nc, "a_oh", [32, FREP_W + 1], F32))
            ones32 = st1.enter_context(SB(nc, "a_ones32", [32, 128], F32))
            rbb = st1.enter_context(SB(nc, "a_rbb", [32, NHEAD, 128], F32))
            frep = st1.enter_context(SB(nc, "a_frep", [128, NHEAD, FREP_W], BF16))
            rb_b, rbb_b, frep_b, fd_b = Buf("rb"), Buf("rbb"), Buf("frep"), Buf("fd")
            S.dma("sp", "ld_c", rbT[:], K.dram["rbT"][:, :], W=[rb_b])
            S.dma("sp", "ld_c", oh[:], K.dram["oh"][:, :], W=[rb_b])
            tk2 = S.dma("sp", "ld_c", ident32[:], K.dram["ident"][:, :], W=[cst_b])
            rb_b.w = tk2
            S.dma("pool", "ld_c2", identb[:], K.dram["ident"][:, :], W=[cst_b])
            tk3 = S.dma("pool", "ld_c2", E8[:], K.dram["e8"][:, :, :], W=[cst_b])
            S.op("dve", lambda e: e.memset(zt[:], 0.0), W=[cst_b])
            S.op("dve", lambda e: e.memset(ones32[:], 1.0), W=[rbb_b])
            for h in range(NHEAD):
                S.op("dve", lambda e, h=h: e.tensor_scalar(out=rbb[:, h, :], in0=ones32[:], scalar1=rbT[:, h:h + 1],
                                                           scalar2=None, op0=ALU.mult), R=[rb_b], W=[rbb_b])
            h2 = FREP_W // 2
            for h in range(NHEAD):
                pa, pab = P[(2 * h) % 4], Pb[(2 * h) % 4]
                pb_, pbb = P[(2 * h + 1) % 4], Pb[(2 * h + 1) % 4]
                S.group("pe", [mm(pa[:, 0:h2], rbb[:, h, :], oh[:, 0:h2], True, True)], R=[rbb_b, rb_b], W=[pab])
                S.group("pe", [mm(pb_[:, 0:h2 + 1], rbb[:, h, :], oh[:, h2:FREP_W + 1], True, True)], R=[rbb_b, rb_b], W=[pbb])
                S.op("act", lambda e, h=h, pa=pa: e.activation(out=frep[:, h, 0:h2], in_=pa[:, 0:h2], func=AF.Copy, scale=1.0 / scale),
                     R=[pab], W=[frep_b])
                S.op("act", lambda e, h=h, pb_=pb_: e.activation(out=frep[:, h, h2:FREP_W], in_=pb_[:, 0:h2], func=AF.Copy, scale=1.0 / scale),
                     R=[pbb], W=[frep_b])
                S.op("act", lambda e, h=h, pb_=pb_: e.activation(out=cb[:, h:h + 1], in_=pb_[:, h2:h2 + 1], func=AF.Copy),
                     R=[pbb], W=[cb_b])
            S.op("dve", lambda e: e.memset(frep[:, :, 0:255], NEG), R=[frep_b], W=[frep_b])
            fd = K.frep_dram
            S.dma("sp", "ld_c", fd.ap(), frep[:].rearrange("p h w -> p (h w)"), R=[frep_b], W=[fd_b])
            skew = bass.AP(tensor=fd, offset=127, ap=[[NHEAD * FREP_W - 1, 128], [FREP_W, NHEAD], [1, 512]])
            with nc.allow_non_contiguous_dma(reason="toeplitz skew"):
                S.dma("sp", "ld_c", M[:], skew, R=[fd_b], W=[M_b])
    K.kv_stack = ExitStack()
    K.xnkv = K.kv_stack.enter_context(SB(nc, "xnkv", [128, KC, S_LEN], BF16, side="right"))
    K.xnkv_b = [Buf("xnkv") for _ in range(S_LEN // 512)]
    g = PV_LAYOUT["kv_norm_g"][0]
    with ExitStack() as st:
        rstd = [st.enter_context(SB(nc, f"kv_rstd{i}", [128, 512], F32)) for i in range(2)]
        rstd_b = [Buf("rstd") for _ in range(2)]
        sq2 = st.enter_context(SB(nc, "kv_sq2", [128, KC, 512], BF16))
        srt2 = st.enter_context(SB(nc, "kv_srt2", [128, 512], F32))
        alts = [None, (sq2, Buf("sq2"), srt2, Buf("srt2"))]
        for t5 in range(S_LEN // 512):
            i = t5 % 2
            prenorm(K, t5, g, K.xnkv[:, :, t5 * 512:(t5 + 1) * 512], K.xnkv_b[t5], rstd[i][:], rstd_b[i],
                    K.ps[6 + i], K.ps_b[6 + i], alt=alts[i])
        S.barrier()
        for e in ("pe", "act", "dve", "pool"):
            S._wait(e, M_b.w)
            S._wait(e, tk3)
            S._wait(e, tk2)
    st1.close()


def attn_stage(K):
    S, nc = K.S, K.nc
    P, Pb = K.ps, K.ps_b
    NT = S_LEN // 512
    pv = K.pv
    gpre = PV_LAYOUT["mix_pre_g1"][0]
    gpost = PV_LAYOUT["mix_post_g1"][0]
    scale = float(HD) ** -0.5
    with ExitStack() as st0:
        T0 = lambda name, shape, dt=F32: st0.enter_context(SB(nc, "a_" + name, shape, dt))
        KT = T0("KT", [128, NHEAD, S_LEN], BF16)
        V = T0("V", [128, S_LEN // 128, D], BF16)
        kmT = T0("kmT", [128, NHEAD, NBLK], BF16)
        K.wp = WStream(K, st0, "wp", [128, KC, 128], 3, 0)
        KT_b = [[Buf("KT") for _ in range(NT)] for _ in range(NHEAD)]
        V_b = [Buf("V") for _ in range(S_LEN // 128)]
        kmT_b = Buf("kmT")
        with ExitStack() as st1:
            wv = st1.enter_context(SB(nc, "a_wv", [128, KC, D], BF16))
            kms = st1.enter_context(SB(nc, "a_kms", [128, NHEAD, NBLK], F32))
            wv_bs, kms_b = [Buf("wv") for _ in range(KC)], Buf("kms")
            K.wp.extend([K.dram["wk"][h] for h in range(NHEAD)])
            tok = None
            for k in range(KC):
                tok = S.dma("pool", K.wsem[6], wv[:, k, :], K.dram["wv"][k], W=[wv_bs[k]])
            for b in wv_bs:
                b.w = tok
            it = 0
            for h in range(NHEAD):
                wt, wb = K.wp.get()
                for t5 in range(NT):
                    t0 = t5 * 512
                    pp, ppb = P[it % 2], Pb[it % 2]
                    it += 1
                    S.group("pe", [mm(pp[:], wt[:, k, :], K.xnkv[:, k, t0:t0 + 512], k == 0, k == KC - 1) for k in range(KC)],
                            R=[wb, K.xnkv_b[t5]], W=[ppb])
                    S.op("act", lambda e, pp=pp, h=h, t0=t0: e.activation(out=KT[:, h, t0:t0 + 512], in_=pp[:], func=AF.Copy),
                         R=[ppb], W=[KT_b[h][t5]])
                    S.op("dve", lambda e, pp=pp, h=h, t5=t5: e.tensor_reduce(
                        out=kms[:, h, 2 * t5:2 * t5 + 2], in_=pp[:].rearrange("p (b j) -> p b j", j=BLK), axis=AX.X, op=ALU.add),
                        R=[ppb], W=[kms_b])
                K.wp.done()
            S.op("dve", lambda e: e.tensor_scalar(out=kmT[:], in0=kms[:], scalar1=1.0 / BLK, scalar2=None, op0=ALU.mult),
                 R=[kms_b], W=[kmT_b])
            it = 0
            for tk in range(S_LEN // 128):
                for half in range(2):
                    pp, ppb = P[2 + it % 2], Pb[2 + it % 2]
                    S.group("pe", [mm(pp[:], K.xnkv[:, k, tk * 128:(tk + 1) * 128], wv[:, k, half * 512:(half + 1) * 512],
                                      k == 0, k == KC - 1) for k in range(KC)],
                            R=wv_bs + [K.xnkv_b[tk // 4]], W=[ppb])
                    if it % 2 == 0:
                        S.op("act", lambda e, pp=pp, tk=tk, half=half: e.activation(
                            out=V[:, tk, half * 512:(half + 1) * 512], in_=pp[:], func=AF.Copy), R=[ppb], W=[V_b[tk]])
                    else:
                        S.op("dve", lambda e, pp=pp, tk=tk, half=half: e.tensor_copy(
                            out=V[:, tk, half * 512:(half + 1) * 512], in_=pp[:]), R=[ppb], W=[V_b[tk]])
                    it += 1
            S.barrier()
        K.kv_stack.close()
        M, cb, identb, ident32, E8, zt = K.M, K.cb, K.identb, K.ident32, K.E8, K.zt
        T1 = T0
        xn = K.sq
        aT_t = T1("aT", [128, NHEAD, 512], BF16)
        QT = T1("QT", [128, NHEAD, 512], BF16)
        aT = aT_t
        AT = st0.enter_context(SB(nc, "a_AT", [8, NHEAD, 512], BF16))
        PT = [T1(f"PT{i}", [128, 256], BF16) for i in range(6)]
        psum_acc = [T1(f"pacc{i}", [128, 256]) for i in range(2)]
        rden = psum_acc
        pacc_b = [Buf("pacc") for _ in range(2)]
        paccbf_b = [Buf("paccbf") for _ in range(2)]
        gs = T1("gs", [128, NHEAD, NBLK])
        mx = T1("mx", [128, NHEAD, 8])
        Am = T1("Am", [128, NHEAD, NBLK])
        pn = PostNorm(K, st0, "a_pn")
        rstd, rstd_b = pn.rstd, pn.rstd_b
        xn_b = K.sq_b
        QT_b = [Buf("QT") for _ in range(NHEAD)]
        aT_b = [Buf("aT") for _ in range(NHEAD)]
        AT_b = [Buf("AT") for _ in range(4)]
        PT_b = [Buf("PT") for _ in range(6)]
        gs_b, mx_b, Am_b = Buf("gs"), Buf("mx"), Buf("Am")
        ipt = 0
        iod = 0
        def front_norm(t5):
            prenorm(K, t5, gpre, xn[:], xn_b, rstd[:], rstd_b, P[7], Pb[7])

        def front_q(t5):
            K.wp.extend([K.dram["wq"][h] for h in range(NHEAD)])
            for h in range(NHEAD):
                wt, wb = K.wp.get()
                pp, ppb = P[6 + h % 2], Pb[6 + h % 2]
                S.group("pe", [mm(pp[:], wt[:, k, :], xn[:, k, :], k == 0, k == KC - 1) for k in range(KC)],
                        R=[wb, xn_b], W=[ppb])
                K.wp.done()
                if h % 2 == 0:
                    S.op("act", lambda e, pp=pp, h=h: e.activation(out=QT[:, h, :], in_=pp[:], func=AF.Copy), R=[ppb], W=[QT_b[h]])
                else:
                    S.op("dve", lambda e, pp=pp, h=h: e.tensor_copy(out=QT[:, h, :], in_=pp[:]), R=[ppb], W=[QT_b[h]])

        def front_gates(t5):
            if 2 * t5 >= 4:
                for s4 in range(4):
                    qb = 2 * t5 + s4 // 2
                    c0 = s4 * 128
                    S.group("pe", [mm(P[6][:, h * NBLK:(h + 1) * NBLK], QT[:, h, c0:c0 + 128], kmT[:, h, :], True, True)
                                   for h in range(NHEAD)], R=QT_b + [kmT_b], W=[Pb[6]])
                    S.op("dve", lambda e: e.memset(gs[:], -1e30), W=[gs_b])
                    S.op("dve", lambda e, qb=qb: e.tensor_copy(
                        out=gs[:, :, 0:qb], in_=P[6][:, 0:NHEAD * NBLK].rearrange("p (h n) -> p h n", n=NBLK)[:, :, 0:qb]),
                        R=[Pb[6]], W=[gs_b])
                    for h in range(NHEAD):
                        S.op("dve", lambda e, h=h: e.max(out=mx[:, h, :], in_=gs[:, h, :]), R=[gs_b], W=[mx_b])
                    for h in range(NHEAD):
                        S.op("dve", lambda e, h=h: e.tensor_scalar(out=Am[:, h, :], in0=gs[:, h, :], scalar1=mx[:, h, 2:3],
                                                                   scalar2=NEG, op0=ALU.is_lt, op1=ALU.mult),
                             R=[gs_b, mx_b], W=[Am_b])
                    for g in range(2):
                        pp, ppb = P[7], Pb[7]
                        S.group("pe", [(lambda e, j=j, g=g, pp=pp: e.transpose(pp[0:8, j * 128:(j + 1) * 128], Am[:, g * 4 + j, :], ident32[:]))
                                       for j in range(4)], R=[Am_b], W=[ppb])
                        S.op("act", lambda e, g=g, c0=c0, pp=pp: e.activation(
                            out=AT[0:8, g * 4:(g + 1) * 4, c0:c0 + 128], in_=pp[0:8, :].rearrange("p (h t) -> p h t", t=128),
                            func=AF.Copy), R=[ppb], W=[AT_b[s4]])

        def inner(t5, hook=None):
            items = []
            for h in range(NHEAD):
                for qb2 in range(2):
                    qb = 2 * t5 + qb2
                    for kt in range(2 * qb + 2):
                        items.append((h, qb2, kt))
            SK = 3
            SB_ = [0, 1, 6, 7]
            st_items = {}

            def emit_qk(idx):
                h, qb2, kt = items[idx]
                qb = 2 * t5 + qb2
                qo = qb2 * 256
                far = kt <= 2 * qb - 2
                bk = SB_[idx % 4]
                ps, psb = P[bk], Pb[bk]
                ops = [(KT[:, h, kt * 128:(kt + 1) * 128], QT[:, h, qo:qo + 256])]
                R = [KT_b[h][kt // 4], QT_b[h]]
                if kt < 2 * qb and qb >= 4:
                    ops.append((E8[0:8, kt // 2, :], AT[0:8, h, qo:qo + 256]))
                    R += [AT_b[2 * qb2], AT_b[2 * qb2 + 1]]
                if not far:
                    off = {2 * qb - 1: 256, 2 * qb: 128, 2 * qb + 1: 0}[kt]
                    ops.append((identb[:], M[:, h, off:off + 256]))
                S.group("pe", [mm(ps[:, 0:256], a, b, i == 0, i == len(ops) - 1) for i, (a, b) in enumerate(ops)],
                        R=R, W=[psb])
                i4 = idx % len(PT)
                bias_ap = cb[:, h:h + 1] if far else zt[:, 0:1]
                S.op("act", lambda e: e.activation(out=PT[i4][:], in_=ps[:, 0:256], func=AF.Exp, scale=scale, bias=bias_ap),
                     R=[psb], W=[PT_b[i4]])

            def emit_pv(idx):
                h, qb2, kt = items[idx]
                qb = 2 * t5 + qb2
                qo = qb2 * 256
                nkt = 2 * qb + 2
                j = (h * 2 + qb2) % 2
                po, pob, pd, pdb = P[2 + j], Pb[2 + j], P[4 + j], Pb[4 + j]
                i4 = idx % len(PT)
                S.group("pe", [mm(po[:, 0:256], V[:, kt, h * 128:(h + 1) * 128], PT[i4][:], kt == 0, kt == nkt - 1)],
                        R=[V_b[kt], PT_b[i4]], W=[pob])
                S.group("pe", [mm(pd[:, 0:256], K.ones[:], PT[i4][:], kt == 0, kt == nkt - 1)],
                        R=[PT_b[i4]], W=[pdb])
                if kt == nkt - 1:
                    S.op("dve", lambda e: e.reciprocal(out=rden[j][:], in_=pd[:, 0:256]), R=[pdb], W=[pacc_b[j]])
                    S.op("dve", lambda e: e.tensor_tensor(out=aT[:, h, qo:qo + 256], in0=po[:, 0:256], in1=rden[j][:], op=ALU.mult),
                         R=[pob, pacc_b[j]], W=[aT_b[h]])

            hook_at = (len(items) * 3) // 4
            for idx in range(len(items) + SK):
                if idx < len(items):
                    emit_qk(idx)
                if idx >= SK:
                    emit_pv(idx - SK)
                if idx == hook_at and hook is not None:
                    hook()

        def back(t5):
            K.wp.extend([K.dram["wo"][c] for c in range(KC)])
            for c in range(KC):
                wt, wb = K.wp.get()
                pp, ppb = P[6 + c % 2], Pb[6 + c % 2]
                S.group("pe", [mm(pp[:], wt[:, k, :], aT[:, k, :], k == 0, k == KC - 1) for k in range(KC)],
                        R=[wb] + aT_b, W=[ppb])
                K.wp.done()
                pn.evac(c, pp, ppb, stat_bank=5)
            pn.flush()

        front_norm(0)
        front_q(0)
        front_gates(0)
        for t5 in range(NT):
            nxt = t5 + 1 < NT
            inner(t5, hook=(lambda t=t5 + 1: front_norm(t)) if nxt else None)
            back(t5)
            if nxt:
                front_q(t5 + 1)
                front_gates(t5 + 1)
            pn.finish(t5, K.pv, gpost, stat_bank=5)
        S.barrier()
    K.cst_stack.close()

def build_program(stages, winfo):
    _pv_plan()
    nc = bass.Bass("TRN2", target_bir_lowering=False)
    K = Ctx()
    K.nc = nc
    K.dram = {}
    for name, shape in winfo.items():
        K.dram[name] = nc.dram_tensor(name, list(shape), F32, kind="ExternalInput").ap()
    xT = nc.dram_tensor("xT", [D, S_LEN], F32, kind="ExternalInput").ap()
    pvd = nc.dram_tensor("pv", [128, PV_LAYOUT["_ncol"][0]], F32, kind="ExternalInput").ap()
    onesd = nc.dram_tensor("ones", [128, 128], F32, kind="ExternalInput").ap()
    outT = nc.dram_tensor("outT", [D, S_LEN], F32, kind="ExternalOutput").ap()
    K.frep_dram = nc.dram_tensor("frep_scratch", [128, NHEAD * FREP_W], BF16, kind="Internal")
    ncol = PV_LAYOUT["_ncol"][0]
    with ExitStack() as stack:
        K.stack = stack
        S = K.S = Sched(nc, stack)
        K.x = stack.enter_context(SB(nc, "x_res", [128, KC, S_LEN], F32))
        K.x_b = [[Buf(f"x{t}_{c}") for c in range(KC)] for t in range(S_LEN // 512)]
        K.pv = stack.enter_context(SB(nc, "pv_sb", [128, ncol], F32))
        K.hp = stack.enter_context(SB(nc, "hp_sb", [128, ncol], F32))
        K.ones = stack.enter_context(SB(nc, "ones_sb", [128, 128], BF16))
        K.epst = stack.enter_context(SB(nc, "eps_sb", [128, 1], F32))
        K.eps_ap = K.epst[:, 0:1]
        K.sq = stack.enter_context(SB(nc, "sq", [128, KC, 512], BF16))
        K.sq_b = Buf("sq")
        K.srt = stack.enter_context(SB(nc, "srt", [128, 512], F32))
        K.srt_b = Buf("srt")
        K.ps = [stack.enter_context(nc.psum_tensor(f"ps{i}", [128, 512], F32)) for i in range(8)]
        K.ps_b = [Buf(f"ps{i}", excl=True) for i in range(8)]
        K.wsem = [S.new_sem(f"w{i}") for i in range(8)]
        K.onet = stack.enter_context(SB(nc, "one_sb", [128, 1], F32))
        K.one_ap = K.onet[:, 0:1]
        pv_b, ones_b, eps_b, hp_b = Buf("pv"), Buf("ones"), Buf("eps"), Buf("hp")
        S.new_sem("ld_x")
        S.new_sem("ld_c")
        S.new_sem("st_x")
        S.new_sem("ld_c2")
        S.dma("sp", "ld_c", K.pv[:], pvd[:, :], W=[pv_b])
        tokc = S.dma("pool", "ld_c2", K.ones[:], onesd[:, :], W=[ones_b])
        S.op("dve", lambda e: e.memset(K.epst[:], EPS), W=[eps_b])
        S.op("dve", lambda e: e.memset(K.onet[:], 1.0), W=[eps_b])
        S.op("dve", lambda e: e.tensor_scalar(out=K.hp[:], in0=K.pv[:], scalar1=0.5, scalar2=None, op0=ALU.mult),
             R=[pv_b], W=[hp_b])
        tok = None
        for c in range(KC):
            tok = S.dma("sp", "ld_x", K.x[:, c, :], xT[c * 128:(c + 1) * 128, :],
                        W=[K.x_b[t][c] for t in range(S_LEN // 512)])
        for t in range(S_LEN // 512):
            for c in range(KC):
                K.x_b[t][c].w = tok
        S.barrier()
        for e in ("pe", "act", "dve", "pool"):
            S._wait(e, tokc)
            S._wait(e, hp_b.w)
            S._wait(e, eps_b.w)
        for stg in stages:
            if stg[0] == "ffn":
                _, l, which, TT = stg
                ffn_stage(K, l, which, TT)
            elif stg[0] == "lru":
                lru_stage(K)
            elif stg[0] == "kvnorm":
                kvnorm_stage(K)
            elif stg[0] == "attn":
                attn_stage(K)
            else:
                raise ValueError(stg)
        tok = None
        for c in range(KC):
            tok = S.dma("sp", "st_x", outT[c * 128:(c + 1) * 128, :], K.x[:, c, :],
                        R=[K.x_b[t][c] for t in range(S_LEN // 512)])
        S._wait("sp", tok)
    return nc


ALL_STAGES = [("ffn", 0, "ffn1", 1024), ("lru",), ("ffn", 0, "ffn2", 1024), ("kvnorm",),
              ("ffn", 1, "ffn1", 512), ("attn",), ("ffn", 1, "ffn2", 1024)]


def stage_inputs(stages, W, C):
    need = []
    for stg in stages:
        if stg[0] == "ffn":
            need += [f"{stg[2]}_wgu{stg[1]}", f"{stg[2]}_wd{stg[1]}"]
        elif stg[0] == "lru":
            need += ["lru_win", "lru_wout", "lru_wg"]
        elif stg[0] == "attn":
            need += ["wq", "wo", "wk", "wv", "rbT"]
    d = {k: W[k] for k in need}
    if any(s[0] == "attn" for s in stages):
        for k in ("oh", "ident", "e8"):
            d[k] = C[k]
    return d


def run_stages(stages, xT_list, pv, W, C):
    use = stage_inputs(stages, W, C)
    nc = build_program(stages, {k: v.shape for k, v in use.items()})
    in_maps = []
    for xT in xT_list:
        m = dict(use)
        m["xT"] = xT
        m["pv"] = pv
        m["ones"] = C["ones"]
        in_maps.append(m)
    res = run_bass_kernel_spmd(nc, in_maps, core_ids=list(range(len(xT_list))))
    return [r["outT"] for r in res.results]


def kernel(**inputs):
    inp = {k: np.asarray(v) for k, v in inputs.items()}
    x = inp["x"].astype(np.float32, copy=False)
    pv = pack_pv(inp)
    W = pack_weights(inp)
    C = make_consts()
    xT = [np.ascontiguousarray(x[b].T) for b in range(NB)]
    outT = run_stages(ALL_STAGES, xT, pv, W, C)
    return np.stack([o.T for o in outT], axis=0).astype(np.float32)
```

```python
from contextlib import ExitStack
import math
import numpy as np
import concourse.bass as bass
import concourse.mybir as mybir
from concourse.bass_utils import run_bass_kernel_spmd

F32 = mybir.dt.float32
BF16 = mybir.dt.bfloat16
AF = mybir.ActivationFunctionType
ALU = mybir.AluOpType
AX = mybir.AxisListType

D = 1024
S_LEN = 2048
NB = 8
DFF = 2816
KC = D // 128
FC = DFF // 128
EPS = 1e-6
NHEAD = 8
HD = 128
BLK = 256
NBLK = S_LEN // BLK
NEG = -30000.0
FIN_POOL_FROM = 5
FIN_ORDER = [5, 6, 7, 0, 1, 2, 3, 4]


_UNIQ = [0]


def SB(nc, name, shape, dt, **kw):
    _UNIQ[0] += 1
    return nc.sbuf_tensor(f"{name}_{_UNIQ[0]}", shape, dt, **kw)


class Buf:
    __slots__ = ("name", "w", "r", "excl")

    def __init__(self, name, excl=False):
        self.name = name
        self.w = None
        self.r = []
        self.excl = excl


class Sched:
    ENGS = ("pe", "act", "dve", "pool", "sp")

    def __init__(self, nc, stack):
        self.nc = nc
        self.stack = stack
        self.eng = dict(pe=nc.tensor, act=nc.scalar, dve=nc.vector, pool=nc.gpsimd, sp=nc.sync)
        self.sem = {}
        self.cnt = {}
        self.seen = {e: {} for e in self.ENGS}
        for e in self.ENGS:
            self.new_sem("c_" + e)

    def new_sem(self, name):
        self.sem[name] = self.stack.enter_context(self.nc.semaphore(name))
        self.cnt[name] = 0
        return name

    def _wait(self, e, tok):
        if tok is None:
            return
        s, v = tok
        if e == "pe" and s == "c_pe":
            return
        if self.seen[e].get(s, 0) >= v:
            return
        self.eng[e].wait_ge(self.sem[s], v)
        self.seen[e][s] = v

    @staticmethod
    def _split(R, W):
        if any(b.excl for b in R):
            W = list(W) + [b for b in R if b.excl]
            R = [b for b in R if not b.excl]
        return R, W

    def _deps(self, e, R, W):
        for b in R:
            self._wait(e, b.w)
        for b in W:
            self._wait(e, b.w)
            for t in b.r:
                self._wait(e, t)

    def _commit(self, tok, R, W):
        for b in W:
            b.w = tok
            b.r = []
        for b in R:
            b.r.append(tok)
            if len(b.r) > 12:
                best = {}
                for (s, v) in b.r:
                    if best.get(s, 0) < v:
                        best[s] = v
                b.r = list(best.items())

    def op(self, e, fn, R=(), W=()):
        R, W = self._split(R, W)
        self._deps(e, R, W)
        ins = fn(self.eng[e])
        s = "c_" + e
        self.cnt[s] += 1
        ins.then_inc(self.sem[s], 1)
        tok = (s, self.cnt[s])
        self._commit(tok, R, W)
        return tok

    def group(self, e, fns, R=(), W=()):
        R, W = self._split(R, W)
        self._deps(e, R, W)
        ins = None
        for fn in fns:
            ins = fn(self.eng[e])
        s = "c_" + e
        self.cnt[s] += 1
        ins.then_inc(self.sem[s], 1)
        tok = (s, self.cnt[s])
        self._commit(tok, R, W)
        return tok

    def dma(self, e, sem, out, in_, R=(), W=(), n=1):
        self._deps(e, R, W)
        self.eng[e].dma_start(out=out, in_=in_).then_inc(self.sem[sem], 16)
        self.cnt[sem] += 16
        tok = (sem, self.cnt[sem])
        self._commit(tok, R, W)
        return tok

    def barrier(self):
        for e in self.ENGS:
            for p in self.ENGS:
                if self.cnt["c_" + p] > 0:
                    self._wait(e, ("c_" + p, self.cnt["c_" + p]))


PV_LAYOUT = {}


def _pv_plan():
    if PV_LAYOUT:
        return
    col = 0

    def add(name, n):
        nonlocal col
        PV_LAYOUT[name] = (col, n // 128)
        col += n // 128

    for l in range(2):
        for nm in ("ffn1_pre_g", "ffn1_post_g", "ffn2_pre_g", "ffn2_post_g", "mix_pre_g", "mix_post_g"):
            add(f"{nm}{l}", D)
    add("lru_b_in", 2 * D)
    for k in range(4):
        add(f"lru_conv_w{k}", D)
    for nm in ("lru_conv_b", "lru_b_r", "lru_b_i", "lru_lambda", "lru_b_out", "kv_norm_g"):
        add(nm, D)
    PV_LAYOUT["_ncol"] = (col, 0)


def _fm(v):
    v = np.asarray(v, dtype=np.float32).reshape(-1, 128)
    return np.ascontiguousarray(v.T)


def pack_pv(inp):
    _pv_plan()
    ncol = PV_LAYOUT["_ncol"][0]
    pv = np.zeros((128, ncol), np.float32)

    def put(name, v):
        c0, n = PV_LAYOUT[name]
        pv[:, c0:c0 + n] = _fm(v)

    for l in range(2):
        for nm in ("ffn1_pre_g", "ffn1_post_g", "ffn2_pre_g", "ffn2_post_g", "mix_pre_g", "mix_post_g"):
            put(f"{nm}{l}", inp[nm][l])
    put("lru_b_in", inp["lru_b_in"][0])
    for k in range(4):
        put(f"lru_conv_w{k}", inp["lru_conv_w"][0, k])
    for nm in ("lru_conv_b", "lru_b_r", "lru_b_i", "lru_lambda", "lru_b_out"):
        put(nm, inp[nm][0])
    put("kv_norm_g", inp["kv_norm_g"])
    return pv


def tile_w_out_chunks(w):
    K, N = w.shape
    a = np.asarray(w, np.float32).reshape(K // 128, 128, N // 128, 128)
    return np.ascontiguousarray(a.transpose(2, 1, 0, 3))


def tile_w_rows(w):
    K, N = w.shape
    return np.ascontiguousarray(np.asarray(w, np.float32).reshape(K // 128, 128, N))


def pack_weights(inp):
    out = {}
    for l in range(2):
        for which in ("ffn1", "ffn2"):
            g = tile_w_out_chunks(inp[f"{which}_w_gate"][l])
            u = tile_w_out_chunks(inp[f"{which}_w_up"][l])
            out[f"{which}_wgu{l}"] = np.ascontiguousarray(np.stack([g, u], axis=2))
            out[f"{which}_wd{l}"] = tile_w_out_chunks(inp[f"{which}_w_down"][l])
    out["lru_win"] = tile_w_out_chunks(inp["lru_w_in"][0])
    out["lru_wout"] = tile_w_out_chunks(inp["lru_w_out"][0])
    wg = np.stack([np.asarray(inp["lru_w_r"][0], np.float32), np.asarray(inp["lru_w_i"][0], np.float32)], axis=1)
    wg = wg.reshape(4, 2, 2, 128, 2, 128)
    out["lru_wg"] = np.ascontiguousarray(wg.transpose(0, 3, 1, 2, 4, 5).reshape(4, 128, 8, 128))
    out["wq"] = tile_w_out_chunks(inp["attn_w_q"][0])
    out["wo"] = tile_w_out_chunks(inp["attn_w_o"][0])
    out["wk"] = tile_w_out_chunks(np.asarray(inp["w_kv"])[:, :D])
    out["wv"] = tile_w_rows(np.asarray(inp["w_kv"])[:, D:])
    out["rbT"] = np.ascontiguousarray(np.asarray(inp["rel_bias"], np.float32).T)
    return out


def t5_bucket_np(d):
    n = np.maximum(d, 0)
    nf = np.maximum(n, 1).astype(np.float32)
    large = 16 + (np.log(nf / np.float32(16.0)) / np.float32(math.log(128 / 16)) * np.float32(16.0)).astype(np.int32)
    large = np.minimum(large, 31)
    return np.where(n < 16, n, large)


FREP_W = 640
FAR_D0 = 129


def make_consts():
    c = {}
    c["ones"] = np.ones((128, 128), np.float32)
    c["ident"] = np.eye(128, dtype=np.float32)
    far = t5_bucket_np(np.arange(FAR_D0, S_LEN))
    assert (far == far[0]).all()
    oh = np.zeros((32, FREP_W + 1), np.float32)
    dd = np.arange(FREP_W)
    d = dd - 255
    bk = t5_bucket_np(d)
    for i in range(FREP_W):
        if d[i] >= 0:
            oh[bk[i], i] = 1.0
    oh[far[0], FREP_W] = 1.0
    c["oh"] = oh
    e8 = np.zeros((8, 8, 128), np.float32)
    for n in range(8):
        e8[n, n, :] = 1.0
    c["e8"] = e8
    return c


class Ctx:
    pass


class WStream:
    def __init__(self, K, st, name, shape, nslot, sem0=0):
        self.K = K
        self.name = name
        self.n = nslot
        self.t = [st.enter_context(SB(K.nc, f"{name}{i}", shape, BF16)) for i in range(nslot)]
        self.b = [Buf(f"{name}{i}") for i in range(nslot)]
        self.s = [K.wsem[sem0 + i] for i in range(nslot)]
        self.plan = []
        self.issued = 0
        self.base = 0

    def extend(self, srcs):
        self.plan.extend(srcs)
        self._pump()

    def _pump(self):
        while self.issued < len(self.plan) and self.issued < self.base + self.n:
            i = self.issued
            sl = i % self.n
            src = self.plan[i]
            dst = self.t[sl]
            self.K.S.dma("pool", self.s[sl], dst[:], src, W=[self.b[sl]])
            self.issued += 1

    def get(self):
        sl = self.base % self.n
        return self.t[sl], self.b[sl]

    def done(self):
        self.base += 1
        self._pump()


def mm(out, lhsT, rhs, start, stop):
    return lambda e: e.matmul(out, lhsT, rhs, start=start, stop=stop)


def rms_rstd(K, ps_stat, ps_b, rstd_t, rstd_b, srt=None, srt_b=None):
    S = K.S
    srt = K.srt if srt is None else srt
    srt_b = K.srt_b if srt_b is None else srt_b
    S.op("act", lambda e: e.activation(out=srt[:], in_=ps_stat[:], func=AF.Sqrt, scale=1.0 / D, bias=K.eps_ap),
         R=[ps_b], W=[srt_b])
    S.op("dve", lambda e: e.reciprocal(out=rstd_t, in_=srt[:]), R=[srt_b], W=[rstd_b])


def prenorm(K, t5, gcol, xn_t, xn_b, rstd_t, rstd_b, ps, ps_b, alt=None):
    S = K.S
    t0 = t5 * 512
    sq, sq_b, srt, srt_b = (K.sq, K.sq_b, K.srt, K.srt_b) if alt is None else alt
    xbs = [K.x_b[t5][c] for c in range(KC)]
    S.op("act", lambda e: e.activation(out=sq[:], in_=K.x[:, :, t0:t0 + 512], func=AF.Square),
         R=xbs, W=[sq_b])
    S.group("pe", [mm(ps[:], K.ones[:], sq[:, c, :], c == 0, c == KC - 1) for c in range(KC)],
            R=[sq_b], W=[ps_b])
    rms_rstd(K, ps, ps_b, rstd_t, rstd_b, srt, srt_b)
    for c in range(KC):
        S.op("dve", lambda e, c=c: e.scalar_tensor_tensor(
            out=xn_t[:, c, :], in0=K.x[:, c, t0:t0 + 512], scalar=K.pv[:, gcol + c:gcol + c + 1],
            in1=rstd_t, op0=ALU.mult, op1=ALU.mult), R=[xbs[c], rstd_b],
            W=[xn_b[c] if isinstance(xn_b, list) else xn_b])


def ffn_stage(K, l, which, TT):
    S, nc = K.S, K.nc
    nsub = TT // 512
    ntt = S_LEN // TT
    gpre = PV_LAYOUT[f"{which}_pre_g{l}"][0]
    gpost = PV_LAYOUT[f"{which}_post_g{l}"][0]
    wgu_src = K.dram[f"{which}_wgu{l}"]
    wd_src = K.dram[f"{which}_wd{l}"]
    P, Pb = K.ps, K.ps_b
    with ExitStack() as st:
        xn = st.enter_context(SB(nc, "f_xn", [128, nsub, KC, 512], BF16))
        h = st.enter_context(SB(nc, "f_h", [128, FC, nsub, 512], BF16))
        y = st.enter_context(SB(nc, "f_y", [128, nsub, KC, 512], F32))
        sg = [st.enter_context(SB(nc, f"f_sg{i}", [128, 512], F32)) for i in range(2)]
        ysq = [st.enter_context(SB(nc, f"f_ysq{i}", [128, 512], BF16)) for i in range(2)]
        rstd = st.enter_context(SB(nc, "f_rstd", [128, nsub, 512], F32))
        rstd2 = st.enter_context(SB(nc, "f_rstd2", [128, nsub, 512], F32))
        rstd2_b = [Buf("rstd2") for _ in range(nsub)]
        xn_b = [Buf("xn") for _ in range(nsub)]
        h_b = [[Buf("h") for _ in range(nsub)] for _ in range(FC)]
        y_b = [[Buf("y") for _ in range(KC)] for _ in range(nsub)]
        sg_b = [Buf("sg") for _ in range(2)]
        ysq_b = [Buf("ysq") for _ in range(2)]
        rstd_b = [Buf("rstd") for _ in range(nsub)]
        K.wgu = WStream(K, st, "wgu", [128, 2, KC, 128], 3, 0)
        K.wd = WStream(K, st, "wd", [128, FC, 128], 2, 3)

        K.wgu.extend([wgu_src[f] for _ in range(ntt) for f in range(FC)])
        K.wd.extend([wd_src[c] for _ in range(ntt) for c in range(KC)])

        def front(tt, subs=None):
            for sub in (range(nsub) if subs is None else subs):
                t5 = tt * nsub + sub
                prenorm(K, t5, gpre, xn[:, sub], xn_b[sub], rstd2[:, sub, :], rstd2_b[sub], P[sub], Pb[sub])

        front(0)
        carry = []
        for tt in range(ntt):
            it = 0
            for f in range(FC):
                wt, wb = K.wgu.get()
                for sub in range(nsub):
                    s2 = it % 2
                    it += 1
                    gp, up = P[2 * s2], P[2 * s2 + 1]
                    S.group("pe", [mm(gp[:], wt[:, 0, k, :], xn[:, sub, k, :], k == 0, k == KC - 1) for k in range(KC)],
                            R=[wb, xn_b[sub]], W=[Pb[2 * s2]])
                    S.group("pe", [mm(up[:], wt[:, 1, k, :], xn[:, sub, k, :], k == 0, k == KC - 1) for k in range(KC)],
                            R=[wb, xn_b[sub]], W=[Pb[2 * s2 + 1]])
                    S.op("act", lambda e, gp=gp, s2=s2: e.activation(out=sg[s2][:], in_=gp[:], func=AF.Silu),
                         R=[Pb[2 * s2]], W=[sg_b[s2]])
                    S.op("dve", lambda e, up=up, s2=s2, f=f, sub=sub: e.tensor_tensor(
                        out=h[:, f, sub, :], in0=up[:], in1=sg[s2][:], op=ALU.mult),
                        R=[Pb[2 * s2 + 1], sg_b[s2]], W=[h_b[f][sub]])
                    if carry and f >= 1:
                        carry.pop(0)()
                K.wgu.done()
            while carry:
                carry.pop(0)()
            pend = None
            it = 0
            for c in range(KC):
                wt, wb = K.wd.get()
                for sub in range(nsub):
                    s2 = it % 2
                    it += 1
                    yp, ypb = P[4 + s2], Pb[4 + s2]
                    S.group("pe", [mm(yp[:], wt[:, f, :], h[:, f, sub, :], f == 0, f == FC - 1) for f in range(FC)],
                            R=[wb] + [h_b[f][sub] for f in range(FC)], W=[ypb])
                    S.op("dve", lambda e, yp=yp, sub=sub, c=c: e.tensor_copy(out=y[:, sub, c, :], in_=yp[:]),
                         R=[ypb], W=[y_b[sub][c]])
                    S.op("act", lambda e, sub=sub, c=c, s2=s2: e.activation(out=ysq[s2][:], in_=y[:, sub, c, :], func=AF.Square),
                         R=[y_b[sub][c]], W=[ysq_b[s2]])
                    if pend is not None:
                        pend()
                    pend = (lambda s2=s2, sub=sub, c=c: S.group(
                        "pe", [mm(P[6 + sub][:], K.ones[:], ysq[s2][:], c == 0, c == KC - 1)],
                        R=[ysq_b[s2]], W=[Pb[6 + sub]]))
                K.wd.done()
                if tt + 1 < ntt:
                    if nsub == 1 and c == KC // 2:
                        front(tt + 1)
                    elif nsub == 2 and c in (2, 5):
                        front(tt + 1, [0 if c == 2 else 1])
            pend()
            pieces = []
            for sub in range(nsub):
                t5 = tt * nsub + sub
                t0 = t5 * 512
                pieces.append(lambda sub=sub: rms_rstd(K, P[6 + sub], Pb[6 + sub], rstd[:, sub, :], rstd_b[sub]))
                for c in FIN_ORDER:
                    def piece(sub=sub, c=c, t0=t0, t5=t5):
                        S.op("dve", lambda e: e.scalar_tensor_tensor(
                            out=y[:, sub, c, :], in0=y[:, sub, c, :], scalar=K.hp[:, gpost + c:gpost + c + 1],
                            in1=rstd[:, sub, :], op0=ALU.mult, op1=ALU.mult),
                            R=[rstd_b[sub]], W=[y_b[sub][c]])
                        if c >= FIN_POOL_FROM:
                            S.op("pool", lambda e: e.tensor_tensor(
                                out=K.x[:, c, t0:t0 + 512], in0=K.x[:, c, t0:t0 + 512], in1=y[:, sub, c, :], op=ALU.add),
                                R=[y_b[sub][c]], W=[K.x_b[t5][c]])
                        else:
                            S.op("dve", lambda e: e.scalar_tensor_tensor(
                                out=K.x[:, c, t0:t0 + 512], in0=y[:, sub, c, :], scalar=1.0, in1=K.x[:, c, t0:t0 + 512],
                                op0=ALU.mult, op1=ALU.add), R=[y_b[sub][c]], W=[K.x_b[t5][c]])
                    pieces.append(piece)
            if tt + 1 < ntt:
                carry.extend(pieces)
            else:
                for p_ in pieces:
                    p_()
        S.barrier()


class PostNorm:
    def __init__(self, K, st, tag, nbuf=1):
        nc = K.nc
        self.K = K
        self.ys = [st.enter_context(SB(nc, f"{tag}_y{i}", [128, KC, 512], F32)) for i in range(nbuf)]
        self.y_bs = [[Buf("y") for _ in range(KC)] for _ in range(nbuf)]
        self.y = self.ys[0]
        self.ysq = [st.enter_context(SB(nc, f"{tag}_ysq{i}", [128, 512], BF16)) for i in range(2)]
        self.rstd = st.enter_context(SB(nc, tag + "_rstd", [128, 512], F32))
        self.y_b = self.y_bs[0]
        self.ysq_b = [Buf("ysq") for _ in range(2)]
        self.rstd_b = Buf("rstd")
        self.pend = None
        self.it = 0

    def use(self, slot):
        self.y = self.ys[slot]
        self.y_b = self.y_bs[slot]

    def evac(self, c, yp, ypb, bias_ap=None, stat_bank=6):
        K, S = self.K, self.K.S
        s2 = self.it % 2
        self.it += 1
        y, y_b = self.y, self.y_b
        if bias_ap is None:
            S.op("dve", lambda e: e.tensor_copy(out=y[:, c, :], in_=yp[:]), R=[ypb], W=[y_b[c]])
        else:
            S.op("dve", lambda e: e.tensor_scalar(out=y[:, c, :], in0=yp[:], scalar1=bias_ap, scalar2=None,
                                                  op0=ALU.add), R=[ypb], W=[y_b[c]])
        S.op("act", lambda e: e.activation(out=self.ysq[s2][:], in_=y[:, c, :], func=AF.Square),
             R=[y_b[c]], W=[self.ysq_b[s2]])
        if self.pend is not None:
            self.pend()
        P, Pb = K.ps[stat_bank], K.ps_b[stat_bank]
        self.pend = lambda: S.group("pe", [mm(P[:], K.ones[:], self.ysq[s2][:], c == 0, c == KC - 1)],
                                    R=[self.ysq_b[s2]], W=[Pb])

    def flush(self):
        if self.pend is not None:
            self.pend()
            self.pend = None

    def finish_pieces(self, t5, gt, gcol, stat_bank=6, slot=None):
        K, S = self.K, self.K.S
        y, y_b = (self.y, self.y_b) if slot is None else (self.ys[slot], self.y_bs[slot])
        t0 = t5 * 512
        pieces = [lambda: rms_rstd(K, K.ps[stat_bank], K.ps_b[stat_bank], self.rstd[:], self.rstd_b)]
        for c in FIN_ORDER:
            def piece(c=c):
                S.op("dve", lambda e: e.scalar_tensor_tensor(
                    out=y[:, c, :], in0=y[:, c, :], scalar=gt[:, gcol + c:gcol + c + 1],
                    in1=self.rstd[:], op0=ALU.mult, op1=ALU.mult),
                    R=[self.rstd_b], W=[y_b[c]])
                if c >= FIN_POOL_FROM:
                    S.op("pool", lambda e: e.tensor_tensor(
                        out=K.x[:, c, t0:t0 + 512], in0=K.x[:, c, t0:t0 + 512], in1=y[:, c, :], op=ALU.add),
                        R=[y_b[c]], W=[K.x_b[t5][c]])
                else:
                    S.op("dve", lambda e: e.scalar_tensor_tensor(
                        out=K.x[:, c, t0:t0 + 512], in0=y[:, c, :], scalar=1.0, in1=K.x[:, c, t0:t0 + 512],
                        op0=ALU.mult, op1=ALU.add), R=[y_b[c]], W=[K.x_b[t5][c]])
            pieces.append(piece)
        return pieces

    def finish(self, t5, gt, gcol, stat_bank=6, slot=None):
        K, S = self.K, self.K.S
        self.flush()
        y, y_b = (self.y, self.y_b) if slot is None else (self.ys[slot], self.y_bs[slot])
        t0 = t5 * 512
        rms_rstd(K, K.ps[stat_bank], K.ps_b[stat_bank], self.rstd[:], self.rstd_b)
        for c in FIN_ORDER:
            S.op("dve", lambda e, c=c: e.scalar_tensor_tensor(
                out=y[:, c, :], in0=y[:, c, :], scalar=gt[:, gcol + c:gcol + c + 1],
                in1=self.rstd[:], op0=ALU.mult, op1=ALU.mult),
                R=[self.rstd_b], W=[y_b[c]])
            if c >= FIN_POOL_FROM:
                S.op("pool", lambda e, c=c: e.tensor_tensor(
                    out=K.x[:, c, t0:t0 + 512], in0=K.x[:, c, t0:t0 + 512], in1=y[:, c, :], op=ALU.add),
                    R=[y_b[c]], W=[K.x_b[t5][c]])
            else:
                S.op("dve", lambda e, c=c: e.scalar_tensor_tensor(
                    out=K.x[:, c, t0:t0 + 512], in0=y[:, c, :], scalar=1.0, in1=K.x[:, c, t0:t0 + 512],
                    op0=ALU.mult, op1=ALU.add), R=[y_b[c]], W=[K.x_b[t5][c]])


def lru_stage(K):
    S, nc = K.S, K.nc
    P, Pb = K.ps, K.ps_b
    NT = S_LEN // 512
    c_bin = PV_LAYOUT["lru_b_in"][0]
    c_cw = [PV_LAYOUT[f"lru_conv_w{k}"][0] for k in range(4)]
    c_cb = PV_LAYOUT["lru_conv_b"][0]
    c_br = PV_LAYOUT["lru_b_r"][0]
    c_bi = PV_LAYOUT["lru_b_i"][0]
    c_lam = PV_LAYOUT["lru_lambda"][0]
    c_bo = PV_LAYOUT["lru_b_out"][0]
    gpre = PV_LAYOUT["mix_pre_g0"][0]
    gpost = PV_LAYOUT["mix_post_g0"][0]
    win, wout, wg = K.dram["lru_win"], K.dram["lru_wout"], K.dram["lru_wg"]
    pv, hp = K.pv, K.hp
    with ExitStack() as st0, ExitStack() as st:
        T = lambda name, shape, dt=F32: st.enter_context(SB(nc, "l_" + name, shape, dt))
        xn = st0.enter_context(SB(nc, "l_xn", [128, KC, S_LEN], BF16))
        gT = st0.enter_context(SB(nc, "l_gT", [128, KC, S_LEN], BF16))
        K.wp = WStream(K, st0, "wp", [128, KC, 128], 6, 0)
        xbr = [T(f"xbr{i}", [128, 2, 515]) for i in range(2)]
        xc = [T(f"xc{i}", [128, 2, 512]) for i in range(2)]
        xcb = [T(f"xcb{i}", [128, 2, 512], BF16) for i in range(2)]
        gy = [T(f"gy{i}", [128, 2, 512]) for i in range(2)]
        r_t = T("r", [128, 2, 512])
        i_t = T("i", [128, 2, 512])
        a_t = T("a", [128, 2, 512])
        m_t = T("m", [128, 2, 512])
        u_t = T("u", [128, 2, 512])
        h_t = T("h", [128, 2, 512])
        hst = T("hst", [128, KC])
        cl = T("cl", [128, 2 * KC])
        rstd = T("rstd", [128, 512])
        xn_b = [Buf("xn") for _ in range(NT)]
        gT_b = [[Buf("gT") for _ in range(KC)] for _ in range(NT)]
        xbr_b = [[Buf("xbr") for _ in range(2)] for _ in range(2)]
        xc_b = [[Buf("xc") for _ in range(2)] for _ in range(2)]
        xcb_b = [[Buf("xcb") for _ in range(2)] for _ in range(2)]
        gy_b = [[Buf("gy") for _ in range(2)] for _ in range(2)]
        r_b = [Buf("r") for _ in range(2)]
        i_b = [Buf("i") for _ in range(2)]
        a_b = [Buf("a") for _ in range(2)]
        m_b = [Buf("m") for _ in range(2)]
        u_b = [Buf("u") for _ in range(2)]
        h_b = [Buf("h") for _ in range(2)]
        hst_b = [Buf("hst") for _ in range(KC)]
        cl_b, rstd_b = Buf("cl"), Buf("rstd")

        iters = [(hb, tt) for hb in range(4) for tt in range(NT)]
        a_tiles = lambda n: [win[iters[n][0] * 2], win[iters[n][0] * 2 + 1], win[8 + iters[n][0] * 2], win[8 + iters[n][0] * 2 + 1]]
        plan = a_tiles(0)
        for n in range(len(iters)):
            plan.append(wg[iters[n][0]])
            if n + 1 < len(iters):
                plan += a_tiles(n + 1)
        for t5 in range(NT):
            plan += [wout[c] for c in range(KC)]
        K.wp.extend(plan)

        S.op("act", lambda e: e.activation(out=cl[:, 0:KC], in_=pv[:, c_lam:c_lam + KC], func=AF.Exp, scale=-1.0), W=[cl_b])
        S.op("act", lambda e: e.activation(out=cl[:, 0:KC], in_=cl[:, 0:KC], func=AF.Ln, bias=K.one_ap), R=[cl_b], W=[cl_b])
        S.op("dve", lambda e: e.tensor_scalar(out=cl[:, KC:2 * KC], in0=cl[:, 0:KC], scalar1=-8.0, scalar2=None, op0=ALU.mult), R=[cl_b], W=[cl_b])
        S.op("dve", lambda e: e.tensor_scalar(out=cl[:, 0:KC], in0=cl[:, 0:KC], scalar1=-4.0, scalar2=None, op0=ALU.mult), R=[cl_b], W=[cl_b])

        def lru_prenorm(t5):
            prenorm(K, t5, gpre, xn[:, :, t5 * 512:(t5 + 1) * 512], xn_b[t5], rstd[:], rstd_b, P[6], Pb[6])

        lru_prenorm(0)

        iters = [(hb, tt) for hb in range(4) for tt in range(NT)]

        def stage_a_pe(n):
            hb, tt = iters[n]
            t0 = tt * 512
            for br in range(2):
                for jc in range(2):
                    wt, wb = K.wp.get()
                    pp, ppb = P[br * 2 + jc], Pb[br * 2 + jc]
                    S.group("pe", [mm(pp[:], wt[:, k, :], xn[:, k, t0:t0 + 512], k == 0, k == KC - 1) for k in range(KC)],
                            R=[wb, xn_b[tt]], W=[ppb])
                    K.wp.done()

        def stage_a_rest(n):
            hb, tt = iters[n]
            t0 = tt * 512
            d = n % 2
            cur, prv = xbr[d], xbr[1 - d]
            cur_b, prv_b = xbr_b[d], xbr_b[1 - d]
            for br in range(2):
                for jc in range(2):
                    ch = hb * 2 + jc
                    pp, ppb = P[br * 2 + jc], Pb[br * 2 + jc]
                    bcol = c_bin + br * KC + ch
                    if br == 0:
                        S.op("act", lambda e, pp=pp, jc=jc, bcol=bcol: e.activation(
                            out=cur[:, jc, 3:515], in_=pp[:], func=AF.Identity, bias=pv[:, bcol:bcol + 1]),
                            R=[ppb], W=[cur_b[jc]])
                    else:
                        S.op("act", lambda e, pp=pp, jc=jc, bcol=bcol: e.activation(
                            out=gy[d][:, jc, :], in_=pp[:], func=AF.Gelu_apprx_tanh, bias=pv[:, bcol:bcol + 1]),
                            R=[ppb], W=[gy_b[d][jc]])
            for jc in range(2):
                ch = hb * 2 + jc
                if tt == 0:
                    S.op("dve", lambda e, jc=jc: e.memset(cur[:, jc, 0:3], 0.0), W=[cur_b[jc]])
                else:
                    S.op("dve", lambda e, jc=jc: e.tensor_copy(out=cur[:, jc, 0:3], in_=prv[:, jc, 512:515]),
                         R=[prv_b[jc]], W=[cur_b[jc]])
                S.op("dve", lambda e, jc=jc, ch=ch: e.tensor_scalar(
                    out=xc[d][:, jc, :], in0=cur[:, jc, 0:512], scalar1=pv[:, c_cw[0] + ch:c_cw[0] + ch + 1],
                    scalar2=pv[:, c_cb + ch:c_cb + ch + 1], op0=ALU.mult, op1=ALU.add),
                    R=[cur_b[jc]], W=[xc_b[d][jc]])
                for k in range(1, 4):
                    S.op("dve", lambda e, jc=jc, ch=ch, k=k: e.scalar_tensor_tensor(
                        out=xc[d][:, jc, :], in0=cur[:, jc, k:k + 512], scalar=pv[:, c_cw[k] + ch:c_cw[k] + ch + 1],
                        in1=xc[d][:, jc, :], op0=ALU.mult, op1=ALU.add),
                        R=[cur_b[jc]], W=[xc_b[d][jc]])
                S.op("act", lambda e, jc=jc: e.activation(out=xcb[d][:, jc, :], in_=xc[d][:, jc, :], func=AF.Copy),
                     R=[xc_b[d][jc]], W=[xcb_b[d][jc]])

        def stage_b_pe(n):
            hb, tt = iters[n]
            d = n % 2
            wt, wb = K.wp.get()
            for jc in range(2):
                for g in range(2):
                    pp, ppb = P[4 + jc * 2 + g], Pb[4 + jc * 2 + g]
                    S.group("pe", [mm(pp[:], wt[:, g * 4 + ic * 2 + jc, :], xcb[d][:, ic, :], ic == 0, ic == 1) for ic in range(2)],
                            R=[wb, xcb_b[d][0], xcb_b[d][1]], W=[ppb])
            K.wp.done()

        def stage_b_rest(n):
            hb, tt = iters[n]
            t0 = tt * 512
            d = n % 2
            for jc in range(2):
                ch = hb * 2 + jc
                S.op("act", lambda e, jc=jc, ch=ch: e.activation(
                    out=r_t[:, jc, :], in_=P[4 + jc * 2][:], func=AF.Tanh, scale=0.5, bias=hp[:, c_br + ch:c_br + ch + 1]),
                    R=[Pb[4 + jc * 2]], W=[r_b[jc]])
                S.op("act", lambda e, jc=jc, ch=ch: e.activation(
                    out=i_t[:, jc, :], in_=P[4 + jc * 2 + 1][:], func=AF.Tanh, scale=0.5, bias=hp[:, c_bi + ch:c_bi + ch + 1]),
                    R=[Pb[4 + jc * 2 + 1]], W=[i_b[jc]])
            for jc in range(2):
                ch = hb * 2 + jc
                S.op("act", lambda e, jc=jc, ch=ch: e.activation(
                    out=a_t[:, jc, :], in_=r_t[:, jc, :], func=AF.Exp, scale=cl[:, ch:ch + 1], bias=cl[:, ch:ch + 1]),
                    R=[r_b[jc], cl_b], W=[a_b[jc]])
                S.op("act", lambda e, jc=jc, ch=ch: e.activation(
                    out=m_t[:, jc, :], in_=r_t[:, jc, :], func=AF.Exp, scale=cl[:, KC + ch:KC + ch + 1], bias=cl[:, KC + ch:KC + ch + 1]),
                    R=[r_b[jc], cl_b], W=[m_b[jc]])
                S.op("dve", lambda e, jc=jc: e.scalar_tensor_tensor(
                    out=u_t[:, jc, :], in0=i_t[:, jc, :], scalar=1.0, in1=xc[d][:, jc, :], op0=ALU.add, op1=ALU.mult),
                    R=[i_b[jc], xc_b[d][jc]], W=[u_b[jc]])
            for jc in range(2):
                S.op("act", lambda e, jc=jc: e.activation(
                    out=m_t[:, jc, :], in_=m_t[:, jc, :], func=AF.Ln, scale=-1.0, bias=K.one_ap),
                    R=[m_b[jc]], W=[m_b[jc]])
            for jc in range(2):
                S.op("act", lambda e, jc=jc: e.activation(
                    out=m_t[:, jc, :], in_=m_t[:, jc, :], func=AF.Exp, scale=0.5),
                    R=[m_b[jc]], W=[m_b[jc]])

        def stage_b_rest2(n):
            hb, tt = iters[n]
            t0 = tt * 512
            d = n % 2
            for jc in range(2):
                ch = hb * 2 + jc
                S.op("dve", lambda e, jc=jc: e.scalar_tensor_tensor(
                    out=u_t[:, jc, :], in0=u_t[:, jc, :], scalar=0.5, in1=m_t[:, jc, :], op0=ALU.mult, op1=ALU.mult),
                    R=[m_b[jc]], W=[u_b[jc]])
                init = 0.0 if tt == 0 else hst[:, ch:ch + 1]
                S.op("dve", lambda e, jc=jc, init=init: e.tensor_tensor_scan(
                    out=h_t[:, jc, :], data0=a_t[:, jc, :], data1=u_t[:, jc, :], initial=init, op0=ALU.mult, op1=ALU.add),
                    R=[a_b[jc], u_b[jc], hst_b[ch]], W=[h_b[jc]])
                S.op("dve", lambda e, jc=jc, ch=ch: e.tensor_copy(out=hst[:, ch:ch + 1], in_=h_t[:, jc, 511:512]),
                     R=[h_b[jc]], W=[hst_b[ch]])
                S.op("pool", lambda e, jc=jc, ch=ch: e.tensor_tensor(
                    out=gT[:, ch, t0:t0 + 512], in0=h_t[:, jc, :], in1=gy[d][:, jc, :], op=ALU.mult),
                    R=[h_b[jc], gy_b[d][jc]], W=[gT_b[tt][ch]])

        lru_prenorm(1)
        stage_a_pe(0)
        stage_a_rest(0)
        for n in range(len(iters)):
            stage_b_pe(n)
            if n + 1 < len(iters):
                stage_a_pe(n + 1)
            stage_b_rest(n)
            if n + 2 < NT:
                lru_prenorm(n + 2)
            if n + 1 < len(iters):
                stage_a_rest(n + 1)
            stage_b_rest2(n)
        S.barrier()
        st.close()
        pn = PostNorm(K, st, "l_pn", nbuf=2)
        it = 0

        def body(t5, pieces=()):
            nonlocal it
            pieces = list(pieces)
            t0 = t5 * 512
            pn.use(t5 % 2)
            for c in range(KC):
                wt, wb = K.wp.get()
                pp, ppb = P[it % 2], Pb[it % 2]
                it += 1
                S.group("pe", [mm(pp[:], wt[:, k, :], gT[:, k, t0:t0 + 512], k == 0, k == KC - 1) for k in range(KC)],
                        R=[wb] + gT_b[t5], W=[ppb])
                K.wp.done()
                pn.evac(c, pp, ppb, bias_ap=pv[:, c_bo + c:c_bo + c + 1], stat_bank=6 + t5 % 2)
                for _ in range(3):
                    if pieces:
                        pieces.pop(0)()
            pn.flush()
            for p_ in pieces:
                p_()

        body(0)
        for t5 in range(NT):
            pcs = pn.finish_pieces(t5, K.pv, gpost, stat_bank=6 + t5 % 2, slot=t5 % 2)
            if t5 + 1 < NT:
                body(t5 + 1, pcs)
            else:
                for p_ in pcs:
                    p_()
        S.barrier()


def kvnorm_stage(K):
    S, nc = K.S, K.nc
    P, Pb = K.ps, K.ps_b
    scale = float(HD) ** -0.5
    K.cst_stack = ExitStack()
    if True:
        TR = lambda name, shape, dt=F32: K.cst_stack.enter_context(SB(nc, "c_" + name, shape, dt, side="right"))
        M = K.M = TR("M", [128, NHEAD, 512], BF16)
        cb = K.cb = TR("cb", [128, NHEAD])
        identb = K.identb = TR("identb", [128, 128], BF16)
        ident32 = K.ident32 = TR("ident32", [128, 128])
        E8 = K.E8 = K.cst_stack.enter_context(SB(nc, "c_E8", [8, NBLK, 128], BF16, side="right"))
        zt = K.zt = TR("zero", [128, 1])
        M_b, cb_b, cst_b = Buf("M"), Buf("cb"), Buf("cst")
        st1 = ExitStack()
        if True:
            rbT = st1.enter_context(SB(nc, "a_rbT", [32, NHEAD], F32))
            oh = st1.enter_context(SB(nc, "a_oh", [32, FREP_W + 1], F32))
            ones32 = st1.enter_context(SB(nc, "a_ones32", [32, 128], F32))
            rbb = st1.enter_context(SB(nc, "a_rbb", [32, NHEAD, 128], F32))
            frep = st1.enter_context(SB(nc, "a_frep", [128, NHEAD, FREP_W], BF16))
            rb_b, rbb_b, frep_b, fd_b = Buf("rb"), Buf("rbb"), Buf("frep"), Buf("fd")
            S.dma("sp", "ld_c", rbT[:], K.dram["rbT"][:, :], W=[rb_b])
            S.dma("sp", "ld_c", oh[:], K.dram["oh"][:, :], W=[rb_b])
            tk2 = S.dma("sp", "ld_c", ident32[:], K.dram["ident"][:, :], W=[cst_b])
            rb_b.w = tk2
            S.dma("pool", "ld_c2", identb[:], K.dram["ident"][:, :], W=[cst_b])
            tk3 = S.dma("pool", "ld_c2", E8[:], K.dram["e8"][:, :, :], W=[cst_b])
            S.op("dve", lambda e: e.memset(zt[:], 0.0), W=[cst_b])
            S.op("dve", lambda e: e.memset(ones32[:], 1.0), W=[rbb_b])
            for h in range(NHEAD):
                S.op("dve", lambda e, h=h: e.tensor_scalar(out=rbb[:, h, :], in0=ones32[:], scalar1=rbT[:, h:h + 1],
                                                           scalar2=None, op0=ALU.mult), R=[rb_b], W=[rbb_b])
            h2 = FREP_W // 2
            for h in range(NHEAD):
                pa, pab = P[(2 * h) % 4], Pb[(2 * h) % 4]
                pb_, pbb = P[(2 * h + 1) % 4], Pb[(2 * h + 1) % 4]
                S.group("pe", [mm(pa[:, 0:h2], rbb[:, h, :], oh[:, 0:h2], True, True)], R=[rbb_b, rb_b], W=[pab])
                S.group("pe", [mm(pb_[:, 0:h2 + 1], rbb[:, h, :], oh[:, h2:FREP_W + 1], True, True)], R=[rbb_b, rb_b], W=[pbb])
                S.op("act", lambda e, h=h, pa=pa: e.activation(out=frep[:, h, 0:h2], in_=pa[:, 0:h2], func=AF.Copy, scale=1.0 / scale),
                     R=[pab], W=[frep_b])
                S.op("act", lambda e, h=h, pb_=pb_: e.activation(out=frep[:, h, h2:FREP_W], in_=pb_[:, 0:h2], func=AF.Copy, scale=1.0 / scale),
                     R=[pbb], W=[frep_b])
                S.op("act", lambda e, h=h, pb_=pb_: e.activation(out=cb[:, h:h + 1], in_=pb_[:, h2:h2 + 1], func=AF.Copy),
                     R=[pbb], W=[cb_b])
            S.op("dve", lambda e: e.memset(frep[:, :, 0:255], NEG), R=[frep_b], W=[frep_b])
            fd = K.frep_dram
            S.dma("sp", "ld_c", fd.ap(), frep[:].rearrange("p h w -> p (h w)"), R=[frep_b], W=[fd_b])
            skew = bass.AP(tensor=fd, offset=127, ap=[[NHEAD * FREP_W - 1, 128], [FREP_W, NHEAD], [1, 512]])
            with nc.allow_non_contiguous_dma(reason="toeplitz skew"):
                S.dma("sp", "ld_c", M[:], skew, R=[fd_b], W=[M_b])
    K.kv_stack = ExitStack()
    K.xnkv = K.kv_stack.enter_context(SB(nc, "xnkv", [128, KC, S_LEN], BF16, side="right"))
    K.xnkv_b = [Buf("xnkv") for _ in range(S_LEN // 512)]
    g = PV_LAYOUT["kv_norm_g"][0]
    with ExitStack() as st:
        rstd = [st.enter_context(SB(nc, f"kv_rstd{i}", [128, 512], F32)) for i in range(2)]
        rstd_b = [Buf("rstd") for _ in range(2)]
        sq2 = st.enter_context(SB(nc, "kv_sq2", [128, KC, 512], BF16))
        srt2 = st.enter_context(SB(nc, "kv_srt2", [128, 512], F32))
        alts = [None, (sq2, Buf("sq2"), srt2, Buf("srt2"))]
        for t5 in range(S_LEN // 512):
            i = t5 % 2
            prenorm(K, t5, g, K.xnkv[:, :, t5 * 512:(t5 + 1) * 512], K.xnkv_b[t5], rstd[i][:], rstd_b[i],
                    K.ps[6 + i], K.ps_b[6 + i], alt=alts[i])
        S.barrier()
        for e in ("pe", "act", "dve", "pool"):
            S._wait(e, M_b.w)
            S._wait(e, tk3)
            S._wait(e, tk2)
    st1.close()


def attn_stage(K):
    S, nc = K.S, K.nc
    P, Pb = K.ps, K.ps_b
    NT = S_LEN // 512
    pv = K.pv
    gpre = PV_LAYOUT["mix_pre_g1"][0]
    gpost = PV_LAYOUT["mix_post_g1"][0]
    scale = float(HD) ** -0.5
    with ExitStack() as st0:
        T0 = lambda name, shape, dt=F32: st0.enter_context(SB(nc, "a_" + name, shape, dt))
        KT = T0("KT", [128, NHEAD, S_LEN], BF16)
        V = T0("V", [128, S_LEN // 128, D], BF16)
        kmT = T0("kmT", [128, NHEAD, NBLK], BF16)
        K.wp = WStream(K, st0, "wp", [128, KC, 128], 3, 0)
        KT_b = [[Buf("KT") for _ in range(NT)] for _ in range(NHEAD)]
        V_b = [Buf("V") for _ in range(S_LEN // 128)]
        kmT_b = Buf("kmT")
        with ExitStack() as st1:
            wv = st1.enter_context(SB(nc, "a_wv", [128, KC, D], BF16))
            kms = st1.enter_context(SB(nc, "a_kms", [128, NHEAD, NBLK], F32))
            wv_bs, kms_b = [Buf("wv") for _ in range(KC)], Buf("kms")
            K.wp.extend([K.dram["wk"][h] for h in range(NHEAD)])
            tok = None
            for k in range(KC):
                tok = S.dma("pool", K.wsem[6], wv[:, k, :], K.dram["wv"][k], W=[wv_bs[k]])
            for b in wv_bs:
                b.w = tok
            it = 0
            for h in range(NHEAD):
                wt, wb = K.wp.get()
                for t5 in range(NT):
                    t0 = t5 * 512
                    pp, ppb = P[it % 2], Pb[it % 2]
                    it += 1
                    S.group("pe", [mm(pp[:], wt[:, k, :], K.xnkv[:, k, t0:t0 + 512], k == 0, k == KC - 1) for k in range(KC)],
                            R=[wb, K.xnkv_b[t5]], W=[ppb])
                    S.op("act", lambda e, pp=pp, h=h, t0=t0: e.activation(out=KT[:, h, t0:t0 + 512], in_=pp[:], func=AF.Copy),
                         R=[ppb], W=[KT_b[h][t5]])
                    S.op("dve", lambda e, pp=pp, h=h, t5=t5: e.tensor_reduce(
                        out=kms[:, h, 2 * t5:2 * t5 + 2], in_=pp[:].rearrange("p (b j) -> p b j", j=BLK), axis=AX.X, op=ALU.add),
                        R=[ppb], W=[kms_b])
                K.wp.done()
            S.op("dve", lambda e: e.tensor_scalar(out=kmT[:], in0=kms[:], scalar1=1.0 / BLK, scalar2=None, op0=ALU.mult),
                 R=[kms_b], W=[kmT_b])
            it = 0
            for tk in range(S_LEN // 128):
                for half in range(2):
                    pp, ppb = P[2 + it % 2], Pb[2 + it % 2]
                    S.group("pe", [mm(pp[:], K.xnkv[:, k, tk * 128:(tk + 1) * 128], wv[:, k, half * 512:(half + 1) * 512],
                                      k == 0, k == KC - 1) for k in range(KC)],
                            R=wv_bs + [K.xnkv_b[tk // 4]], W=[ppb])
                    if it % 2 == 0:
                        S.op("act", lambda e, pp=pp, tk=tk, half=half: e.activation(
                            out=V[:, tk, half * 512:(half + 1) * 512], in_=pp[:], func=AF.Copy), R=[ppb], W=[V_b[tk]])
                    else:
                        S.op("dve", lambda e, pp=pp, tk=tk, half=half: e.tensor_copy(
                            out=V[:, tk, half * 512:(half + 1) * 512], in_=pp[:]), R=[ppb], W=[V_b[tk]])
                    it += 1
            S.barrier()
        K.kv_stack.close()
        M, cb, identb, ident32, E8, zt = K.M, K.cb, K.identb, K.ident32, K.E8, K.zt
        T1 = T0
        xn = K.sq
        aT_t = T1("aT", [128, NHEAD, 512], BF16)
        QT = T1("QT", [128, NHEAD, 512], BF16)
        aT = aT_t
        AT = st0.enter_context(SB(nc, "a_AT", [8, NHEAD, 512], BF16))
        PT = [T1(f"PT{i}", [128, 256], BF16) for i in range(6)]
        psum_acc = [T1(f"pacc{i}", [128, 256]) for i in range(2)]
        rden = psum_acc
        pacc_b = [Buf("pacc") for _ in range(2)]
        paccbf_b = [Buf("paccbf") for _ in range(2)]
        gs = T1("gs", [128, NHEAD, NBLK])
        mx = T1("mx", [128, NHEAD, 8])
        Am = T1("Am", [128, NHEAD, NBLK])
        pn = PostNorm(K, st0, "a_pn")
        rstd, rstd_b = pn.rstd, pn.rstd_b
        xn_b = K.sq_b
        QT_b = [Buf("QT") for _ in range(NHEAD)]
        aT_b = [Buf("aT") for _ in range(NHEAD)]
        AT_b = [Buf("AT") for _ in range(4)]
        PT_b = [Buf("PT") for _ in range(6)]
        gs_b, mx_b, Am_b = Buf("gs"), Buf("mx"), Buf("Am")
        ipt = 0
        iod = 0
        def front_norm(t5):
            prenorm(K, t5, gpre, xn[:], xn_b, rstd[:], rstd_b, P[7], Pb[7])

        def front_q(t5):
            K.wp.extend([K.dram["wq"][h] for h in range(NHEAD)])
            for h in range(NHEAD):
                wt, wb = K.wp.get()
                pp, ppb = P[6 + h % 2], Pb[6 + h % 2]
                S.group("pe", [mm(pp[:], wt[:, k, :], xn[:, k, :], k == 0, k == KC - 1) for k in range(KC)],
                        R=[wb, xn_b], W=[ppb])
                K.wp.done()
                if h % 2 == 0:
                    S.op("act", lambda e, pp=pp, h=h: e.activation(out=QT[:, h, :], in_=pp[:], func=AF.Copy), R=[ppb], W=[QT_b[h]])
                else:
                    S.op("dve", lambda e, pp=pp, h=h: e.tensor_copy(out=QT[:, h, :], in_=pp[:]), R=[ppb], W=[QT_b[h]])

        def front_gates(t5):
            if 2 * t5 >= 4:
                for s4 in range(4):
                    qb = 2 * t5 + s4 // 2
                    c0 = s4 * 128
                    S.group("pe", [mm(P[6][:, h * NBLK:(h + 1) * NBLK], QT[:, h, c0:c0 + 128], kmT[:, h, :], True, True)
                                   for h in range(NHEAD)], R=QT_b + [kmT_b], W=[Pb[6]])
                    S.op("dve", lambda e: e.memset(gs[:], -1e30), W=[gs_b])
                    S.op("dve", lambda e, qb=qb: e.tensor_copy(
                        out=gs[:, :, 0:qb], in_=P[6][:, 0:NHEAD * NBLK].rearrange("p (h n) -> p h n", n=NBLK)[:, :, 0:qb]),
                        R=[Pb[6]], W=[gs_b])
                    for h in range(NHEAD):
                        S.op("dve", lambda e, h=h: e.max(out=mx[:, h, :], in_=gs[:, h, :]), R=[gs_b], W=[mx_b])
                    for h in range(NHEAD):
                        S.op("dve", lambda e, h=h: e.tensor_scalar(out=Am[:, h, :], in0=gs[:, h, :], scalar1=mx[:, h, 2:3],
                                                                   scalar2=NEG, op0=ALU.is_lt, op1=ALU.mult),
                             R=[gs_b, mx_b], W=[Am_b])
                    for g in range(2):
                        pp, ppb = P[7], Pb[7]
                        S.group("pe", [(lambda e, j=j, g=g, pp=pp: e.transpose(pp[0:8, j * 128:(j + 1) * 128], Am[:, g * 4 + j, :], ident32[:]))
                                       for j in range(4)], R=[Am_b], W=[ppb])
                        S.op("act", lambda e, g=g, c0=c0, pp=pp: e.activation(
                            out=AT[0:8, g * 4:(g + 1) * 4, c0:c0 + 128], in_=pp[0:8, :].rearrange("p (h t) -> p h t", t=128),
                            func=AF.Copy), R=[ppb], W=[AT_b[s4]])

        def inner(t5, hook=None):
            items = []
            for h in range(NHEAD):
                for qb2 in range(2):
                    qb = 2 * t5 + qb2
                    for kt in range(2 * qb + 2):
                        items.append((h, qb2, kt))
            SK = 3
            SB_ = [0, 1, 6, 7]
            st_items = {}

            def emit_qk(idx):
                h, qb2, kt = items[idx]
                qb = 2 * t5 + qb2
                qo = qb2 * 256
                far = kt <= 2 * qb - 2
                bk = SB_[idx % 4]
                ps, psb = P[bk], Pb[bk]
                ops = [(KT[:, h, kt * 128:(kt + 1) * 128], QT[:, h, qo:qo + 256])]
                R = [KT_b[h][kt // 4], QT_b[h]]
                if kt < 2 * qb and qb >= 4:
                    ops.append((E8[0:8, kt // 2, :], AT[0:8, h, qo:qo + 256]))
                    R += [AT_b[2 * qb2], AT_b[2 * qb2 + 1]]
                if not far:
                    off = {2 * qb - 1: 256, 2 * qb: 128, 2 * qb + 1: 0}[kt]
                    ops.append((identb[:], M[:, h, off:off + 256]))
                S.group("pe", [mm(ps[:, 0:256], a, b, i == 0, i == len(ops) - 1) for i, (a, b) in enumerate(ops)],
                        R=R, W=[psb])
                i4 = idx % len(PT)
                bias_ap = cb[:, h:h + 1] if far else zt[:, 0:1]
                S.op("act", lambda e: e.activation(out=PT[i4][:], in_=ps[:, 0:256], func=AF.Exp, scale=scale, bias=bias_ap),
                     R=[psb], W=[PT_b[i4]])

            def emit_pv(idx):
                h, qb2, kt = items[idx]
                qb = 2 * t5 + qb2
                qo = qb2 * 256
                nkt = 2 * qb + 2
                j = (h * 2 + qb2) % 2
                po, pob, pd, pdb = P[2 + j], Pb[2 + j], P[4 + j], Pb[4 + j]
                i4 = idx % len(PT)
                S.group("pe", [mm(po[:, 0:256], V[:, kt, h * 128:(h + 1) * 128], PT[i4][:], kt == 0, kt == nkt - 1)],
                        R=[V_b[kt], PT_b[i4]], W=[pob])
                S.group("pe", [mm(pd[:, 0:256], K.ones[:], PT[i4][:], kt == 0, kt == nkt - 1)],
                        R=[PT_b[i4]], W=[pdb])
                if kt == nkt - 1:
                    S.op("dve", lambda e: e.reciprocal(out=rden[j][:], in_=pd[:, 0:256]), R=[pdb], W=[pacc_b[j]])
                    S.op("dve", lambda e: e.tensor_tensor(out=aT[:, h, qo:qo + 256], in0=po[:, 0:256], in1=rden[j][:], op=ALU.mult),
                         R=[pob, pacc_b[j]], W=[aT_b[h]])

            hook_at = (len(items) * 3) // 4
            for idx in range(len(items) + SK):
                if idx < len(items):
                    emit_qk(idx)
                if idx >= SK:
                    emit_pv(idx - SK)
                if idx == hook_at and hook is not None:
                    hook()

        def back(t5):
            K.wp.extend([K.dram["wo"][c] for c in range(KC)])
            for c in range(KC):
                wt, wb = K.wp.get()
                pp, ppb = P[6 + c % 2], Pb[6 + c % 2]
                S.group("pe", [mm(pp[:], wt[:, k, :], aT[:, k, :], k == 0, k == KC - 1) for k in range(KC)],
                        R=[wb] + aT_b, W=[ppb])
                K.wp.done()
                pn.evac(c, pp, ppb, stat_bank=5)
            pn.flush()

        front_norm(0)
        front_q(0)
        front_gates(0)
        for t5 in range(NT):
            nxt = t5 + 1 < NT
            inner(t5, hook=(lambda t=t5 + 1: front_norm(t)) if nxt else None)
            back(t5)
            if nxt:
                front_q(t5 + 1)
                front_gates(t5 + 1)
            pn.finish(t5, K.pv, gpost, stat_bank=5)
        S.barrier()
    K.cst_stack.close()

def build_program(stages, winfo):
    _pv_plan()
    nc = bass.Bass("TRN2", target_bir_lowering=False)
    K = Ctx()
    K.nc = nc
    K.dram = {}
    for name, shape in winfo.items():
        K.dram[name] = nc.dram_tensor(name, list(shape), F32, kind="ExternalInput").ap()
    xT = nc.dram_tensor("xT", [D, S_LEN], F32, kind="ExternalInput").ap()
    pvd = nc.dram_tensor("pv", [128, PV_LAYOUT["_ncol"][0]], F32, kind="ExternalInput").ap()
    onesd = nc.dram_tensor("ones", [128, 128], F32, kind="ExternalInput").ap()
    outT = nc.dram_tensor("outT", [D, S_LEN], F32, kind="ExternalOutput").ap()
    K.frep_dram = nc.dram_tensor("frep_scratch", [128, NHEAD * FREP_W], BF16, kind="Internal")
    ncol = PV_LAYOUT["_ncol"][0]
    with ExitStack() as stack:
        K.stack = stack
        S = K.S = Sched(nc, stack)
        K.x = stack.enter_context(SB(nc, "x_res", [128, KC, S_LEN], F32))
        K.x_b = [[Buf(f"x{t}_{c}") for c in range(KC)] for t in range(S_LEN // 512)]
        K.pv = stack.enter_context(SB(nc, "pv_sb", [128, ncol], F32))
        K.hp = stack.enter_context(SB(nc, "hp_sb", [128, ncol], F32))
        K.ones = stack.enter_context(SB(nc, "ones_sb", [128, 128], BF16))
        K.epst = stack.enter_context(SB(nc, "eps_sb", [128, 1], F32))
        K.eps_ap = K.epst[:, 0:1]
        K.sq = stack.enter_context(SB(nc, "sq", [128, KC, 512], BF16))
        K.sq_b = Buf("sq")
        K.srt = stack.enter_context(SB(nc, "srt", [128, 512], F32))
        K.srt_b = Buf("srt")
        K.ps = [stack.enter_context(nc.psum_tensor(f"ps{i}", [128, 512], F32)) for i in range(8)]
        K.ps_b = [Buf(f"ps{i}", excl=True) for i in range(8)]
        K.wsem = [S.new_sem(f"w{i}") for i in range(8)]
        K.onet = stack.enter_context(SB(nc, "one_sb", [128, 1], F32))
        K.one_ap = K.onet[:, 0:1]
        pv_b, ones_b, eps_b, hp_b = Buf("pv"), Buf("ones"), Buf("eps"), Buf("hp")
        S.new_sem("ld_x")
        S.new_sem("ld_c")
        S.new_sem("st_x")
        S.new_sem("ld_c2")
        S.dma("sp", "ld_c", K.pv[:], pvd[:, :], W=[pv_b])
        tokc = S.dma("pool", "ld_c2", K.ones[:], onesd[:, :], W=[ones_b])
        S.op("dve", lambda e: e.memset(K.epst[:], EPS), W=[eps_b])
        S.op("dve", lambda e: e.memset(K.onet[:], 1.0), W=[eps_b])
        S.op("dve", lambda e: e.tensor_scalar(out=K.hp[:], in0=K.pv[:], scalar1=0.5, scalar2=None, op0=ALU.mult),
             R=[pv_b], W=[hp_b])
        xT3 = xT.rearrange("(c p) t -> p c t", p=128)
        for t in range(S_LEN // 512):
            sem = S.new_sem(f"ld_x{t}")
            with nc.allow_non_contiguous_dma(reason="per-token-tile input load (2 KiB runs)"):
                S.dma("sp", sem, K.x[:, :, t * 512:(t + 1) * 512], xT3[:, :, t * 512:(t + 1) * 512], W=K.x_b[t])
        S.barrier()
        for e in ("pe", "act", "dve", "pool"):
            S._wait(e, tokc)
            S._wait(e, hp_b.w)
            S._wait(e, eps_b.w)
        for stg in stages:
            if stg[0] == "ffn":
                _, l, which, TT = stg
                ffn_stage(K, l, which, TT)
            elif stg[0] == "lru":
                lru_stage(K)
            elif stg[0] == "kvnorm":
                kvnorm_stage(K)
            elif stg[0] == "attn":
                attn_stage(K)
            else:
                raise ValueError(stg)
        outT3 = outT.rearrange("(c p) t -> p c t", p=128)
        toks = []
        for t in range(S_LEN // 512):
            sem = S.new_sem(f"st_x{t}")
            with nc.allow_non_contiguous_dma(reason="per-token-tile output store (2 KiB runs)"):
                toks.append(S.dma("sp", sem, outT3[:, :, t * 512:(t + 1) * 512], K.x[:, :, t * 512:(t + 1) * 512], R=K.x_b[t]))
        for tok in toks:
            S._wait("sp", tok)
    return nc


ALL_STAGES = [("ffn", 0, "ffn1", 1024), ("lru",), ("ffn", 0, "ffn2", 1024), ("kvnorm",),
              ("ffn", 1, "ffn1", 512), ("attn",), ("ffn", 1, "ffn2", 1024)]


def stage_inputs(stages, W, C):
    need = []
    for stg in stages:
        if stg[0] == "ffn":
            need += [f"{stg[2]}_wgu{stg[1]}", f"{stg[2]}_wd{stg[1]}"]
        elif stg[0] == "lru":
            need += ["lru_win", "lru_wout", "lru_wg"]
        elif stg[0] == "attn":
            need += ["wq", "wo", "wk", "wv", "rbT"]
    d = {k: W[k] for k in need}
    if any(s[0] == "attn" for s in stages):
        for k in ("oh", "ident", "e8"):
            d[k] = C[k]
    return d


def run_stages(stages, xT_list, pv, W, C):
    use = stage_inputs(stages, W, C)
    nc = build_program(stages, {k: v.shape for k, v in use.items()})
    in_maps = []
    for xT in xT_list:
        m = dict(use)
        m["xT"] = xT
        m["pv"] = pv
        m["ones"] = C["ones"]
        in_maps.append(m)
    res = run_bass_kernel_spmd(nc, in_maps, core_ids=list(range(len(xT_list))))
    return [r["outT"] for r in res.results]


def kernel(**inputs):
    inp = {k: np.asarray(v) for k, v in inputs.items()}
    x = inp["x"].astype(np.float32, copy=False)
    pv = pack_pv(inp)
    W = pack_weights(inp)
    C = make_consts()
    xT = [np.ascontiguousarray(x[b].T) for b in range(NB)]
    outT = run_stages(ALL_STAGES, xT, pv, W, C)
    return np.stack([o.T for o in outT], axis=0).astype(np.float32)
```

```python
from contextlib import ExitStack
import math
import numpy as np
import concourse.bass as bass
import concourse.mybir as mybir
from concourse.bass_utils import run_bass_kernel_spmd

F32 = mybir.dt.float32
BF16 = mybir.dt.bfloat16
AF = mybir.ActivationFunctionType
ALU = mybir.AluOpType
AX = mybir.AxisListType

D = 1024
S_LEN = 2048
NB = 8
DFF = 2816
KC = D // 128
FC = DFF // 128
EPS = 1e-6
NHEAD = 8
HD = 128
BLK = 256
NBLK = S_LEN // BLK
NEG = -30000.0
FIN_POOL_FROM = 5
FIN_ORDER = [5, 6, 7, 0, 1, 2, 3, 4]


_UNIQ = [0]


def SB(nc, name, shape, dt, **kw):
    _UNIQ[0] += 1
    return nc.sbuf_tensor(f"{name}_{_UNIQ[0]}", shape, dt, **kw)


class Buf:
    __slots__ = ("name", "w", "r", "excl")

    def __init__(self, name, excl=False):
        self.name = name
        self.w = None
        self.r = []
        self.excl = excl


class Sched:
    ENGS = ("pe", "act", "dve", "pool", "sp")

    def __init__(self, nc, stack):
        self.nc = nc
        self.stack = stack
        self.eng = dict(pe=nc.tensor, act=nc.scalar, dve=nc.vector, pool=nc.gpsimd, sp=nc.sync)
        self.sem = {}
        self.cnt = {}
        self.seen = {e: {} for e in self.ENGS}
        for e in self.ENGS:
            self.new_sem("c_" + e)

    def new_sem(self, name):
        self.sem[name] = self.stack.enter_context(self.nc.semaphore(name))
        self.cnt[name] = 0
        return name

    def _wait(self, e, tok):
        if tok is None:
            return
        s, v = tok
        if e == "pe" and s == "c_pe":
            return
        if self.seen[e].get(s, 0) >= v:
            return
        self.eng[e].wait_ge(self.sem[s], v)
        self.seen[e][s] = v

    @staticmethod
    def _split(R, W):
        if any(b.excl for b in R):
            W = list(W) + [b for b in R if b.excl]
            R = [b for b in R if not b.excl]
        return R, W

    def _deps(self, e, R, W):
        for b in R:
            self._wait(e, b.w)
        for b in W:
            self._wait(e, b.w)
            for t in b.r:
                self._wait(e, t)

    def _commit(self, tok, R, W):
        for b in W:
            b.w = tok
            b.r = []
        for b in R:
            b.r.append(tok)
            if len(b.r) > 12:
                best = {}
                for (s, v) in b.r:
                    if best.get(s, 0) < v:
                        best[s] = v
                b.r = list(best.items())

    def op(self, e, fn, R=(), W=()):
        R, W = self._split(R, W)
        self._deps(e, R, W)
        ins = fn(self.eng[e])
        s = "c_" + e
        self.cnt[s] += 1
        ins.then_inc(self.sem[s], 1)
        tok = (s, self.cnt[s])
        self._commit(tok, R, W)
        return tok

    def group(self, e, fns, R=(), W=()):
        R, W = self._split(R, W)
        self._deps(e, R, W)
        ins = None
        for fn in fns:
            ins = fn(self.eng[e])
        s = "c_" + e
        self.cnt[s] += 1
        ins.then_inc(self.sem[s], 1)
        tok = (s, self.cnt[s])
        self._commit(tok, R, W)
        return tok

    def dma(self, e, sem, out, in_, R=(), W=(), n=1):
        self._deps(e, R, W)
        self.eng[e].dma_start(out=out, in_=in_).then_inc(self.sem[sem], 16)
        self.cnt[sem] += 16
        tok = (sem, self.cnt[sem])
        self._commit(tok, R, W)
        return tok

    def barrier(self):
        for e in self.ENGS:
            for p in self.ENGS:
                if self.cnt["c_" + p] > 0:
                    self._wait(e, ("c_" + p, self.cnt["c_" + p]))


PV_LAYOUT = {}


def _pv_plan():
    if PV_LAYOUT:
        return
    col = 0

    def add(name, n):
        nonlocal col
        PV_LAYOUT[name] = (col, n // 128)
        col += n // 128

    for l in range(2):
        for nm in ("ffn1_pre_g", "ffn1_post_g", "ffn2_pre_g", "ffn2_post_g", "mix_pre_g", "mix_post_g"):
            add(f"{nm}{l}", D)
    add("lru_b_in", 2 * D)
    for k in range(4):
        add(f"lru_conv_w{k}", D)
    for nm in ("lru_conv_b", "lru_b_r", "lru_b_i", "lru_lambda", "lru_b_out", "kv_norm_g"):
        add(nm, D)
    PV_LAYOUT["_ncol"] = (col, 0)


def _fm(v):
    v = np.asarray(v, dtype=np.float32).reshape(-1, 128)
    return np.ascontiguousarray(v.T)


def pack_pv(inp):
    _pv_plan()
    ncol = PV_LAYOUT["_ncol"][0]
    pv = np.zeros((128, ncol), np.float32)

    def put(name, v):
        c0, n = PV_LAYOUT[name]
        pv[:, c0:c0 + n] = _fm(v)

    for l in range(2):
        for nm in ("ffn1_pre_g", "ffn1_post_g", "ffn2_pre_g", "ffn2_post_g", "mix_pre_g", "mix_post_g"):
            put(f"{nm}{l}", inp[nm][l])
    put("lru_b_in", inp["lru_b_in"][0])
    for k in range(4):
        put(f"lru_conv_w{k}", inp["lru_conv_w"][0, k])
    for nm in ("lru_conv_b", "lru_b_r", "lru_b_i", "lru_lambda", "lru_b_out"):
        put(nm, inp[nm][0])
    put("kv_norm_g", inp["kv_norm_g"])
    return pv


def tile_w_out_chunks(w):
    K, N = w.shape
    a = np.asarray(w, np.float32).reshape(K // 128, 128, N // 128, 128)
    return np.ascontiguousarray(a.transpose(2, 1, 0, 3))


def tile_w_rows(w):
    K, N = w.shape
    return np.ascontiguousarray(np.asarray(w, np.float32).reshape(K // 128, 128, N))


def pack_weights(inp):
    out = {}
    for l in range(2):
        for which in ("ffn1", "ffn2"):
            g = tile_w_out_chunks(inp[f"{which}_w_gate"][l])
            u = tile_w_out_chunks(inp[f"{which}_w_up"][l])
            out[f"{which}_wgu{l}"] = np.ascontiguousarray(np.stack([g, u], axis=2))
            out[f"{which}_wd{l}"] = tile_w_out_chunks(inp[f"{which}_w_down"][l])
    out["lru_win"] = tile_w_out_chunks(inp["lru_w_in"][0])
    out["lru_wout"] = tile_w_out_chunks(inp["lru_w_out"][0])
    wg = np.stack([np.asarray(inp["lru_w_r"][0], np.float32), np.asarray(inp["lru_w_i"][0], np.float32)], axis=1)
    wg = wg.reshape(4, 2, 2, 128, 2, 128)
    out["lru_wg"] = np.ascontiguousarray(wg.transpose(0, 3, 1, 2, 4, 5).reshape(4, 128, 8, 128))
    out["wq"] = tile_w_out_chunks(inp["attn_w_q"][0])
    out["wo"] = tile_w_out_chunks(inp["attn_w_o"][0])
    out["wk"] = tile_w_out_chunks(np.asarray(inp["w_kv"])[:, :D])
    out["wv"] = tile_w_rows(np.asarray(inp["w_kv"])[:, D:])
    out["rbT"] = np.ascontiguousarray(np.asarray(inp["rel_bias"], np.float32).T)
    return out


def t5_bucket_np(d):
    n = np.maximum(d, 0)
    nf = np.maximum(n, 1).astype(np.float32)
    large = 16 + (np.log(nf / np.float32(16.0)) / np.float32(math.log(128 / 16)) * np.float32(16.0)).astype(np.int32)
    large = np.minimum(large, 31)
    return np.where(n < 16, n, large)


FREP_W = 640
FAR_D0 = 129


def make_consts():
    c = {}
    c["ones"] = np.ones((128, 128), np.float32)
    c["ident"] = np.eye(128, dtype=np.float32)
    far = t5_bucket_np(np.arange(FAR_D0, S_LEN))
    assert (far == far[0]).all()
    oh = np.zeros((32, FREP_W + 1), np.float32)
    dd = np.arange(FREP_W)
    d = dd - 255
    bk = t5_bucket_np(d)
    for i in range(FREP_W):
        if d[i] >= 0:
            oh[bk[i], i] = 1.0
    oh[far[0], FREP_W] = 1.0
    c["oh"] = oh
    e8 = np.zeros((8, 8, 128), np.float32)
    for n in range(8):
        e8[n, n, :] = 1.0
    c["e8"] = e8
    return c


class Ctx:
    pass


class WStream:
    def __init__(self, K, st, name, shape, nslot, sem0=0):
        self.K = K
        self.name = name
        self.n = nslot
        self.t = [st.enter_context(SB(K.nc, f"{name}{i}", shape, BF16)) for i in range(nslot)]
        self.b = [Buf(f"{name}{i}") for i in range(nslot)]
        self.s = [K.wsem[sem0 + i] for i in range(nslot)]
        self.plan = []
        self.issued = 0
        self.base = 0

    def extend(self, srcs):
        self.plan.extend(srcs)
        self._pump()

    def _pump(self):
        while self.issued < len(self.plan) and self.issued < self.base + self.n:
            i = self.issued
            sl = i % self.n
            src = self.plan[i]
            dst = self.t[sl]
            self.K.S.dma("pool", self.s[sl], dst[:], src, W=[self.b[sl]])
            self.issued += 1

    def get(self):
        sl = self.base % self.n
        return self.t[sl], self.b[sl]

    def done(self):
        self.base += 1
        self._pump()


def mm(out, lhsT, rhs, start, stop):
    return lambda e: e.matmul(out, lhsT, rhs, start=start, stop=stop)


def rms_rstd(K, ps_stat, ps_b, rstd_t, rstd_b, srt=None, srt_b=None):
    S = K.S
    srt = K.srt if srt is None else srt
    srt_b = K.srt_b if srt_b is None else srt_b
    S.op("act", lambda e: e.activation(out=srt[:], in_=ps_stat[:], func=AF.Sqrt, scale=1.0 / D, bias=K.eps_ap),
         R=[ps_b], W=[srt_b])
    S.op("dve", lambda e: e.reciprocal(out=rstd_t, in_=srt[:]), R=[srt_b], W=[rstd_b])


def prenorm(K, t5, gcol, xn_t, xn_b, rstd_t, rstd_b, ps, ps_b, alt=None):
    S = K.S
    t0 = t5 * 512
    sq, sq_b, srt, srt_b = (K.sq, K.sq_b, K.srt, K.srt_b) if alt is None else alt
    xbs = [K.x_b[t5][c] for c in range(KC)]
    S.op("act", lambda e: e.activation(out=sq[:], in_=K.x[:, :, t0:t0 + 512], func=AF.Square),
         R=xbs, W=[sq_b])
    S.group("pe", [mm(ps[:], K.ones[:], sq[:, c, :], c == 0, c == KC - 1) for c in range(KC)],
            R=[sq_b], W=[ps_b])
    rms_rstd(K, ps, ps_b, rstd_t, rstd_b, srt, srt_b)
    for c in range(KC):
        S.op("dve", lambda e, c=c: e.scalar_tensor_tensor(
            out=xn_t[:, c, :], in0=K.x[:, c, t0:t0 + 512], scalar=K.pv[:, gcol + c:gcol + c + 1],
            in1=rstd_t, op0=ALU.mult, op1=ALU.mult), R=[xbs[c], rstd_b],
            W=[xn_b[c] if isinstance(xn_b, list) else xn_b])


def ffn_stage(K, l, which, TT):
    S, nc = K.S, K.nc
    nsub = TT // 512
    ntt = S_LEN // TT
    gpre = PV_LAYOUT[f"{which}_pre_g{l}"][0]
    gpost = PV_LAYOUT[f"{which}_post_g{l}"][0]
    wgu_src = K.dram[f"{which}_wgu{l}"]
    wd_src = K.dram[f"{which}_wd{l}"]
    P, Pb = K.ps, K.ps_b
    with ExitStack() as st:
        xn = st.enter_context(SB(nc, "f_xn", [128, nsub, KC, 512], BF16))
        h = st.enter_context(SB(nc, "f_h", [128, FC, nsub, 512], BF16))
        y = st.enter_context(SB(nc, "f_y", [128, nsub, KC, 512], F32))
        sg = [st.enter_context(SB(nc, f"f_sg{i}", [128, 512], F32)) for i in range(2)]
        ysq = [st.enter_context(SB(nc, f"f_ysq{i}", [128, 512], BF16)) for i in range(2)]
        rstd = st.enter_context(SB(nc, "f_rstd", [128, nsub, 512], F32))
        rstd2 = st.enter_context(SB(nc, "f_rstd2", [128, nsub, 512], F32))
        rstd2_b = [Buf("rstd2") for _ in range(nsub)]
        xn_b = [Buf("xn") for _ in range(nsub)]
        h_b = [[Buf("h") for _ in range(nsub)] for _ in range(FC)]
        y_b = [[Buf("y") for _ in range(KC)] for _ in range(nsub)]
        sg_b = [Buf("sg") for _ in range(2)]
        ysq_b = [Buf("ysq") for _ in range(2)]
        rstd_b = [Buf("rstd") for _ in range(nsub)]
        K.wgu = WStream(K, st, "wgu", [128, 2, KC, 128], 3, 0)
        K.wd = WStream(K, st, "wd", [128, FC, 128], 2, 3)

        K.wgu.extend([wgu_src[f] for _ in range(ntt) for f in range(FC)])
        K.wd.extend([wd_src[c] for _ in range(ntt) for c in range(KC)])

        def front(tt, subs=None):
            for sub in (range(nsub) if subs is None else subs):
                t5 = tt * nsub + sub
                prenorm(K, t5, gpre, xn[:, sub], xn_b[sub], rstd2[:, sub, :], rstd2_b[sub], P[sub], Pb[sub])

        front(0)
        carry = []
        for tt in range(ntt):
            it = 0
            for f in range(FC):
                wt, wb = K.wgu.get()
                for sub in range(nsub):
                    s2 = it % 2
                    it += 1
                    gp, up = P[2 * s2], P[2 * s2 + 1]
                    S.group("pe", [mm(gp[:], wt[:, 0, k, :], xn[:, sub, k, :], k == 0, k == KC - 1) for k in range(KC)],
                            R=[wb, xn_b[sub]], W=[Pb[2 * s2]])
                    S.group("pe", [mm(up[:], wt[:, 1, k, :], xn[:, sub, k, :], k == 0, k == KC - 1) for k in range(KC)],
                            R=[wb, xn_b[sub]], W=[Pb[2 * s2 + 1]])
                    S.op("act", lambda e, gp=gp, s2=s2: e.activation(out=sg[s2][:], in_=gp[:], func=AF.Silu),
                         R=[Pb[2 * s2]], W=[sg_b[s2]])
                    S.op("dve", lambda e, up=up, s2=s2, f=f, sub=sub: e.tensor_tensor(
                        out=h[:, f, sub, :], in0=up[:], in1=sg[s2][:], op=ALU.mult),
                        R=[Pb[2 * s2 + 1], sg_b[s2]], W=[h_b[f][sub]])
                    if carry and f >= 1:
                        carry.pop(0)()
                K.wgu.done()
            while carry:
                carry.pop(0)()
            pend = None
            it = 0
            for c in range(KC):
                wt, wb = K.wd.get()
                for sub in range(nsub):
                    s2 = it % 2
                    it += 1
                    yp, ypb = P[4 + s2], Pb[4 + s2]
                    S.group("pe", [mm(yp[:], wt[:, f, :], h[:, f, sub, :], f == 0, f == FC - 1) for f in range(FC)],
                            R=[wb] + [h_b[f][sub] for f in range(FC)], W=[ypb])
                    S.op("dve", lambda e, yp=yp, sub=sub, c=c: e.tensor_copy(out=y[:, sub, c, :], in_=yp[:]),
                         R=[ypb], W=[y_b[sub][c]])
                    S.op("act", lambda e, sub=sub, c=c, s2=s2: e.activation(out=ysq[s2][:], in_=y[:, sub, c, :], func=AF.Square),
                         R=[y_b[sub][c]], W=[ysq_b[s2]])
                    if pend is not None:
                        pend()
                    pend = (lambda s2=s2, sub=sub, c=c: S.group(
                        "pe", [mm(P[6 + sub][:], K.ones[:], ysq[s2][:], c == 0, c == KC - 1)],
                        R=[ysq_b[s2]], W=[Pb[6 + sub]]))
                K.wd.done()
                if tt + 1 < ntt:
                    if nsub == 1 and c == KC // 2:
                        front(tt + 1)
                    elif nsub == 2 and c in (2, 5):
                        front(tt + 1, [0 if c == 2 else 1])
            pend()
            pieces = []
            for sub in range(nsub):
                t5 = tt * nsub + sub
                t0 = t5 * 512
                pieces.append(lambda sub=sub: rms_rstd(K, P[6 + sub], Pb[6 + sub], rstd[:, sub, :], rstd_b[sub]))
                for c in FIN_ORDER:
                    def piece(sub=sub, c=c, t0=t0, t5=t5):
                        S.op("dve", lambda e: e.scalar_tensor_tensor(
                            out=y[:, sub, c, :], in0=y[:, sub, c, :], scalar=K.hp[:, gpost + c:gpost + c + 1],
                            in1=rstd[:, sub, :], op0=ALU.mult, op1=ALU.mult),
                            R=[rstd_b[sub]], W=[y_b[sub][c]])
                        if c >= FIN_POOL_FROM:
                            S.op("pool", lambda e: e.tensor_tensor(
                                out=K.x[:, c, t0:t0 + 512], in0=K.x[:, c, t0:t0 + 512], in1=y[:, sub, c, :], op=ALU.add),
                                R=[y_b[sub][c]], W=[K.x_b[t5][c]])
                        else:
                            S.op("dve", lambda e: e.scalar_tensor_tensor(
                                out=K.x[:, c, t0:t0 + 512], in0=y[:, sub, c, :], scalar=1.0, in1=K.x[:, c, t0:t0 + 512],
                                op0=ALU.mult, op1=ALU.add), R=[y_b[sub][c]], W=[K.x_b[t5][c]])
                    pieces.append(piece)
            if tt + 1 < ntt:
                carry.extend(pieces)
            else:
                for p_ in pieces:
                    p_()
        S.barrier()


class PostNorm:
    def __init__(self, K, st, tag, nbuf=1):
        nc = K.nc
        self.K = K
        self.ys = [st.enter_context(SB(nc, f"{tag}_y{i}", [128, KC, 512], F32)) for i in range(nbuf)]
        self.y_bs = [[Buf("y") for _ in range(KC)] for _ in range(nbuf)]
        self.y = self.ys[0]
        self.ysq = [st.enter_context(SB(nc, f"{tag}_ysq{i}", [128, 512], BF16)) for i in range(2)]
        self.rstd = st.enter_context(SB(nc, tag + "_rstd", [128, 512], F32))
        self.y_b = self.y_bs[0]
        self.ysq_b = [Buf("ysq") for _ in range(2)]
        self.rstd_b = Buf("rstd")
        self.pend = None
        self.it = 0

    def use(self, slot):
        self.y = self.ys[slot]
        self.y_b = self.y_bs[slot]

    def evac(self, c, yp, ypb, bias_ap=None, stat_bank=6):
        K, S = self.K, self.K.S
        s2 = self.it % 2
        self.it += 1
        y, y_b = self.y, self.y_b
        if bias_ap is None:
            S.op("dve", lambda e: e.tensor_copy(out=y[:, c, :], in_=yp[:]), R=[ypb], W=[y_b[c]])
        else:
            S.op("dve", lambda e: e.tensor_scalar(out=y[:, c, :], in0=yp[:], scalar1=bias_ap, scalar2=None,
                                                  op0=ALU.add), R=[ypb], W=[y_b[c]])
        S.op("act", lambda e: e.activation(out=self.ysq[s2][:], in_=y[:, c, :], func=AF.Square),
             R=[y_b[c]], W=[self.ysq_b[s2]])
        if self.pend is not None:
            self.pend()
        P, Pb = K.ps[stat_bank], K.ps_b[stat_bank]
        self.pend = lambda: S.group("pe", [mm(P[:], K.ones[:], self.ysq[s2][:], c == 0, c == KC - 1)],
                                    R=[self.ysq_b[s2]], W=[Pb])

    def flush(self):
        if self.pend is not None:
            self.pend()
            self.pend = None

    def finish_pieces(self, t5, gt, gcol, stat_bank=6, slot=None):
        K, S = self.K, self.K.S
        y, y_b = (self.y, self.y_b) if slot is None else (self.ys[slot], self.y_bs[slot])
        t0 = t5 * 512
        pieces = [lambda: rms_rstd(K, K.ps[stat_bank], K.ps_b[stat_bank], self.rstd[:], self.rstd_b)]
        for c in FIN_ORDER:
            def piece(c=c):
                S.op("dve", lambda e: e.scalar_tensor_tensor(
                    out=y[:, c, :], in0=y[:, c, :], scalar=gt[:, gcol + c:gcol + c + 1],
                    in1=self.rstd[:], op0=ALU.mult, op1=ALU.mult),
                    R=[self.rstd_b], W=[y_b[c]])
                if c >= FIN_POOL_FROM:
                    S.op("pool", lambda e: e.tensor_tensor(
                        out=K.x[:, c, t0:t0 + 512], in0=K.x[:, c, t0:t0 + 512], in1=y[:, c, :], op=ALU.add),
                        R=[y_b[c]], W=[K.x_b[t5][c]])
                else:
                    S.op("dve", lambda e: e.scalar_tensor_tensor(
                        out=K.x[:, c, t0:t0 + 512], in0=y[:, c, :], scalar=1.0, in1=K.x[:, c, t0:t0 + 512],
                        op0=ALU.mult, op1=ALU.add), R=[y_b[c]], W=[K.x_b[t5][c]])
            pieces.append(piece)
        return pieces

    def finish(self, t5, gt, gcol, stat_bank=6, slot=None):
        K, S = self.K, self.K.S
        self.flush()
        y, y_b = (self.y, self.y_b) if slot is None else (self.ys[slot], self.y_bs[slot])
        t0 = t5 * 512
        rms_rstd(K, K.ps[stat_bank], K.ps_b[stat_bank], self.rstd[:], self.rstd_b)
        for c in FIN_ORDER:
            S.op("dve", lambda e, c=c: e.scalar_tensor_tensor(
                out=y[:, c, :], in0=y[:, c, :], scalar=gt[:, gcol + c:gcol + c + 1],
                in1=self.rstd[:], op0=ALU.mult, op1=ALU.mult),
                R=[self.rstd_b], W=[y_b[c]])
            if c >= FIN_POOL_FROM:
                S.op("pool", lambda e, c=c: e.tensor_tensor(
                    out=K.x[:, c, t0:t0 + 512], in0=K.x[:, c, t0:t0 + 512], in1=y[:, c, :], op=ALU.add),
                    R=[y_b[c]], W=[K.x_b[t5][c]])
            else:
                S.op("dve", lambda e, c=c: e.scalar_tensor_tensor(
                    out=K.x[:, c, t0:t0 + 512], in0=y[:, c, :], scalar=1.0, in1=K.x[:, c, t0:t0 + 512],
                    op0=ALU.mult, op1=ALU.add), R=[y_b[c]], W=[K.x_b[t5][c]])


def lru_stage(K):
    S, nc = K.S, K.nc
    P, Pb = K.ps, K.ps_b
    NT = S_LEN // 512
    c_bin = PV_LAYOUT["lru_b_in"][0]
    c_cw = [PV_LAYOUT[f"lru_conv_w{k}"][0] for k in range(4)]
    c_cb = PV_LAYOUT["lru_conv_b"][0]
    c_br = PV_LAYOUT["lru_b_r"][0]
    c_bi = PV_LAYOUT["lru_b_i"][0]
    c_lam = PV_LAYOUT["lru_lambda"][0]
    c_bo = PV_LAYOUT["lru_b_out"][0]
    gpre = PV_LAYOUT["mix_pre_g0"][0]
    gpost = PV_LAYOUT["mix_post_g0"][0]
    win, wout, wg = K.dram["lru_win"], K.dram["lru_wout"], K.dram["lru_wg"]
    pv, hp = K.pv, K.hp
    with ExitStack() as st0, ExitStack() as st:
        T = lambda name, shape, dt=F32: st.enter_context(SB(nc, "l_" + name, shape, dt))
        xn = st0.enter_context(SB(nc, "l_xn", [128, KC, S_LEN], BF16))
        gT = st0.enter_context(SB(nc, "l_gT", [128, KC, S_LEN], BF16))
        K.wp = WStream(K, st0, "wp", [128, KC, 128], 6, 0)
        xbr = [T(f"xbr{i}", [128, 2, 515]) for i in range(2)]
        xc = [T(f"xc{i}", [128, 2, 512]) for i in range(2)]
        xcb = [T(f"xcb{i}", [128, 2, 512], BF16) for i in range(2)]
        gy = [T(f"gy{i}", [128, 2, 512]) for i in range(2)]
        r_t = T("r", [128, 2, 512])
        i_t = T("i", [128, 2, 512])
        a_t = T("a", [128, 2, 512])
        m_t = T("m", [128, 2, 512])
        u_t = T("u", [128, 2, 512])
        h_t = T("h", [128, 2, 512])
        hst = T("hst", [128, KC])
        cl = T("cl", [128, 2 * KC])
        rstd = T("rstd", [128, 512])
        xn_b = [Buf("xn") for _ in range(NT)]
        gT_b = [[Buf("gT") for _ in range(KC)] for _ in range(NT)]
        xbr_b = [[Buf("xbr") for _ in range(2)] for _ in range(2)]
        xc_b = [[Buf("xc") for _ in range(2)] for _ in range(2)]
        xcb_b = [[Buf("xcb") for _ in range(2)] for _ in range(2)]
        gy_b = [[Buf("gy") for _ in range(2)] for _ in range(2)]
        r_b = [Buf("r") for _ in range(2)]
        i_b = [Buf("i") for _ in range(2)]
        a_b = [Buf("a") for _ in range(2)]
        m_b = [Buf("m") for _ in range(2)]
        u_b = [Buf("u") for _ in range(2)]
        h_b = [Buf("h") for _ in range(2)]
        hst_b = [Buf("hst") for _ in range(KC)]
        cl_b, rstd_b = Buf("cl"), Buf("rstd")

        iters = [(hb, tt) for hb in range(4) for tt in range(NT)]
        a_tiles = lambda n: [win[iters[n][0] * 2], win[iters[n][0] * 2 + 1], win[8 + iters[n][0] * 2], win[8 + iters[n][0] * 2 + 1]]
        plan = a_tiles(0)
        for n in range(len(iters)):
            plan.append(wg[iters[n][0]])
            if n + 1 < len(iters):
                plan += a_tiles(n + 1)
        for t5 in range(NT):
            plan += [wout[c] for c in range(KC)]
        K.wp.extend(plan)

        S.op("act", lambda e: e.activation(out=cl[:, 0:KC], in_=pv[:, c_lam:c_lam + KC], func=AF.Exp, scale=-1.0), W=[cl_b])
        S.op("act", lambda e: e.activation(out=cl[:, 0:KC], in_=cl[:, 0:KC], func=AF.Ln, bias=K.one_ap), R=[cl_b], W=[cl_b])
        S.op("dve", lambda e: e.tensor_scalar(out=cl[:, KC:2 * KC], in0=cl[:, 0:KC], scalar1=-8.0, scalar2=None, op0=ALU.mult), R=[cl_b], W=[cl_b])
        S.op("dve", lambda e: e.tensor_scalar(out=cl[:, 0:KC], in0=cl[:, 0:KC], scalar1=-4.0, scalar2=None, op0=ALU.mult), R=[cl_b], W=[cl_b])

        def lru_prenorm(t5):
            prenorm(K, t5, gpre, xn[:, :, t5 * 512:(t5 + 1) * 512], xn_b[t5], rstd[:], rstd_b, P[6], Pb[6])

        lru_prenorm(0)

        iters = [(hb, tt) for hb in range(4) for tt in range(NT)]

        def stage_a_pe(n):
            hb, tt = iters[n]
            t0 = tt * 512
            for br in range(2):
                for jc in range(2):
                    wt, wb = K.wp.get()
                    pp, ppb = P[br * 2 + jc], Pb[br * 2 + jc]
                    S.group("pe", [mm(pp[:], wt[:, k, :], xn[:, k, t0:t0 + 512], k == 0, k == KC - 1) for k in range(KC)],
                            R=[wb, xn_b[tt]], W=[ppb])
                    K.wp.done()

        def stage_a_rest(n):
            hb, tt = iters[n]
            t0 = tt * 512
            d = n % 2
            cur, prv = xbr[d], xbr[1 - d]
            cur_b, prv_b = xbr_b[d], xbr_b[1 - d]
            for br in range(2):
                for jc in range(2):
                    ch = hb * 2 + jc
                    pp, ppb = P[br * 2 + jc], Pb[br * 2 + jc]
                    bcol = c_bin + br * KC + ch
                    if br == 0:
                        S.op("act", lambda e, pp=pp, jc=jc, bcol=bcol: e.activation(
                            out=cur[:, jc, 3:515], in_=pp[:], func=AF.Identity, bias=pv[:, bcol:bcol + 1]),
                            R=[ppb], W=[cur_b[jc]])
                    else:
                        S.op("act", lambda e, pp=pp, jc=jc, bcol=bcol: e.activation(
                            out=gy[d][:, jc, :], in_=pp[:], func=AF.Gelu_apprx_tanh, bias=pv[:, bcol:bcol + 1]),
                            R=[ppb], W=[gy_b[d][jc]])
            for jc in range(2):
                ch = hb * 2 + jc
                if tt == 0:
                    S.op("dve", lambda e, jc=jc: e.memset(cur[:, jc, 0:3], 0.0), W=[cur_b[jc]])
                else:
                    S.op("dve", lambda e, jc=jc: e.tensor_copy(out=cur[:, jc, 0:3], in_=prv[:, jc, 512:515]),
                         R=[prv_b[jc]], W=[cur_b[jc]])
                S.op("dve", lambda e, jc=jc, ch=ch: e.tensor_scalar(
                    out=xc[d][:, jc, :], in0=cur[:, jc, 0:512], scalar1=pv[:, c_cw[0] + ch:c_cw[0] + ch + 1],
                    scalar2=pv[:, c_cb + ch:c_cb + ch + 1], op0=ALU.mult, op1=ALU.add),
                    R=[cur_b[jc]], W=[xc_b[d][jc]])
                for k in range(1, 4):
                    S.op("dve", lambda e, jc=jc, ch=ch, k=k: e.scalar_tensor_tensor(
                        out=xc[d][:, jc, :], in0=cur[:, jc, k:k + 512], scalar=pv[:, c_cw[k] + ch:c_cw[k] + ch + 1],
                        in1=xc[d][:, jc, :], op0=ALU.mult, op1=ALU.add),
                        R=[cur_b[jc]], W=[xc_b[d][jc]])
                S.op("act", lambda e, jc=jc: e.activation(out=xcb[d][:, jc, :], in_=xc[d][:, jc, :], func=AF.Copy),
                     R=[xc_b[d][jc]], W=[xcb_b[d][jc]])

        def stage_b_pe(n):
            hb, tt = iters[n]
            d = n % 2
            wt, wb = K.wp.get()
            for jc in range(2):
                for g in range(2):
                    pp, ppb = P[4 + jc * 2 + g], Pb[4 + jc * 2 + g]
                    S.group("pe", [mm(pp[:], wt[:, g * 4 + ic * 2 + jc, :], xcb[d][:, ic, :], ic == 0, ic == 1) for ic in range(2)],
                            R=[wb, xcb_b[d][0], xcb_b[d][1]], W=[ppb])
            K.wp.done()

        def stage_b_rest(n):
            hb, tt = iters[n]
            t0 = tt * 512
            d = n % 2
            for jc in range(2):
                ch = hb * 2 + jc
                S.op("act", lambda e, jc=jc, ch=ch: e.activation(
                    out=r_t[:, jc, :], in_=P[4 + jc * 2][:], func=AF.Tanh, scale=0.5, bias=hp[:, c_br + ch:c_br + ch + 1]),
                    R=[Pb[4 + jc * 2]], W=[r_b[jc]])
                S.op("act", lambda e, jc=jc, ch=ch: e.activation(
                    out=i_t[:, jc, :], in_=P[4 + jc * 2 + 1][:], func=AF.Tanh, scale=0.5, bias=hp[:, c_bi + ch:c_bi + ch + 1]),
                    R=[Pb[4 + jc * 2 + 1]], W=[i_b[jc]])
            for jc in range(2):
                ch = hb * 2 + jc
                S.op("act", lambda e, jc=jc, ch=ch: e.activation(
                    out=a_t[:, jc, :], in_=r_t[:, jc, :], func=AF.Exp, scale=cl[:, ch:ch + 1], bias=cl[:, ch:ch + 1]),
                    R=[r_b[jc], cl_b], W=[a_b[jc]])
                S.op("act", lambda e, jc=jc, ch=ch: e.activation(
                    out=m_t[:, jc, :], in_=r_t[:, jc, :], func=AF.Exp, scale=cl[:, KC + ch:KC + ch + 1], bias=cl[:, KC + ch:KC + ch + 1]),
                    R=[r_b[jc], cl_b], W=[m_b[jc]])
                S.op("dve", lambda e, jc=jc: e.scalar_tensor_tensor(
                    out=u_t[:, jc, :], in0=i_t[:, jc, :], scalar=1.0, in1=xc[d][:, jc, :], op0=ALU.add, op1=ALU.mult),
                    R=[i_b[jc], xc_b[d][jc]], W=[u_b[jc]])
            for jc in range(2):
                S.op("act", lambda e, jc=jc: e.activation(
                    out=m_t[:, jc, :], in_=m_t[:, jc, :], func=AF.Ln, scale=-1.0, bias=K.one_ap),
                    R=[m_b[jc]], W=[m_b[jc]])
            for jc in range(2):
                S.op("act", lambda e, jc=jc: e.activation(
                    out=m_t[:, jc, :], in_=m_t[:, jc, :], func=AF.Exp, scale=0.5),
                    R=[m_b[jc]], W=[m_b[jc]])

        def stage_b_rest2(n):
            hb, tt = iters[n]
            t0 = tt * 512
            d = n % 2
            for jc in range(2):
                ch = hb * 2 + jc
                S.op("dve", lambda e, jc=jc: e.scalar_tensor_tensor(
                    out=u_t[:, jc, :], in0=u_t[:, jc, :], scalar=0.5, in1=m_t[:, jc, :], op0=ALU.mult, op1=ALU.mult),
                    R=[m_b[jc]], W=[u_b[jc]])
                init = 0.0 if tt == 0 else hst[:, ch:ch + 1]
                S.op("dve", lambda e, jc=jc, init=init: e.tensor_tensor_scan(
                    out=h_t[:, jc, :], data0=a_t[:, jc, :], data1=u_t[:, jc, :], initial=init, op0=ALU.mult, op1=ALU.add),
                    R=[a_b[jc], u_b[jc], hst_b[ch]], W=[h_b[jc]])
                S.op("dve", lambda e, jc=jc, ch=ch: e.tensor_copy(out=hst[:, ch:ch + 1], in_=h_t[:, jc, 511:512]),
                     R=[h_b[jc]], W=[hst_b[ch]])
                S.op("pool", lambda e, jc=jc, ch=ch: e.tensor_tensor(
                    out=gT[:, ch, t0:t0 + 512], in0=h_t[:, jc, :], in1=gy[d][:, jc, :], op=ALU.mult),
                    R=[h_b[jc], gy_b[d][jc]], W=[gT_b[tt][ch]])

        lru_prenorm(1)
        stage_a_pe(0)
        stage_a_rest(0)
        for n in range(len(iters)):
            stage_b_pe(n)
            if n + 1 < len(iters):
                stage_a_pe(n + 1)
            stage_b_rest(n)
            if n + 2 < NT:
                lru_prenorm(n + 2)
            if n + 1 < len(iters):
                stage_a_rest(n + 1)
            stage_b_rest2(n)
        S.barrier()
        st.close()
        pn = PostNorm(K, st, "l_pn", nbuf=2)
        it = 0

        def body(t5, pieces=()):
            nonlocal it
            pieces = list(pieces)
            t0 = t5 * 512
            pn.use(t5 % 2)
            for c in range(KC):
                wt, wb = K.wp.get()
                pp, ppb = P[it % 2], Pb[it % 2]
                it += 1
                S.group("pe", [mm(pp[:], wt[:, k, :], gT[:, k, t0:t0 + 512], k == 0, k == KC - 1) for k in range(KC)],
                        R=[wb] + gT_b[t5], W=[ppb])
                K.wp.done()
                pn.evac(c, pp, ppb, bias_ap=pv[:, c_bo + c:c_bo + c + 1], stat_bank=6 + t5 % 2)
                for _ in range(3):
                    if pieces:
                        pieces.pop(0)()
            pn.flush()
            for p_ in pieces:
                p_()

        body(0)
        for t5 in range(NT):
            pcs = pn.finish_pieces(t5, K.pv, gpost, stat_bank=6 + t5 % 2, slot=t5 % 2)
            if t5 + 1 < NT:
                body(t5 + 1, pcs)
            else:
                for p_ in pcs:
                    p_()
        S.barrier()


def kvnorm_stage(K):
    S, nc = K.S, K.nc
    P, Pb = K.ps, K.ps_b
    scale = float(HD) ** -0.5
    K.cst_stack = ExitStack()
    if True:
        TR = lambda name, shape, dt=F32: K.cst_stack.enter_context(SB(nc, "c_" + name, shape, dt, side="right"))
        M = K.M = TR("M", [128, NHEAD, 512], BF16)
        cb = K.cb = TR("cb", [128, NHEAD])
        identb = K.identb = TR("identb", [128, 128], BF16)
        ident32 = K.ident32 = TR("ident32", [128, 128])
        E8 = K.E8 = K.cst_stack.enter_context(SB(nc, "c_E8", [8, NBLK, 128], BF16, side="right"))
        zt = K.zt = TR("zero", [128, 1])
        M_b, cb_b, cst_b = Buf("M"), Buf("cb"), Buf("cst")
        st1 = ExitStack()
        if True:
            rbT = st1.enter_context(SB(nc, "a_rbT", [32, NHEAD], F32))
            oh = st1.enter_context(SB(nc, "a_oh", [32, FREP_W + 1], F32))
            ones32 = st1.enter_context(SB(nc, "a_ones32", [32, 128], F32))
            rbb = st1.enter_context(SB(nc, "a_rbb", [32, NHEAD, 128], F32))
            frep = st1.enter_context(SB(nc, "a_frep", [128, NHEAD, FREP_W], BF16))
            rb_b, rbb_b, frep_b, fd_b = Buf("rb"), Buf("rbb"), Buf("frep"), Buf("fd")
            S.dma("sp", "ld_c", rbT[:], K.dram["rbT"][:, :], W=[rb_b])
            S.dma("sp", "ld_c", oh[:], K.dram["oh"][:, :], W=[rb_b])
            tk2 = S.dma("sp", "ld_c", ident32[:], K.dram["ident"][:, :], W=[cst_b])
            rb_b.w = tk2
            S.dma("pool", "ld_c2", identb[:], K.dram["ident"][:, :], W=[cst_b])
            tk3 = S.dma("pool", "ld_c2", E8[:], K.dram["e8"][:, :, :], W=[cst_b])
            S.op("dve", lambda e: e.memset(zt[:], 0.0), W=[cst_b])
            S.op("dve", lambda e: e.memset(ones32[:], 1.0), W=[rbb_b])
            for h in range(NHEAD):
                S.op("dve", lambda e, h=h: e.tensor_scalar(out=rbb[:, h, :], in0=ones32[:], scalar1=rbT[:, h:h + 1],
                                                           scalar2=None, op0=ALU.mult), R=[rb_b], W=[rbb_b])
            h2 = FREP_W // 2
            for h in range(NHEAD):
                pa, pab = P[(2 * h) % 4], Pb[(2 * h) % 4]
                pb_, pbb = P[(2 * h + 1) % 4], Pb[(2 * h + 1) % 4]
                S.group("pe", [mm(pa[:, 0:h2], rbb[:, h, :], oh[:, 0:h2], True, True)], R=[rbb_b, rb_b], W=[pab])
                S.group("pe", [mm(pb_[:, 0:h2 + 1], rbb[:, h, :], oh[:, h2:FREP_W + 1], True, True)], R=[rbb_b, rb_b], W=[pbb])
                S.op("act", lambda e, h=h, pa=pa: e.activation(out=frep[:, h, 0:h2], in_=pa[:, 0:h2], func=AF.Copy, scale=1.0 / scale),
                     R=[pab], W=[frep_b])
                S.op("act", lambda e, h=h, pb_=pb_: e.activation(out=frep[:, h, h2:FREP_W], in_=pb_[:, 0:h2], func=AF.Copy, scale=1.0 / scale),
                     R=[pbb], W=[frep_b])
                S.op("act", lambda e, h=h, pb_=pb_: e.activation(out=cb[:, h:h + 1], in_=pb_[:, h2:h2 + 1], func=AF.Copy),
                     R=[pbb], W=[cb_b])
            S.op("dve", lambda e: e.memset(frep[:, :, 0:255], NEG), R=[frep_b], W=[frep_b])
            fd = K.frep_dram
            S.dma("sp", "ld_c", fd.ap(), frep[:].rearrange("p h w -> p (h w)"), R=[frep_b], W=[fd_b])
            skew = bass.AP(tensor=fd, offset=127, ap=[[NHEAD * FREP_W - 1, 128], [FREP_W, NHEAD], [1, 512]])
            with nc.allow_non_contiguous_dma(reason="toeplitz skew"):
                S.dma("sp", "ld_c", M[:], skew, R=[fd_b], W=[M_b])
    K.kv_stack = ExitStack()
    K.xnkv = K.kv_stack.enter_context(SB(nc, "xnkv", [128, KC, S_LEN], BF16, side="right"))
    K.xnkv_b = [Buf("xnkv") for _ in range(S_LEN // 512)]
    g = PV_LAYOUT["kv_norm_g"][0]
    with ExitStack() as st:
        rstd = [st.enter_context(SB(nc, f"kv_rstd{i}", [128, 512], F32)) for i in range(2)]
        rstd_b = [Buf("rstd") for _ in range(2)]
        sq2 = st.enter_context(SB(nc, "kv_sq2", [128, KC, 512], BF16))
        srt2 = st.enter_context(SB(nc, "kv_srt2", [128, 512], F32))
        alts = [None, (sq2, Buf("sq2"), srt2, Buf("srt2"))]
        for t5 in range(S_LEN // 512):
            i = t5 % 2
            prenorm(K, t5, g, K.xnkv[:, :, t5 * 512:(t5 + 1) * 512], K.xnkv_b[t5], rstd[i][:], rstd_b[i],
                    K.ps[6 + i], K.ps_b[6 + i], alt=alts[i])
        S.barrier()
        for e in ("pe", "act", "dve", "pool"):
            S._wait(e, M_b.w)
            S._wait(e, tk3)
            S._wait(e, tk2)
    st1.close()


def attn_stage(K):
    S, nc = K.S, K.nc
    P, Pb = K.ps, K.ps_b
    NT = S_LEN // 512
    pv = K.pv
    gpre = PV_LAYOUT["mix_pre_g1"][0]
    gpost = PV_LAYOUT["mix_post_g1"][0]
    scale = float(HD) ** -0.5
    with ExitStack() as st0:
        T0 = lambda name, shape, dt=F32: st0.enter_context(SB(nc, "a_" + name, shape, dt))
        KT = T0("KT", [128, NHEAD, S_LEN], BF16)
        V = T0("V", [128, S_LEN // 128, D], BF16)
        kmT = T0("kmT", [128, NHEAD, NBLK], BF16)
        K.wp = WStream(K, st0, "wp", [128, KC, 128], 3, 0)
        KT_b = [[Buf("KT") for _ in range(NT)] for _ in range(NHEAD)]
        V_b = [Buf("V") for _ in range(S_LEN // 128)]
        kmT_b = Buf("kmT")
        with ExitStack() as st1:
            wv = st1.enter_context(SB(nc, "a_wv", [128, KC, D], BF16))
            kms = st1.enter_context(SB(nc, "a_kms", [128, NHEAD, NBLK], F32))
            wv_bs, kms_b = [Buf("wv") for _ in range(KC)], Buf("kms")
            K.wp.extend([K.dram["wk"][h] for h in range(NHEAD)])
            tok = None
            for k in range(KC):
                tok = S.dma("pool", K.wsem[6], wv[:, k, :], K.dram["wv"][k], W=[wv_bs[k]])
            for b in wv_bs:
                b.w = tok
            it = 0
            for h in range(NHEAD):
                wt, wb = K.wp.get()
                for t5 in range(NT):
                    t0 = t5 * 512
                    pp, ppb = P[it % 2], Pb[it % 2]
                    it += 1
                    S.group("pe", [mm(pp[:], wt[:, k, :], K.xnkv[:, k, t0:t0 + 512], k == 0, k == KC - 1) for k in range(KC)],
                            R=[wb, K.xnkv_b[t5]], W=[ppb])
                    S.op("act", lambda e, pp=pp, h=h, t0=t0: e.activation(out=KT[:, h, t0:t0 + 512], in_=pp[:], func=AF.Copy),
                         R=[ppb], W=[KT_b[h][t5]])
                    S.op("dve", lambda e, pp=pp, h=h, t5=t5: e.tensor_reduce(
                        out=kms[:, h, 2 * t5:2 * t5 + 2], in_=pp[:].rearrange("p (b j) -> p b j", j=BLK), axis=AX.X, op=ALU.add),
                        R=[ppb], W=[kms_b])
                K.wp.done()
            S.op("dve", lambda e: e.tensor_scalar(out=kmT[:], in0=kms[:], scalar1=1.0 / BLK, scalar2=None, op0=ALU.mult),
                 R=[kms_b], W=[kmT_b])
            it = 0
            for tk in range(S_LEN // 128):
                for half in range(2):
                    pp, ppb = P[2 + it % 2], Pb[2 + it % 2]
                    S.group("pe", [mm(pp[:], K.xnkv[:, k, tk * 128:(tk + 1) * 128], wv[:, k, half * 512:(half + 1) * 512],
                                      k == 0, k == KC - 1) for k in range(KC)],
                            R=wv_bs + [K.xnkv_b[tk // 4]], W=[ppb])
                    if it % 2 == 0:
                        S.op("act", lambda e, pp=pp, tk=tk, half=half: e.activation(
                            out=V[:, tk, half * 512:(half + 1) * 512], in_=pp[:], func=AF.Copy), R=[ppb], W=[V_b[tk]])
                    else:
                        S.op("dve", lambda e, pp=pp, tk=tk, half=half: e.tensor_copy(
                            out=V[:, tk, half * 512:(half + 1) * 512], in_=pp[:]), R=[ppb], W=[V_b[tk]])
                    it += 1
            S.barrier()
        K.kv_stack.close()
        M, cb, identb, ident32, E8, zt = K.M, K.cb, K.identb, K.ident32, K.E8, K.zt
        T1 = T0
        xn = K.sq
        aT_t = T1("aT", [128, NHEAD, 512], BF16)
        QT = T1("QT", [128, NHEAD, 512], BF16)
        aT = aT_t
        AT = st0.enter_context(SB(nc, "a_AT", [8, NHEAD, 512], BF16))
        PT = [T1(f"PT{i}", [128, 256], BF16) for i in range(6)]
        psum_acc = [T1(f"pacc{i}", [128, 256]) for i in range(2)]
        rden = psum_acc
        pacc_b = [Buf("pacc") for _ in range(2)]
        paccbf_b = [Buf("paccbf") for _ in range(2)]
        gs = [T1(f"gs{i}", [128, NHEAD, NBLK]) for i in range(2)]
        mx = [T1(f"mx{i}", [128, NHEAD, 8]) for i in range(2)]
        Am = [T1(f"Am{i}", [128, NHEAD, NBLK]) for i in range(2)]
        pn = PostNorm(K, st0, "a_pn")
        rstd, rstd_b = pn.rstd, pn.rstd_b
        xn_b = K.sq_b
        QT_b = [Buf("QT") for _ in range(NHEAD)]
        aT_b = [Buf("aT") for _ in range(NHEAD)]
        AT_b = [Buf("AT") for _ in range(4)]
        PT_b = [Buf("PT") for _ in range(6)]
        gs_b, mx_b, Am_b = [Buf("gs"), Buf("gs")], [Buf("mx"), Buf("mx")], [Buf("Am"), Buf("Am")]
        ipt = 0
        iod = 0
        def front_norm(t5):
            prenorm(K, t5, gpre, xn[:], xn_b, rstd[:], rstd_b, P[7], Pb[7])

        def front_q(t5):
            K.wp.extend([K.dram["wq"][h] for h in range(NHEAD)])
            for h in range(NHEAD):
                wt, wb = K.wp.get()
                pp, ppb = P[6 + h % 2], Pb[6 + h % 2]
                S.group("pe", [mm(pp[:], wt[:, k, :], xn[:, k, :], k == 0, k == KC - 1) for k in range(KC)],
                        R=[wb, xn_b], W=[ppb])
                K.wp.done()
                if h % 2 == 0:
                    S.op("act", lambda e, pp=pp, h=h: e.activation(out=QT[:, h, :], in_=pp[:], func=AF.Copy), R=[ppb], W=[QT_b[h]])
                else:
                    S.op("dve", lambda e, pp=pp, h=h: e.tensor_copy(out=QT[:, h, :], in_=pp[:]), R=[ppb], W=[QT_b[h]])

        def front_gates(t5):
            if 2 * t5 < 4:
                return

            def gate_part(s4):
                qb = 2 * t5 + s4 // 2
                c0 = s4 * 128
                i = s4 % 2
                S.group("pe", [mm(P[6][:, h * NBLK:(h + 1) * NBLK], QT[:, h, c0:c0 + 128], kmT[:, h, :], True, True)
                               for h in range(NHEAD)], R=QT_b + [kmT_b], W=[Pb[6]])
                S.op("dve", lambda e: e.memset(gs[i][:], -1e30), W=[gs_b[i]])
                S.op("dve", lambda e: e.tensor_copy(
                    out=gs[i][:, :, 0:qb], in_=P[6][:, 0:NHEAD * NBLK].rearrange("p (h n) -> p h n", n=NBLK)[:, :, 0:qb]),
                    R=[Pb[6]], W=[gs_b[i]])
                for h in range(NHEAD):
                    S.op("dve", lambda e, h=h: e.max(out=mx[i][:, h, :], in_=gs[i][:, h, :]), R=[gs_b[i]], W=[mx_b[i]])
                for h in range(NHEAD):
                    S.op("dve", lambda e, h=h: e.tensor_scalar(out=Am[i][:, h, :], in0=gs[i][:, h, :], scalar1=mx[i][:, h, 2:3],
                                                               scalar2=NEG, op0=ALU.is_lt, op1=ALU.mult),
                         R=[gs_b[i], mx_b[i]], W=[Am_b[i]])

            def tr_part(s4):
                c0 = s4 * 128
                i = s4 % 2
                for g in range(2):
                    pp, ppb = P[7], Pb[7]
                    S.group("pe", [(lambda e, j=j: e.transpose(pp[0:8, j * 128:(j + 1) * 128], Am[i][:, g * 4 + j, :], ident32[:]))
                                   for j in range(4)], R=[Am_b[i]], W=[ppb])
                    S.op("act", lambda e: e.activation(
                        out=AT[0:8, g * 4:(g + 1) * 4, c0:c0 + 128], in_=pp[0:8, :].rearrange("p (h t) -> p h t", t=128),
                        func=AF.Copy), R=[ppb], W=[AT_b[s4]])

            for s4 in range(4):
                gate_part(s4)
                if s4 >= 1:
                    tr_part(s4 - 1)
            tr_part(3)

        def inner(t5, hook=None):
            items = []
            for h in range(NHEAD):
                for qb2 in range(2):
                    qb = 2 * t5 + qb2
                    for kt in range(2 * qb + 2):
                        items.append((h, qb2, kt))
            SK = 3
            SB_ = [0, 1, 6, 7]
            st_items = {}

            def emit_qk(idx):
                h, qb2, kt = items[idx]
                qb = 2 * t5 + qb2
                qo = qb2 * 256
                far = kt <= 2 * qb - 2
                bk = SB_[idx % 4]
                ps, psb = P[bk], Pb[bk]
                ops = [(KT[:, h, kt * 128:(kt + 1) * 128], QT[:, h, qo:qo + 256])]
                R = [KT_b[h][kt // 4], QT_b[h]]
                if kt < 2 * qb and qb >= 4:
                    ops.append((E8[0:8, kt // 2, :], AT[0:8, h, qo:qo + 256]))
                    R += [AT_b[2 * qb2], AT_b[2 * qb2 + 1]]
                if not far:
                    off = {2 * qb - 1: 256, 2 * qb: 128, 2 * qb + 1: 0}[kt]
                    ops.append((identb[:], M[:, h, off:off + 256]))
                S.group("pe", [mm(ps[:, 0:256], a, b, i == 0, i == len(ops) - 1) for i, (a, b) in enumerate(ops)],
                        R=R, W=[psb])
                i4 = idx % len(PT)
                bias_ap = cb[:, h:h + 1] if far else zt[:, 0:1]
                S.op("act", lambda e: e.activation(out=PT[i4][:], in_=ps[:, 0:256], func=AF.Exp, scale=scale, bias=bias_ap),
                     R=[psb], W=[PT_b[i4]])

            def emit_pv(idx):
                h, qb2, kt = items[idx]
                qb = 2 * t5 + qb2
                qo = qb2 * 256
                nkt = 2 * qb + 2
                j = (h * 2 + qb2) % 2
                po, pob, pd, pdb = P[2 + j], Pb[2 + j], P[4 + j], Pb[4 + j]
                i4 = idx % len(PT)
                S.group("pe", [mm(po[:, 0:256], V[:, kt, h * 128:(h + 1) * 128], PT[i4][:], kt == 0, kt == nkt - 1)],
                        R=[V_b[kt], PT_b[i4]], W=[pob])
                S.group("pe", [mm(pd[:, 0:256], K.ones[:], PT[i4][:], kt == 0, kt == nkt - 1)],
                        R=[PT_b[i4]], W=[pdb])
                if kt == nkt - 1:
                    S.op("dve", lambda e: e.reciprocal(out=rden[j][:], in_=pd[:, 0:256]), R=[pdb], W=[pacc_b[j]])
                    S.op("dve", lambda e: e.tensor_tensor(out=aT[:, h, qo:qo + 256], in0=po[:, 0:256], in1=rden[j][:], op=ALU.mult),
                         R=[pob, pacc_b[j]], W=[aT_b[h]])

            hook_at = (len(items) * 3) // 4
            for idx in range(len(items) + SK):
                if idx < len(items):
                    emit_qk(idx)
                if idx >= SK:
                    emit_pv(idx - SK)
                if idx == hook_at and hook is not None:
                    hook()

        def back(t5):
            K.wp.extend([K.dram["wo"][c] for c in range(KC)])
            for c in range(KC):
                wt, wb = K.wp.get()
                pp, ppb = P[6 + c % 2], Pb[6 + c % 2]
                S.group("pe", [mm(pp[:], wt[:, k, :], aT[:, k, :], k == 0, k == KC - 1) for k in range(KC)],
                        R=[wb] + aT_b, W=[ppb])
                K.wp.done()
                pn.evac(c, pp, ppb, stat_bank=5)
            pn.flush()

        front_norm(0)
        front_q(0)
        front_gates(0)
        for t5 in range(NT):
            nxt = t5 + 1 < NT
            inner(t5, hook=(lambda t=t5 + 1: front_norm(t)) if nxt else None)
            back(t5)
            if nxt:
                front_q(t5 + 1)
                front_gates(t5 + 1)
            pn.finish(t5, K.pv, gpost, stat_bank=5)
        S.barrier()
    K.cst_stack.close()

def build_program(stages, winfo):
    _pv_plan()
    nc = bass.Bass("TRN2", target_bir_lowering=False)
    K = Ctx()
    K.nc = nc
    K.dram = {}
    for name, shape in winfo.items():
        K.dram[name] = nc.dram_tensor(name, list(shape), F32, kind="ExternalInput").ap()
    xT = nc.dram_tensor("xT", [D, S_LEN], F32, kind="ExternalInput").ap()
    pvd = nc.dram_tensor("pv", [128, PV_LAYOUT["_ncol"][0]], F32, kind="ExternalInput").ap()
    onesd = nc.dram_tensor("ones", [128, 128], F32, kind="ExternalInput").ap()
    outT = nc.dram_tensor("outT", [D, S_LEN], F32, kind="ExternalOutput").ap()
    K.frep_dram = nc.dram_tensor("frep_scratch", [128, NHEAD * FREP_W], BF16, kind="Internal")
    ncol = PV_LAYOUT["_ncol"][0]
    with ExitStack() as stack:
        K.stack = stack
        S = K.S = Sched(nc, stack)
        K.x = stack.enter_context(SB(nc, "x_res", [128, KC, S_LEN], F32))
        K.x_b = [[Buf(f"x{t}_{c}") for c in range(KC)] for t in range(S_LEN // 512)]
        K.pv = stack.enter_context(SB(nc, "pv_sb", [128, ncol], F32))
        K.hp = stack.enter_context(SB(nc, "hp_sb", [128, ncol], F32))
        K.ones = stack.enter_context(SB(nc, "ones_sb", [128, 128], BF16))
        K.epst = stack.enter_context(SB(nc, "eps_sb", [128, 1], F32))
        K.eps_ap = K.epst[:, 0:1]
        K.sq = stack.enter_context(SB(nc, "sq", [128, KC, 512], BF16))
        K.sq_b = Buf("sq")
        K.srt = stack.enter_context(SB(nc, "srt", [128, 512], F32))
        K.srt_b = Buf("srt")
        K.ps = [stack.enter_context(nc.psum_tensor(f"ps{i}", [128, 512], F32)) for i in range(8)]
        K.ps_b = [Buf(f"ps{i}", excl=True) for i in range(8)]
        K.wsem = [S.new_sem(f"w{i}") for i in range(8)]
        K.onet = stack.enter_context(SB(nc, "one_sb", [128, 1], F32))
        K.one_ap = K.onet[:, 0:1]
        pv_b, ones_b, eps_b, hp_b = Buf("pv"), Buf("ones"), Buf("eps"), Buf("hp")
        S.new_sem("ld_x")
        S.new_sem("ld_c")
        S.new_sem("st_x")
        S.new_sem("ld_c2")
        S.dma("sp", "ld_c", K.pv[:], pvd[:, :], W=[pv_b])
        tokc = S.dma("pool", "ld_c2", K.ones[:], onesd[:, :], W=[ones_b])
        S.op("dve", lambda e: e.memset(K.epst[:], EPS), W=[eps_b])
        S.op("dve", lambda e: e.memset(K.onet[:], 1.0), W=[eps_b])
        S.op("dve", lambda e: e.tensor_scalar(out=K.hp[:], in0=K.pv[:], scalar1=0.5, scalar2=None, op0=ALU.mult),
             R=[pv_b], W=[hp_b])
        xT3 = xT.rearrange("(c p) t -> p c t", p=128)
        for t in range(S_LEN // 512):
            sem = S.new_sem(f"ld_x{t}")
            with nc.allow_non_contiguous_dma(reason="per-token-tile input load (2 KiB runs)"):
                S.dma("sp", sem, K.x[:, :, t * 512:(t + 1) * 512], xT3[:, :, t * 512:(t + 1) * 512], W=K.x_b[t])
        S.barrier()
        for e in ("pe", "act", "dve", "pool"):
            S._wait(e, tokc)
            S._wait(e, hp_b.w)
            S._wait(e, eps_b.w)
        for stg in stages:
            if stg[0] == "ffn":
                _, l, which, TT = stg
                ffn_stage(K, l, which, TT)
            elif stg[0] == "lru":
                lru_stage(K)
            elif stg[0] == "kvnorm":
                kvnorm_stage(K)
            elif stg[0] == "attn":
                attn_stage(K)
            else:
                raise ValueError(stg)
        outT3 = outT.rearrange("(c p) t -> p c t", p=128)
        toks = []
        for t in range(S_LEN // 512):
            sem = S.new_sem(f"st_x{t}")
            with nc.allow_non_contiguous_dma(reason="per-token-tile output store (2 KiB runs)"):
                toks.append(S.dma("sp", sem, outT3[:, :, t * 512:(t + 1) * 512], K.x[:, :, t * 512:(t + 1) * 512], R=K.x_b[t]))
        for tok in toks:
            S._wait("sp", tok)
    return nc


ALL_STAGES = [("ffn", 0, "ffn1", 1024), ("lru",), ("ffn", 0, "ffn2", 1024), ("kvnorm",),
              ("ffn", 1, "ffn1", 512), ("attn",), ("ffn", 1, "ffn2", 1024)]


def stage_inputs(stages, W, C):
    need = []
    for stg in stages:
        if stg[0] == "ffn":
            need += [f"{stg[2]}_wgu{stg[1]}", f"{stg[2]}_wd{stg[1]}"]
        elif stg[0] == "lru":
            need += ["lru_win", "lru_wout", "lru_wg"]
        elif stg[0] == "attn":
            need += ["wq", "wo", "wk", "wv", "rbT"]
    d = {k: W[k] for k in need}
    if any(s[0] == "attn" for s in stages):
        for k in ("oh", "ident", "e8"):
            d[k] = C[k]
    return d


def run_stages(stages, xT_list, pv, W, C):
    use = stage_inputs(stages, W, C)
    nc = build_program(stages, {k: v.shape for k, v in use.items()})
    in_maps = []
    for xT in xT_list:
        m = dict(use)
        m["xT"] = xT
        m["pv"] = pv
        m["ones"] = C["ones"]
        in_maps.append(m)
    res = run_bass_kernel_spmd(nc, in_maps, core_ids=list(range(len(xT_list))))
    return [r["outT"] for r in res.results]


def kernel(**inputs):
    inp = {k: np.asarray(v) for k, v in inputs.items()}
    x = inp["x"].astype(np.float32, copy=False)
    pv = pack_pv(inp)
    W = pack_weights(inp)
    C = make_consts()
    xT = [np.ascontiguousarray(x[b].T) for b in range(NB)]
    outT = run_stages(ALL_STAGES, xT, pv, W, C)
    return np.stack([o.T for o in outT], axis=0).astype(np.float32)
```
